# Optimizing a Trainium2 kernel written in Bass

```python
import math
import jax, jax.numpy as jnp
from jax import lax
import numpy as np


D_MODEL = 2048
BATCH = 16
SEQ = 2048
DEPTH = 2

N_EVEN = (DEPTH + 1) // 2
N_ODD = DEPTH // 2
EPS = 1e-6
CONV_K = 4

LRU_WIDTH = D_MODEL // 2
LRU_BLOCKS = 8
LRU_BLOCK = LRU_WIDTH // LRU_BLOCKS
LRU_C = 8.0

MLSTM_WIDTH = D_MODEL // 2
MLSTM_HEADS = 4
MLSTM_HD = MLSTM_WIDTH // MLSTM_HEADS
MLSTM_CHUNK = 64

IN_COLS = 2 * LRU_WIDTH + 4 * MLSTM_WIDTH + 2 * MLSTM_HEADS
IN_SPLITS = (LRU_WIDTH, 2 * LRU_WIDTH, 2 * LRU_WIDTH + 2 * MLSTM_WIDTH,
             2 * LRU_WIDTH + 3 * MLSTM_WIDTH, 2 * LRU_WIDTH + 4 * MLSTM_WIDTH,
             2 * LRU_WIDTH + 4 * MLSTM_WIDTH + MLSTM_HEADS)

S5_WIDTH = D_MODEL
S5_GROUP = 16
S5_GROUPS = S5_WIDTH // S5_GROUP
S5_STATE = 64
S5_CHUNK = 64

MOE_GROUPS = 4
MOE_PER_GROUP = 4
MOE_EXPERTS = MOE_GROUPS * MOE_PER_GROUP
MOE_TOP_K = 2
MOE_FF = 512

kernel_name = 'hybrid_rglru_mlstm_s5_hmoe'


def rmsnorm(x, g):
    xf = x.astype(jnp.float32)
    y = xf * lax.rsqrt(jnp.mean(xf * xf, axis=-1, keepdims=True) + EPS)
    return y.astype(x.dtype) * g


def causal_dwconv(x, w, b):
    k, s = w.shape[0], x.shape[1]
    xp = jnp.pad(x, ((0, 0), (k - 1, 0), (0, 0)))
    out = b
    for j in range(k):
        out = out + w[j] * xp[:, j:j + s]
    return out


def _lin_combine(e1, e2):
    a1, b1 = e1
    a2, b2 = e2
    return a1 * a2, a2 * b1 + b2


def cmul(ar, ai, br, bi):
    return ar * br - ai * bi, ar * bi + ai * br


def _cplx_combine(e1, e2):
    ar1, ai1, br1, bi1 = e1
    ar2, ai2, br2, bi2 = e2
    ar, ai = cmul(ar1, ai1, ar2, ai2)
    pr, pi = cmul(ar2, ai2, br1, bi1)
    return ar, ai, pr + br2, pi + bi2


def rg_lru(x, w_a, b_a, w_x, b_x, lam):
    bsz, s, width = x.shape
    xf = x.astype(jnp.float32)
    xb = xf.reshape(bsz, s, LRU_BLOCKS, LRU_BLOCK)
    r = jax.nn.sigmoid(jnp.einsum('bsni,nij->bsnj', xb, w_a).reshape(bsz, s, width) + b_a)
    i = jax.nn.sigmoid(jnp.einsum('bsni,nij->bsnj', xb, w_x).reshape(bsz, s, width) + b_x)
    log_a = -LRU_C * r * jax.nn.softplus(-lam.astype(jnp.float32))
    a = jnp.exp(log_a)
    gated = jnp.sqrt(-jnp.expm1(2.0 * log_a)) * (i * xf)
    _, h = lax.associative_scan(_lin_combine, (a, gated), axis=1)
    return h


def mlstm_chunkwise(q, k, v, i_pre, f_pre):
    bsz, s, nh, dh = q.shape
    L = MLSTM_CHUNK
    nc = s // L
    f32 = jnp.float32

    def to_chunks(t):
        t = t.astype(f32).reshape((bsz, nc, L) + t.shape[2:])
        return jnp.moveaxis(t, (1, 3), (0, 2))

    qc = to_chunks(q)
    kc = to_chunks(k) * (dh ** -0.5)
    vc = to_chunks(v)
    ic = to_chunks(i_pre)
    lfc = to_chunks(jax.nn.log_sigmoid(f_pre.astype(f32)))
    causal = jnp.tril(jnp.ones((L, L), dtype=bool))

    def step(carry, inp):
        c_st, n_st, m_st = carry
        qb, kb, vb, ib, lfb = inp
        bcum = jnp.cumsum(lfb, axis=-1)
        d = bcum[..., :, None] - bcum[..., None, :] + ib[..., None, :]
        d = jnp.where(causal, d, -jnp.inf)
        g = bcum + m_st[..., None]
        m_t = jnp.maximum(g, jnp.max(d, axis=-1))
        w_intra = jnp.exp(d - m_t[..., None])
        w_inter = jnp.exp(g - m_t)
        s_qk = jnp.einsum('bhtd,bhjd->bhtj', qb, kb) * w_intra
        num = (jnp.einsum('bhtj,bhjd->bhtd', s_qk, vb)
               + w_inter[..., None] * jnp.einsum('bhde,bhte->bhtd', c_st, qb))
        den = jnp.sum(s_qk, axis=-1) + w_inter * jnp.einsum('bhd,bhtd->bht', n_st, qb)
        h = num / jnp.maximum(jnp.abs(den), jnp.exp(-m_t))[..., None]
        b_last = bcum[..., -1]
        d_end = b_last[..., None] - bcum + ib
        m_new = jnp.maximum(b_last + m_st, jnp.max(d_end, axis=-1))
        w_end = jnp.exp(d_end - m_new[..., None])
        decay = jnp.exp(b_last + m_st - m_new)
        c_new = decay[..., None, None] * c_st + jnp.einsum('bhj,bhjd,bhje->bhde', w_end, vb, kb)
        n_new = decay[..., None] * n_st + jnp.einsum('bhj,bhjd->bhd', w_end, kb)
        return (c_new, n_new, m_new), h

    init = (jnp.zeros((bsz, nh, dh, dh), f32), jnp.zeros((bsz, nh, dh), f32),
            jnp.zeros((bsz, nh), f32))
    _, h = lax.scan(step, init, (qc, kc, vc, ic, lfc))
    return jnp.transpose(h, (1, 0, 3, 2, 4)).reshape(bsz, s, nh, dh)


def rglru_mlstm_mixer(h, w_in, lru_conv_w, lru_conv_b, lru_w_a, lru_b_a, lru_w_x, lru_b_x,
                      lru_lam, m_conv_w, m_conv_b, m_i_bias, m_f_bias, m_head_g, w_out):
    bsz, s, _ = h.shape
    f32 = jnp.float32
    p = h @ w_in
    lru_x, lru_z, m_qk, m_v, m_o, m_i, m_f = jnp.split(p, IN_SPLITS, axis=-1)
    y_a = rg_lru(causal_dwconv(lru_x, lru_conv_w, lru_conv_b), lru_w_a, lru_b_a,
                 lru_w_x, lru_b_x, lru_lam) * jax.nn.gelu(lru_z.astype(f32))
    qk = jax.nn.silu(causal_dwconv(m_qk, m_conv_w, m_conv_b))
    q, k = jnp.split(qk, 2, axis=-1)

    def heads(t):
        return t.reshape(bsz, s, MLSTM_HEADS, MLSTM_HD)

    i_pre = m_i.astype(f32) + m_i_bias
    f_pre = m_f.astype(f32) + m_f_bias
    hm = mlstm_chunkwise(heads(q), heads(k), heads(m_v), i_pre, f_pre)
    hm = jax.nn.sigmoid(heads(m_o).astype(f32)) * hm
    hm = hm * lax.rsqrt(jnp.mean(hm * hm, axis=-1, keepdims=True) + EPS)
    y_b = hm.reshape(bsz, s, MLSTM_WIDTH) * m_head_g
    y = jnp.concatenate([y_a, y_b], axis=-1).astype(h.dtype)
    return (y @ w_out).astype(h.dtype)


def s5_mixer(h, w_in, a_re, a_im, log_step, b_re, b_im, c_re, c_im, d_skip, w_glu_v, w_glu_g):
    bsz, s, _ = h.shape
    f32 = jnp.float32
    nc = s // S5_CHUNK
    u = (h @ w_in).astype(f32).reshape(bsz, nc, S5_CHUNK, S5_GROUPS, S5_GROUP)
    u = jnp.swapaxes(u, 0, 1)
    a_re = a_re.astype(f32)
    a_im = a_im.astype(f32)
    dt = jnp.exp(log_step.astype(f32))[:, None]
    mag = jnp.exp(a_re * dt)
    lr, li = mag * jnp.cos(a_im * dt), mag * jnp.sin(a_im * dt)
    den = a_re * a_re + a_im * a_im
    fr = ((lr - 1.0) * a_re + li * a_im) / den
    fi = (li * a_re - (lr - 1.0) * a_im) / den
    bbr, bbi = cmul(fr[..., None], fi[..., None], b_re.astype(f32), b_im.astype(f32))
    c_re = c_re.astype(f32)
    c_im = c_im.astype(f32)
    dsk = d_skip.astype(f32).reshape(S5_GROUPS, S5_GROUP)

    def step(carry, ub):
        xr0, xi0 = carry
        bur = jnp.einsum('blgc,gpc->blgp', ub, bbr)
        bui = jnp.einsum('blgc,gpc->blgp', ub, bbi)
        ar = jnp.broadcast_to(lr, bur.shape)
        ai = jnp.broadcast_to(li, bur.shape)
        pr, pi, sr, si = lax.associative_scan(_cplx_combine, (ar, ai, bur, bui), axis=1)
        cr, ci = cmul(pr, pi, xr0[:, None], xi0[:, None])
        xr = sr + cr
        xi = si + ci
        y = jnp.einsum('blgp,gcp->blgc', xr, c_re) - jnp.einsum('blgp,gcp->blgc', xi, c_im)
        return (xr[:, -1], xi[:, -1]), y + dsk * ub

    init = (jnp.zeros((bsz, S5_GROUPS, S5_STATE), f32), jnp.zeros((bsz, S5_GROUPS, S5_STATE), f32))
    _, y = lax.scan(step, init, u)
    y = jax.nn.gelu(jnp.swapaxes(y, 0, 1).reshape(bsz, s, S5_WIDTH)).astype(h.dtype)
    return ((y @ w_glu_v) * jax.nn.sigmoid(y @ w_glu_g)).astype(h.dtype)


def hier_moe(h, w_coarse, b_coarse, w_fine, b_fine, w_gate, w_up, w_down):
    bsz, s, d = h.shape
    f32 = jnp.float32
    t = h.reshape(-1, d)
    lc = (t @ w_coarse).astype(f32) + b_coarse
    pc = jax.nn.softmax(lc, axis=-1)
    g_idx = jnp.argmax(lc, axis=-1)
    p_g = jnp.take_along_axis(pc, g_idx[:, None], axis=-1)
    lf = ((t @ w_fine).astype(f32) + b_fine).reshape(-1, MOE_GROUPS, MOE_PER_GROUP)
    lf_sel = jnp.take_along_axis(lf, g_idx[:, None, None], axis=1)[:, 0]
    top_v, top_i = lax.top_k(lf_sel, MOE_TOP_K)
    w2 = jax.nn.softmax(top_v, axis=-1) * p_g
    expert_id = g_idx[:, None] * MOE_PER_GROUP + top_i
    gates = jnp.sum(jax.nn.one_hot(expert_id, MOE_EXPERTS, dtype=f32) * w2[..., None], axis=1)
    y = jnp.zeros(t.shape, f32)
    for e in range(MOE_EXPERTS):
        he = jax.nn.silu(t @ w_gate[e]) * (t @ w_up[e])
        y = y + gates[:, e:e + 1] * (he @ w_down[e])
    return y.reshape(bsz, s, d).astype(h.dtype)


def setup_inputs(seed: int = 0) -> dict:
    key = jax.random.key(seed)
    ks = iter(jax.random.split(key, 64))
    f32 = jnp.float32
    NE, NO = N_EVEN, N_ODD

    def nrm(shape, scale):
        return jax.random.normal(next(ks), shape, f32) * scale

    x = nrm((BATCH, SEQ, D_MODEL), 1.0)
    norm_mix = 1.0 + nrm((DEPTH, D_MODEL), 0.02)
    norm_ffn = 1.0 + nrm((DEPTH, D_MODEL), 0.02)
    norm_final = 1.0 + nrm((D_MODEL,), 0.02)
    ab_w_in = nrm((NE, D_MODEL, IN_COLS), D_MODEL ** -0.5)
    lru_conv_w = nrm((NE, CONV_K, LRU_WIDTH), CONV_K ** -0.5)
    lru_conv_b = nrm((NE, LRU_WIDTH), 0.01)
    lru_w_a = nrm((NE, LRU_BLOCKS, LRU_BLOCK, LRU_BLOCK), LRU_BLOCK ** -0.5)
    lru_b_a = nrm((NE, LRU_WIDTH), 0.01)
    lru_w_x = nrm((NE, LRU_BLOCKS, LRU_BLOCK, LRU_BLOCK), LRU_BLOCK ** -0.5)
    lru_b_x = nrm((NE, LRU_WIDTH), 0.01)
    u = jax.random.uniform(next(ks), (NE, LRU_WIDTH), f32, 0.9, 0.999)
    a0 = u ** (1.0 / LRU_C)
    lru_lam = jnp.log(a0) - jnp.log1p(-a0)
    m_conv_w = nrm((NE, CONV_K, 2 * MLSTM_WIDTH), CONV_K ** -0.5)
    m_conv_b = nrm((NE, 2 * MLSTM_WIDTH), 0.01)
    m_i_bias = nrm((NE, MLSTM_HEADS), 0.1)
    m_f_bias = jnp.linspace(3.0, 6.0, MLSTM_HEADS, dtype=f32)[None] + nrm((NE, MLSTM_HEADS), 0.1)
    m_head_g = 1.0 + nrm((NE, MLSTM_WIDTH), 0.02)
    ab_w_out = nrm((NE, D_MODEL, D_MODEL), D_MODEL ** -0.5)
    s5_w_in = nrm((NO, D_MODEL, S5_WIDTH), D_MODEL ** -0.5)
    n_idx = jnp.arange(S5_STATE, dtype=f32)
    s5_a_re = -0.5 + nrm((NO, S5_GROUPS, S5_STATE), 0.01)
    s5_a_im = math.pi * n_idx + nrm((NO, S5_GROUPS, S5_STATE), 0.01)
    s5_log_step = jax.random.uniform(next(ks), (NO, S5_GROUPS), f32, math.log(0.001), math.log(0.1))
    s5_b_re = nrm((NO, S5_GROUPS, S5_STATE, S5_GROUP), S5_GROUP ** -0.5)
    s5_b_im = nrm((NO, S5_GROUPS, S5_STATE, S5_GROUP), S5_GROUP ** -0.5)
    s5_c_re = nrm((NO, S5_GROUPS, S5_GROUP, S5_STATE), S5_STATE ** -0.5)
    s5_c_im = nrm((NO, S5_GROUPS, S5_GROUP, S5_STATE), S5_STATE ** -0.5)
    s5_d = nrm((NO, S5_WIDTH), 1.0)
    s5_w_glu_v = nrm((NO, S5_WIDTH, D_MODEL), S5_WIDTH ** -0.5)
    s5_w_glu_g = nrm((NO, S5_WIDTH, D_MODEL), S5_WIDTH ** -0.5)
    moe_w_coarse = nrm((DEPTH, D_MODEL, MOE_GROUPS), D_MODEL ** -0.5)
    moe_b_coarse = nrm((DEPTH, MOE_GROUPS), 0.01)
    moe_w_fine = nrm((DEPTH, D_MODEL, MOE_EXPERTS), D_MODEL ** -0.5)
    moe_b_fine = nrm((DEPTH, MOE_EXPERTS), 0.01)
    moe_w_gate = nrm((DEPTH, MOE_EXPERTS, D_MODEL, MOE_FF), D_MODEL ** -0.5)
    moe_w_up = nrm((DEPTH, MOE_EXPERTS, D_MODEL, MOE_FF), D_MODEL ** -0.5)
    moe_w_down = nrm((DEPTH, MOE_EXPERTS, MOE_FF, D_MODEL), MOE_FF ** -0.5)
    return {'x': x, 'norm_mix': norm_mix, 'norm_ffn': norm_ffn, 'norm_final': norm_final,
            'ab_w_in': ab_w_in, 'lru_conv_w': lru_conv_w, 'lru_conv_b': lru_conv_b,
            'lru_w_a': lru_w_a, 'lru_b_a': lru_b_a, 'lru_w_x': lru_w_x, 'lru_b_x': lru_b_x,
            'lru_lam': lru_lam, 'm_conv_w': m_conv_w, 'm_conv_b': m_conv_b,
            'm_i_bias': m_i_bias, 'm_f_bias': m_f_bias, 'm_head_g': m_head_g, 'ab_w_out': ab_w_out,
            's5_w_in': s5_w_in, 's5_a_re': s5_a_re, 's5_a_im': s5_a_im, 's5_log_step': s5_log_step,
            's5_b_re': s5_b_re, 's5_b_im': s5_b_im, 's5_c_re': s5_c_re, 's5_c_im': s5_c_im,
            's5_d': s5_d, 's5_w_glu_v': s5_w_glu_v, 's5_w_glu_g': s5_w_glu_g,
            'moe_w_coarse': moe_w_coarse, 'moe_b_coarse': moe_b_coarse,
            'moe_w_fine': moe_w_fine, 'moe_b_fine': moe_b_fine,
            'moe_w_gate': moe_w_gate, 'moe_w_up': moe_w_up, 'moe_w_down': moe_w_down}


def reference(x, norm_mix, norm_ffn, norm_final, ab_w_in, lru_conv_w, lru_conv_b, lru_w_a, lru_b_a,
              lru_w_x, lru_b_x, lru_lam, m_conv_w, m_conv_b, m_i_bias, m_f_bias, m_head_g, ab_w_out,
              s5_w_in, s5_a_re, s5_a_im, s5_log_step, s5_b_re, s5_b_im, s5_c_re, s5_c_im, s5_d,
              s5_w_glu_v, s5_w_glu_g, moe_w_coarse, moe_b_coarse, moe_w_fine, moe_b_fine,
              moe_w_gate, moe_w_up, moe_w_down):
    for layer in range(DEPTH):
        j = layer // 2
        h = rmsnorm(x, norm_mix[layer])
        if layer % 2 == 0:
            x = x + rglru_mlstm_mixer(h, ab_w_in[j], lru_conv_w[j], lru_conv_b[j], lru_w_a[j],
                                      lru_b_a[j], lru_w_x[j], lru_b_x[j], lru_lam[j], m_conv_w[j],
                                      m_conv_b[j], m_i_bias[j], m_f_bias[j], m_head_g[j], ab_w_out[j])
        else:
            x = x + s5_mixer(h, s5_w_in[j], s5_a_re[j], s5_a_im[j], s5_log_step[j], s5_b_re[j],
                             s5_b_im[j], s5_c_re[j], s5_c_im[j], s5_d[j], s5_w_glu_v[j], s5_w_glu_g[j])
        x = x + hier_moe(rmsnorm(x, norm_ffn[layer]), moe_w_coarse[layer], moe_b_coarse[layer],
                         moe_w_fine[layer], moe_b_fine[layer], moe_w_gate[layer], moe_w_up[layer],
                         moe_w_down[layer])
    return rmsnorm(x, norm_final)
```

```python
import numpy as np
from contextlib import ExitStack
import concourse.bass as bass
import concourse.mybir as mybir
from concourse.bass_utils import run_bass_kernel_spmd

F32 = mybir.dt.float32
BF16 = mybir.dt.bfloat16
ALU = mybir.AluOpType
AF = mybir.ActivationFunctionType
AX = mybir.AxisListType


class Buf:
    __slots__ = ("name", "lw", "rd")

    def __init__(self, name):
        self.name = name
        self.lw = None
        self.rd = []


class SemKey:
    __slots__ = ("name", "sem", "count", "group", "epoch", "totals")

    def __init__(self, name, group=False):
        self.name = name
        self.sem = None
        self.count = 0
        self.group = group


class Op:
    __slots__ = ("eng", "fn", "deps", "sig", "signo", "key", "idx", "ep")

    def __init__(self, eng, fn, key=None):
        self.eng = eng
        self.fn = fn
        self.deps = []
        self.sig = False
        self.signo = 0
        self.key = key
        self.idx = 0


ENGS = ("pe", "act", "dve", "pool", "sp")


class Prog:
    def __init__(self, nc):
        self.nc = nc
        self.stack = ExitStack()
        self.ops = {e: [] for e in ENGS}
        self.nops = 0
        self.keys = []
        self.ntile = 0
        self.bar_idx = 0
        self.out_ops = []
        self.stage_stacks = []
        self._uid = 0
        self._dbufs = {}
        self._keyreg = {}

    def uid(self):
        self._uid += 1
        return self._uid

    def dram_buf(self, ap, r0):
        k = (ap.tensor.name, r0)
        if k not in self._dbufs:
            self._dbufs[k] = Buf(f"d_{k}")
        return self._dbufs[k]

    def finish(self):
        op = Op("sp", None)
        op.idx = self.nops
        self.nops += 1
        op.deps = list(self.out_ops)
        self.ops["sp"].append(op)

    def sb(self, shape, dtype, name=None):
        self.ntile += 1
        name = name or f"t{self.ntile}"
        return self.stack.enter_context(self.nc.sbuf_tensor(name, list(shape), dtype))

    def ps(self, shape, dtype, name=None):
        self.ntile += 1
        name = name or f"p{self.ntile}"
        return self.stack.enter_context(self.nc.psum_tensor(name, list(shape), dtype))

    def key(self, name, group=False):
        if name in self._keyreg:
            k = self._keyreg[name]
            assert k.group == group
            return k
        k = SemKey(name, group)
        k.epoch = 0
        k.totals = {}
        self._keyreg[name] = k
        self.keys.append(k)
        return k

    def close_epochs(self):
        for k in self.keys:
            if k.group:
                k.totals[k.epoch] = k.count
                k.epoch += 1

    def _deps(self, op, reads, writes):
        deps = []
        for b in reads:
            if b.lw is not None:
                deps.append(b.lw)
        for b in writes:
            if b.lw is not None:
                deps.append(b.lw)
            deps.extend(b.rd)
        for b in reads:
            b.rd.append(op)
        for b in writes:
            b.lw = op
            b.rd = []
        seen = set()
        for d in deps:
            if d is op or id(d) in seen:
                continue
            seen.add(id(d))
            if d.key is None and d.eng == "pe" and op.eng == "pe" and op.key is None:
                continue
            op.deps.append(d)
            d.sig = True

    def emit(self, eng, fn, reads=(), writes=()):
        op = Op(eng, fn)
        op.idx = self.nops
        self.nops += 1
        self._deps(op, reads, writes)
        self.ops[eng].append(op)
        return op

    def dma(self, eng, out, in_, key, reads=(), writes=(), **kw):
        def fn(e, out=out, in_=in_, kw=kw):
            return e.dma_start(out=out, in_=in_, **kw)
        op = Op(eng, fn, key=key)
        op.idx = self.nops
        self.nops += 1
        self._deps(op, reads, writes)
        key.count += 16
        op.signo = key.count
        op.ep = key.epoch
        op.sig = True
        self.ops[eng].append(op)
        return op

    def build(self):
        nc = self.nc
        st = self.stack
        self.close_epochs()
        esem = {e: st.enter_context(nc.semaphore(f"s_{e}")) for e in ENGS}
        for k in self.keys:
            if k.count > 0:
                k.sem = st.enter_context(nc.semaphore(f"k_{k.name}"))
        for e in ENGS:
            c = 0
            for op in self.ops[e]:
                if op.key is None and op.sig:
                    c += 1
                    op.signo = c
        ops = self.ops

        def run(ename, eng):
            waited = {}
            for op in ops[ename]:
                need = {}
                for d in op.deps:
                    if d.key is not None:
                        s = d.key.sem
                        v = d.key.totals[d.ep] if d.key.group else d.signo
                    else:
                        s = esem[d.eng]
                        v = d.signo
                    sid = id(s)
                    if v > need.get(sid, (None, 0))[1]:
                        need[sid] = (s, v)
                for sid, (s, v) in need.items():
                    if waited.get(sid, 0) >= v:
                        continue
                    waited[sid] = v
                    eng.wait_ge(s, v)
                if op.fn is None:
                    continue
                ins = op.fn(eng)
                if op.key is not None:
                    ins.then_inc(op.key.sem, 16)
                elif op.sig:
                    ins.then_inc(esem[ename], 1)

        block = st.enter_context(nc.Block())

        @block.tensor
        def _(e):
            run("pe", e)

        @block.scalar
        def _(e):
            run("act", e)

        @block.vector
        def _(e):
            run("dve", e)

        @block.gpsimd
        def _(e):
            run("pool", e)

        @block.sync
        def _(e):
            run("sp", e)

    def close(self):
        self.stack.close()
        for st in reversed(self.stage_stacks):
            st.close()


D = 2048
EPS = 1e-6
NEXP = 16
FF = 512


class Ctx:
    pass


def barrier(P):
    lasts = []
    for e in ENGS:
        if P.ops[e]:
            for op in reversed(P.ops[e]):
                if op.fn is not None and op.key is None:
                    lasts.append(op)
                    op.sig = True
                    break
    dmas = [op for e in ENGS for op in P.ops[e] if op.key is not None and op.idx >= P.bar_idx]
    for e in ENGS:
        op = Op(e, None)
        op.idx = P.nops
        P.nops += 1
        op.deps = [d for d in lasts] + dmas
        P.ops[e].append(op)
    P.bar_idx = P.nops
    P.close_epochs()


def rmsnorm_rows(P, src, dst, gbc, b_src, b_dst, b_g, scr, tag):
    junk, ssq, rstd, b_junk, b_ssq, b_rstd = scr
    P.emit("act", lambda e: e.activation(out=junk, in_=src, func=AF.Square, accum_out=ssq),
           reads=[b_src], writes=[b_junk, b_ssq])
    P.emit("dve", lambda e: e.tensor_scalar(out=rstd, in0=ssq, scalar1=1.0 / D, scalar2=EPS,
                                            op0=ALU.mult, op1=ALU.add), reads=[b_ssq], writes=[b_rstd])
    P.emit("act", lambda e: e.activation(out=rstd, in_=rstd, func=AF.Sqrt), reads=[b_rstd], writes=[b_rstd])
    P.emit("dve", lambda e: e.reciprocal(out=rstd, in_=rstd), reads=[b_rstd], writes=[b_rstd])
    P.emit("dve", lambda e: e.scalar_tensor_tensor(out=dst, in0=src, scalar=rstd, in1=gbc,
                                                   op0=ALU.mult, op1=ALU.mult),
           reads=[b_src, b_rstd, b_g], writes=[b_dst])


def moe_stage(P, xin, xout, T, g_ffn, w_coarse, b_coarse, w_fine, b_fine, w_gate, w_up, w_down,
              g_final=None, nexp=NEXP, TT=1024):
    nc = P.nc
    NS = TT // 128
    NH = TT // 512
    ntiles = T // TT
    st = ExitStack()
    sbuf = lambda shape, dt, name: st.enter_context(nc.sbuf_tensor(name, list(shape), dt))
    psum = lambda shape, dt, name: st.enter_context(nc.psum_tensor(name, list(shape), dt))
    u = P.uid()

    yacc = sbuf([128, NS, D], F32, f"yacc{u}")
    hT = sbuf([128, 16, TT], BF16, f"hT{u}")
    hn = sbuf([128, D], BF16, f"hn{u}")
    gbc = sbuf([128, D], F32, f"gbc{u}")
    NSLOT = 5 if g_final is None else 4
    ring = [sbuf([128, 16 * 512], BF16, f"ring{u}_{i}") for i in range(NSLOT)]
    sg = [sbuf([128, 512], F32, f"sg{u}_{i}") for i in range(2)]
    he = sbuf([128, 4, TT], BF16, f"he{u}")
    wr = sbuf([128, 16, 20], BF16, f"wr{u}")
    brc = sbuf([128, 20], F32, f"brc{u}")
    ident = sbuf([128, 128], BF16, f"ident{u}")
    gates = sbuf([128, NS, 16], F32, f"gates{u}")
    sm = sbuf([128, NS, 64], F32, f"sm{u}")
    ssq = sbuf([128, NS], F32, f"ssq{u}")
    rstd = sbuf([128, NS], F32, f"rstd{u}")
    if g_final is not None:
        gfin = sbuf([128, D], F32, f"gfin{u}")

    psG = [psum([128, 512], F32, f"psG{u}_{i}") for i in range(2)]
    psU = [psum([128, 512], F32, f"psU{u}_{i}") for i in range(2)]
    psY = [psum([128, 512], F32, f"psY{u}_{i}") for i in range(2)]
    psT = [psum([128, 8, 128], BF16, f"psT{u}_{i}") for i in range(2)]

    B = lambda n: Buf(f"{n}{u}")
    b_yacc = [B(f"yacc{s}") for s in range(NS)]
    b_hT = [B(f"hT{s}") for s in range(NS)]
    b_hn, b_g, b_wr, b_br, b_id, b_he = B("hn"), B("g"), B("wr"), B("br"), B("id"), B("he")
    b_ring = [B(f"ring{i}") for i in range(NSLOT)]
    b_sg = [B("sg0"), B("sg1")]
    b_gates = [B(f"gates{s}") for s in range(NS)]
    b_sm = [B(f"sm{s}") for s in range(NS)]
    b_ssq = [B(f"ssq{s}") for s in range(NS)]
    b_rstd = [B(f"rstd{s}") for s in range(NS)]
    b_psG, b_psU, b_psY, b_psT = [B("pg0"), B("pg1")], [B("pu0"), B("pu1")], [B("py0"), B("py1")], [B("pt0"), B("pt1")]
    k_c = P.key(f"mc", group=True)
    k_cp = P.key(f"mcp", group=True)
    k_x = [P.key(f"mx_{s}") for s in range(NS)]
    k_ring = [P.key(f"mr_{i}") for i in range(NSLOT)]

    P.dma("sp", gbc[:], g_ffn.partition_broadcast(128), k_c, writes=[b_g])
    b_br2, b_wr2 = B("br2"), B("wr2")
    P.dma("sp", brc[:, 0:4], b_coarse.partition_broadcast(128), k_c, writes=[b_br])
    P.dma("sp", brc[:, 4:20], b_fine.partition_broadcast(128), k_c, writes=[b_br2])
    P.dma("pool", wr[:, :, 0:4], w_coarse.rearrange("(kc p) n -> p kc n", p=128), k_cp, writes=[b_wr])
    P.dma("pool", wr[:, :, 4:20], w_fine.rearrange("(kc p) n -> p kc n", p=128), k_cp, writes=[b_wr2])
    if g_final is not None:
        b_gf = B("gf")
        P.dma("sp", gfin[:], g_final.partition_broadcast(128), k_c, writes=[b_gf])
    P.emit("pool", lambda e: e.memset(ident[:], 1.0), writes=[b_id])
    P.emit("pool", lambda e: e.affine_select(out=ident[:], in_=ident[:], pattern=[[-1, 128]],
                                              compare_op=ALU.is_equal, fill=0.0, base=0,
                                              channel_multiplier=1), reads=[b_id], writes=[b_id])

    wseq = []
    for t in range(ntiles):
        for ex in range(nexp):
            for which in range(3):
                wseq.append((t, ex, which))
    wslot = {}
    state = {"next": 0}

    def issue_w(upto):
        while state["next"] < min(upto, len(wseq)):
            i = state["next"]
            t, ex, which = wseq[i]
            sl = i % NSLOT
            wslot[(t, ex, which)] = sl
            if which < 2:
                src = (w_gate if which == 0 else w_up)[ex].rearrange("(kc p) n -> p kc n", p=128)
                dst = ring[sl][:, :].rearrange("p (kc n) -> p kc n", kc=16)
            else:
                src = w_down[ex].rearrange("(f p) n -> p f n", p=128)
                dst = ring[sl][:, :].rearrange("p (f n) -> p f n", f=4)
            P.dma("pool", dst, src, k_ring[sl], writes=[b_ring[sl]])
            state["next"] += 1

    issue_w(NSLOT)
    gcount = 0
    ycount = 0
    for t in range(ntiles):
        for s in range(NS):
            r0 = t * TT + s * 128
            P.dma("sp", yacc[:, s, :], xin[r0:r0 + 128, :], k_x[s], reads=[P.dram_buf(xin, r0)], writes=[b_yacc[s]])
        for s in range(NS):
            rmsnorm_rows(P, yacc[:, s, :], hn[:], gbc[:], b_yacc[s], b_hn, b_g,
                         (hn[:], ssq[:, s:s + 1], rstd[:, s:s + 1], b_hn, b_ssq[s], b_rstd[s]), "m")
            for half in range(2):
                pt = psT[half]
                for j in range(8):
                    kc = half * 8 + j
                    P.emit("pe", lambda e, pt=pt, j=j, kc=kc: e.transpose(
                        out=pt[:, j, :], in_=hn[:, kc * 128:(kc + 1) * 128], identity=ident[:]),
                        reads=[b_hn, b_id], writes=[b_psT[half]])
                eng = "act" if half == 0 else "dve"
                if eng == "act":
                    P.emit("act", lambda e, pt=pt, half=half, s=s: e.copy(
                        out=hT[:, half * 8:half * 8 + 8, s * 128:(s + 1) * 128], in_=pt[:, :, :]),
                        reads=[b_psT[half]], writes=[b_hT[s]])
                else:
                    P.emit("dve", lambda e, pt=pt, half=half, s=s: e.tensor_copy(
                        out=hT[:, half * 8:half * 8 + 8, s * 128:(s + 1) * 128], in_=pt[:, :, :]),
                        reads=[b_psT[half]], writes=[b_hT[s]])
            pr = psY[1]
            for kc in range(16):
                P.emit("pe", lambda e, kc=kc, s=s, pr=pr: e.matmul(
                    out=pr[:, 0:20], lhsT=hT[:, kc, s * 128:(s + 1) * 128], rhs=wr[:, kc, :],
                    start=(kc == 0), stop=(kc == 15)),
                    reads=[b_hT[s], b_wr, b_wr2], writes=[b_psY[1]])
            S = sm[:, s, :]
            lg, gmax, ohg, ngmax, ex4, sume, pg = S[:, 0:20], S[:, 20:21], S[:, 21:25], S[:, 25:26], S[:, 26:30], S[:, 30:31], S[:, 31:32]
            lfs, m1, mk1, lf2, m2, mk2 = S[:, 32:36], S[:, 36:37], S[:, 37:41], S[:, 41:45], S[:, 45:46], S[:, 46:50]
            d21, e2, den, wa, wb, gsel = S[:, 50:51], S[:, 51:52], S[:, 52:53], S[:, 53:54], S[:, 54:55], S[:, 55:59]
            bs = b_sm[s]

            def dv(fn, extra_r=(), extra_w=()):
                P.emit("dve", fn, reads=[bs] + list(extra_r), writes=[bs] + list(extra_w))

            def ac(fn):
                P.emit("act", fn, reads=[bs], writes=[bs])
            dv(lambda e, lg=lg, pr=pr: e.tensor_tensor(out=lg, in0=pr[:, 0:20], in1=brc[:], op=ALU.add),
               extra_r=[b_psY[1], b_br, b_br2])
            dv(lambda e, lg=lg, gmax=gmax: e.reduce_max(out=gmax, in_=lg[:, 0:4], axis=AX.X))
            dv(lambda e, lg=lg, gmax=gmax, ohg=ohg: e.tensor_scalar(out=ohg, in0=lg[:, 0:4], scalar1=gmax, scalar2=None, op0=ALU.is_ge))
            dv(lambda e, gmax=gmax, ngmax=ngmax: e.tensor_scalar(out=ngmax, in0=gmax, scalar1=-1.0, scalar2=None, op0=ALU.mult))
            ac(lambda e, lg=lg, ngmax=ngmax, ex4=ex4, sume=sume: e.activation(out=ex4, in_=lg[:, 0:4], func=AF.Exp, bias=ngmax, scale=1.0, accum_out=sume))
            dv(lambda e, pg=pg, sume=sume: e.reciprocal(out=pg, in_=sume))
            dv(lambda e, lg=lg, lfs=lfs, ohg=ohg: e.tensor_scalar(out=lfs, in0=lg[:, 4:8], scalar1=ohg[:, 0:1], scalar2=None, op0=ALU.mult))
            for g in range(1, 4):
                dv(lambda e, lg=lg, lfs=lfs, ohg=ohg, g=g: e.scalar_tensor_tensor(
                    out=lfs, in0=lg[:, 4 + 4 * g:8 + 4 * g], scalar=ohg[:, g:g + 1], in1=lfs, op0=ALU.mult, op1=ALU.add))
            dv(lambda e, lfs=lfs, m1=m1: e.reduce_max(out=m1, in_=lfs, axis=AX.X))
            dv(lambda e, lfs=lfs, m1=m1, mk1=mk1: e.tensor_scalar(out=mk1, in0=lfs, scalar1=m1, scalar2=None, op0=ALU.is_ge))
            dv(lambda e, lfs=lfs, lf2=lf2, mk1=mk1: e.scalar_tensor_tensor(out=lf2, in0=mk1, scalar=-1e30, in1=lfs, op0=ALU.mult, op1=ALU.add))
            dv(lambda e, lf2=lf2, m2=m2: e.reduce_max(out=m2, in_=lf2, axis=AX.X))
            dv(lambda e, lf2=lf2, m2=m2, mk2=mk2: e.tensor_scalar(out=mk2, in0=lf2, scalar1=m2, scalar2=None, op0=ALU.is_ge))
            dv(lambda e, d21=d21, m1=m1, m2=m2: e.tensor_tensor(out=d21, in0=m2, in1=m1, op=ALU.subtract))
            ac(lambda e, d21=d21, e2=e2: e.activation(out=e2, in_=d21, func=AF.Exp))
            dv(lambda e, e2=e2, den=den: e.tensor_scalar(out=den, in0=e2, scalar1=1.0, scalar2=None, op0=ALU.add))
            dv(lambda e, den=den: e.reciprocal(out=den, in_=den))
            dv(lambda e, den=den, pg=pg, wa=wa: e.tensor_tensor(out=wa, in0=den, in1=pg, op=ALU.mult))
            dv(lambda e, wa=wa, wb=wb, e2=e2: e.tensor_tensor(out=wb, in0=wa, in1=e2, op=ALU.mult))
            dv(lambda e, gsel=gsel, mk1=mk1, wa=wa: e.tensor_scalar(out=gsel, in0=mk1, scalar1=wa, scalar2=None, op0=ALU.mult))
            dv(lambda e, gsel=gsel, mk2=mk2, wb=wb: e.scalar_tensor_tensor(out=gsel, in0=mk2, scalar=wb, in1=gsel, op0=ALU.mult, op1=ALU.add))
            for g in range(4):
                dv(lambda e, gsel=gsel, ohg=ohg, g=g, s=s: e.tensor_scalar(
                    out=gates[:, s, 4 * g:4 * g + 4], in0=gsel, scalar1=ohg[:, g:g + 1], scalar2=None, op0=ALU.mult),
                    extra_w=[b_gates[s]])

        for ex in range(nexp):
            wi = (t * nexp + ex) * 3
            issue_w(wi + NSLOT)
            sg_, su_, sd_ = wslot[(t, ex, 0)], wslot[(t, ex, 1)], wslot[(t, ex, 2)]
            Wg = ring[sg_][:, :].rearrange("p (kc n) -> p kc n", kc=16)
            Wu = ring[su_][:, :].rearrange("p (kc n) -> p kc n", kc=16)
            Wd = ring[sd_][:, :].rearrange("p (f n) -> p f n", f=4)
            for f in range(4):
                for h in range(NH):
                    gi = gcount % 2
                    gcount += 1
                    for kc in range(16):
                        P.emit("pe", lambda e, kc=kc, f=f, h=h, gi=gi, Wg=Wg: e.matmul(
                            out=psG[gi][:, :], lhsT=Wg[:, kc, f * 128:(f + 1) * 128],
                            rhs=hT[:, kc, h * 512:(h + 1) * 512], start=(kc == 0), stop=(kc == 15)),
                            reads=[b_ring[sg_]] + b_hT[h * 4:(h + 1) * 4], writes=[b_psG[gi]])
                    for kc in range(16):
                        P.emit("pe", lambda e, kc=kc, f=f, h=h, gi=gi, Wu=Wu: e.matmul(
                            out=psU[gi][:, :], lhsT=Wu[:, kc, f * 128:(f + 1) * 128],
                            rhs=hT[:, kc, h * 512:(h + 1) * 512], start=(kc == 0), stop=(kc == 15)),
                            reads=[b_ring[su_]] + b_hT[h * 4:(h + 1) * 4], writes=[b_psU[gi]])
                    P.emit("act", lambda e, gi=gi: e.activation(out=sg[gi][:], in_=psG[gi][:], func=AF.Silu),
                           reads=[b_psG[gi]], writes=[b_sg[gi]])
                    P.emit("dve", lambda e, gi=gi, f=f, h=h: e.tensor_tensor(
                        out=he[:, f, h * 512:(h + 1) * 512], in0=psU[gi][:], in1=sg[gi][:], op=ALU.mult),
                        reads=[b_psU[gi], b_sg[gi]], writes=[b_he])
            for s in range(NS):
                for c in range(4):
                    yi = ycount % 2
                    ycount += 1
                    for f in range(4):
                        P.emit("pe", lambda e, f=f, s=s, c=c, yi=yi, Wd=Wd: e.matmul(
                            out=psY[yi][:, :], lhsT=he[:, f, s * 128:(s + 1) * 128],
                            rhs=Wd[:, f, c * 512:(c + 1) * 512], start=(f == 0), stop=(f == 3)),
                            reads=[b_he, b_ring[sd_]], writes=[b_psY[yi]])
                    P.emit("dve", lambda e, s=s, c=c, yi=yi, ex=ex: e.scalar_tensor_tensor(
                        out=yacc[:, s, c * 512:(c + 1) * 512], in0=psY[yi][:], scalar=gates[:, s, ex:ex + 1],
                        in1=yacc[:, s, c * 512:(c + 1) * 512], op0=ALU.mult, op1=ALU.add),
                        reads=[b_psY[yi], b_gates[s], b_yacc[s]], writes=[b_yacc[s]])
        for s in range(NS):
            r0 = t * TT + s * 128
            if g_final is not None:
                rmsnorm_rows(P, yacc[:, s, :], yacc[:, s, :], gfin[:], b_yacc[s], b_yacc[s], b_gf,
                             (hn[:], ssq[:, s:s + 1], rstd[:, s:s + 1], b_hn, b_ssq[s], b_rstd[s]), "f")
            P.out_ops.append(P.dma("sp", xout[r0:r0 + 128, :], yacc[:, s, :], k_x[s],
                                   reads=[b_yacc[s]], writes=[P.dram_buf(xout, r0)]))
    st.close()


TS = 2048
NT = 16
GELU_K = 1.5957691216057308


class Pool_:
    def __init__(self, P, tag):
        self.P, self.nc, self.tag = P, P.nc, tag
        self.st = ExitStack()

    def sb(self, shape, dt, name):
        t = self.st.enter_context(self.nc.sbuf_tensor(f"{name}_{self.tag}", list(shape), dt))
        return t, Buf(f"{name}_{self.tag}")

    def ps(self, shape, dt, name):
        t = self.st.enter_context(self.nc.psum_tensor(f"{name}_{self.tag}", list(shape), dt))
        return t, Buf(f"{name}_{self.tag}")

    def close(self):
        self.st.close()


def make_ident(P, ident, b_id):
    P.emit("pool", lambda e: e.memset(ident[:], 1.0), writes=[b_id])
    P.emit("pool", lambda e: e.affine_select(out=ident[:], in_=ident[:], pattern=[[-1, 128]],
                                              compare_op=ALU.is_equal, fill=0.0, base=0,
                                              channel_multiplier=1), reads=[b_id], writes=[b_id])


def build_hT(P, tag, xin, base, hT, b_hT, gbc, b_g, ident, b_id):
    A = Pool_(P, f"h{tag}")
    xt = [A.sb([128, D], F32, f"xt{i}") for i in range(2)]
    hn, b_hn = A.sb([128, D], BF16, "hn")
    ssq, b_ssq = A.sb([128, 2], F32, "ssq")
    rstd, b_rstd = A.sb([128, 2], F32, "rstd")
    psT = [A.ps([128, 8, 128], BF16, f"psT{i}") for i in range(2)]
    kx = [P.key(f"hx_{i}") for i in range(2)]
    for tt in range(NT):
        i = tt % 2
        x_t, b_x = xt[i]
        r0 = base + tt * 128
        P.dma("sp", x_t[:], xin[r0:r0 + 128, :], kx[i], reads=[P.dram_buf(xin, r0)], writes=[b_x])
        rmsnorm_rows(P, x_t[:], hn[:], gbc[:], b_x, b_hn, b_g,
                     (hn[:], ssq[:, 0:1], rstd[:, 0:1], b_hn, b_ssq, b_rstd), "h")
        for half in range(2):
            pt, b_pt = psT[half]
            for j in range(8):
                kc = half * 8 + j
                P.emit("pe", lambda e, pt=pt, j=j, kc=kc: e.transpose(
                    out=pt[:, j, :], in_=hn[:, kc * 128:(kc + 1) * 128], identity=ident[:]),
                    reads=[b_hn, b_id], writes=[b_pt])
            if half == 0:
                P.emit("act", lambda e, pt=pt, tt=tt: e.copy(out=hT[:, 0:8, tt * 128:(tt + 1) * 128], in_=pt[:, :, :]),
                       reads=[b_pt], writes=[b_hT])
            else:
                P.emit("dve", lambda e, pt=pt, tt=tt: e.tensor_copy(out=hT[:, 8:16, tt * 128:(tt + 1) * 128], in_=pt[:, :, :]),
                       reads=[b_pt], writes=[b_hT])
    A.close()


def load_cols(P, dst, src1d, n, key, b):
    P.dma("sp", dst, src1d.rearrange("(n f) -> f n", f=128), key, writes=[b], allow_slow_non_contiguous=True)


def conv4(P, src, b_src, cw, cb, out, b_out, b_c):
    P.emit("dve", lambda e: e.tensor_scalar(out=out, in0=src[:, 3:3 + TS], scalar1=cw[:, 3:4], scalar2=cb,
                                            op0=ALU.mult, op1=ALU.add), reads=[b_src] + list(b_c), writes=[b_out])
    for j in (2, 1, 0):
        P.emit("dve", lambda e, j=j: e.scalar_tensor_tensor(out=out, in0=src[:, j:j + TS], scalar=cw[:, j:j + 1],
                                                          in1=out, op0=ALU.mult, op1=ALU.add),
               reads=[b_src, b_out] + list(b_c), writes=[b_out])


def proj_fm(P, hT, b_hT, wblk, b_w, pss, evac):
    for ch in range(4):
        ps, b_ps = pss[ch % len(pss)]
        for kc in range(16):
            P.emit("pe", lambda e, kc=kc, ch=ch, ps=ps: e.matmul(
                out=ps[:, :], lhsT=wblk[:, kc, :], rhs=hT[:, kc, ch * 512:(ch + 1) * 512],
                start=(kc == 0), stop=(kc == 15)), reads=[b_hT, b_w], writes=[b_ps])
        evac(ch, ps, b_ps)


def outproj_phase(P, tag, xin, xout, base, yscr, Wlist, combine):
    A = Pool_(P, f"o{tag}")
    nW = len(Wlist)
    Wsb = [A.sb([128, 16, D], BF16, f"W{i}") for i in range(nW)]
    kW = P.key(f"oW", group=True)
    b_wc = [[Buf(f"W{tag}_{w}_{c}") for c in range(4)] for w in range(nW)]
    for w, ((w_t, b_w), wd) in enumerate(zip(Wsb, Wlist)):
        for c in range(4):
            P.dma("pool", w_t[:, :, c * 512:(c + 1) * 512],
                  wd[:, c * 512:(c + 1) * 512].rearrange("(kc p) n -> p kc n", p=128), kW, writes=[b_wc[w][c]])
    ys = [A.sb([128, 16, 128], BF16, f"ys{i}") for i in range(2)]
    xt = [A.sb([128, D], F32, f"xo{i}") for i in range(2)]
    tmp = [A.sb([128, 512], F32, f"tmp{i}") for i in range(2)]
    pss = [[A.ps([128, 512], F32, f"po{w}_{i}") for i in range(2)] for w in range(nW)]
    ky = [P.key(f"oy_{i}") for i in range(2)]
    kx = [P.key(f"ox_{i}") for i in range(2)]
    cnt = 0
    for tt in range(NT):
        i = tt % 2
        y_t, b_y = ys[i]
        x_t, b_x = xt[i]
        r0 = base + tt * 128
        P.dma("sp", y_t[:], yscr.rearrange("(kc p) t -> p kc t", p=128)[:, :, tt * 128:(tt + 1) * 128], ky[i],
              reads=[P.dram_buf(yscr, 0)], writes=[b_y])
        P.dma("sp", x_t[:], xin[r0:r0 + 128, :], kx[i], reads=[P.dram_buf(xin, r0)], writes=[b_x])
        for c in range(4):
            cur = []
            for w in range(nW):
                ps, b_ps = pss[w][cnt % 2]
                w_t, b_w = Wsb[w]
                for kc in range(16):
                    P.emit("pe", lambda e, kc=kc, c=c, ps=ps, w_t=w_t, y_t=y_t: e.matmul(
                        out=ps[:, :], lhsT=y_t[:, kc, :], rhs=w_t[:, kc, c * 512:(c + 1) * 512],
                        start=(kc == 0), stop=(kc == 15)), reads=[b_y, b_wc[w][c]], writes=[b_ps])
                cur.append((ps, b_ps))
            combine(P, cur, x_t, b_x, c, tmp[cnt % 2])
            cnt += 1
        P.out_ops.append(P.dma("sp", xout[r0:r0 + 128, :], x_t[:], kx[i], reads=[b_x],
                               writes=[P.dram_buf(xout, r0)]))
    A.close()


LN16 = 2.772588722239781
STOP = [0]


def mixer_a_stage(P, xin, xout, NSEQ, W, yscr_all, GELU_NATIVE=False):
    nc = P.nc
    u = P.uid()
    w_in = W["w_in"]
    def seq_body(sq):
        tag = f"a{u}s{sq}"
        base = sq * TS
        yscr = yscr_all[sq]
        O = Pool_(P, f"O{tag}")
        hT, b_hT = O.sb([128, 16, TS], BF16, "hT")
        ident, b_id = O.sb([128, 128], BF16, "ident")
        Gp = Pool_(P, f"G{tag}")
        gbc, b_g = Gp.sb([128, D], F32, "gbc")
        kc_ = P.key(f"c", group=True)
        kcp = P.key(f"cp", group=True)
        P.dma("sp", gbc[:], W["norm"].partition_broadcast(128), P.key(f"g"), writes=[b_g])
        make_ident(P, ident, b_id)
        build_hT(P, tag, xin, base, hT, b_hT, gbc, b_g, ident, b_id)
        barrier(P)
        Gp.close()
        if STOP[0] == 1:
            O.close()
            return
        wq = [O.sb([128, 16, 128], BF16, f"wq{i}") for i in range(3)]
        kwq = [P.key(f"wq_{i}") for i in range(3)]
        wqc = {"n": 0}

        def load_wblk(c0, width=128):
            i = wqc["n"] % 3
            wqc["n"] += 1
            t, b = wq[i]
            P.dma("pool", t[:, :, 0:width], w_in[:, c0:c0 + width].rearrange("(kc p) n -> p kc n", p=128),
                  kwq[i], writes=[b])
            return t, b

        def lru_phase():
            L = Pool_(P, f"L{tag}")
            sets = []
            for i_ in range(2):
                sets.append(L.sb([128, TS + 4], F32, f"xpad{i_}") + L.sb([128, TS], F32, f"xc{i_}") + L.sb([128, TS], BF16, f"xcb{i_}")
                            + L.sb([128, TS], F32, f"t1_{i_}") + L.sb([128, TS], F32, f"t2_{i_}"))
            t3, b_t3 = L.sb([128, TS], F32, "t3")
            hh, b_hh = L.sb([128, TS], F32, "hh")
            gz, b_gz = L.sb([128, TS], F32, "gz")
            yb, b_yb = L.sb([128, TS], BF16, "yb")
            cw, b_cw = L.sb([128, 8, 4], F32, "cw")
            cb, b_cb = L.sb([128, 8], F32, "cb")
            ba, b_ba = L.sb([128, 8], F32, "ba")
            bx, b_bx = L.sb([128, 8], F32, "bx")
            lam, b_lam = L.sb([128, 8], F32, "lam")
            cneg, b_cneg = L.sb([128, 8], F32, "cneg")
            cneg2, b_cneg2 = L.sb([128, 8], F32, "cneg2")
            waT, b_wa = L.sb([128, 8, 128], BF16, "waT")
            wxT, b_wx = L.sb([128, 8, 128], BF16, "wxT")
            pss = [L.ps([128, 512], F32, f"pl{i}") for i in range(4)]
            kyb = P.key(f"yb")
            b_cwj = [Buf(f"cwj{j}") for j in range(4)]
            for j in range(4):
                P.dma("sp", cw[:, :, j], W["lru_conv_w"][j, :].rearrange("(n f) -> f n", f=128), kc_,
                      writes=[b_cwj[j]], allow_slow_non_contiguous=True)
            load_cols(P, cb[:], W["lru_conv_b"], 8, kc_, b_cb)
            load_cols(P, ba[:], W["lru_b_a"], 8, kc_, b_ba)
            load_cols(P, bx[:], W["lru_b_x"], 8, kc_, b_bx)
            load_cols(P, lam[:], W["lru_lam"], 8, kc_, b_lam)
            P.dma("pool", waT[:], W["lru_w_a"].rearrange("n i j -> i n j"), kcp, writes=[b_wa])
            P.dma("pool", wxT[:], W["lru_w_x"].rearrange("n i j -> i n j"), kcp, writes=[b_wx])
            P.emit("act", lambda e: e.activation(out=cneg[:], in_=lam[:], func=AF.Exp, scale=-1.0), reads=[b_lam], writes=[b_cneg])
            P.emit("act", lambda e: e.activation(out=cneg[:], in_=cneg[:], func=AF.Ln, bias=1.0, scale=1.0), reads=[b_cneg], writes=[b_cneg])
            P.emit("dve", lambda e: e.tensor_scalar(out=cneg2[:], in0=cneg[:], scalar1=-16.0, scalar2=None, op0=ALU.mult), reads=[b_cneg], writes=[b_cneg2])
            P.emit("dve", lambda e: e.tensor_scalar(out=cneg[:], in0=cneg[:], scalar1=-8.0, scalar2=None, op0=ALU.mult), reads=[b_cneg, b_cneg2], writes=[b_cneg])
            for st_ in sets:
                P.emit("pool", lambda e, xp=st_[0]: e.memset(xp[:, 0:3], 0.0), writes=[st_[1]])

            def lru_front(n, xpad, b_xpad, xc, b_xc, xcb, b_xcb, t1, b_t1, t2, b_t2):
                wx_t, b_wxb = load_wblk(n * 128)

                def ev_x(ch, ps, b_ps):
                    P.emit("act", lambda e, ch=ch, ps=ps: e.copy(out=xpad[:, 3 + ch * 512:3 + (ch + 1) * 512], in_=ps[:, :]),
                           reads=[b_ps], writes=[b_xpad])
                proj_fm(P, hT, b_hT, wx_t, b_wxb, pss[0:2], ev_x)
                conv4(P, xpad, b_xpad, cw[:, n, :], cb[:, n:n + 1], xc[:], b_xc, b_cwj + [b_cb])
                P.emit("pool", lambda e: e.tensor_copy(out=xcb[:], in_=xc[:]), reads=[b_xc], writes=[b_xcb])
                for ch in range(4):
                    pr, b_pr = pss[ch % 2]
                    pi, b_pi = pss[2 + ch % 2]
                    P.emit("pe", lambda e, n=n, ch=ch, pr=pr: e.matmul(out=pr[:, :], lhsT=waT[:, n, :], rhs=xcb[:, ch * 512:(ch + 1) * 512],
                                                                        start=True, stop=True), reads=[b_wa, b_xcb], writes=[b_pr])
                    P.emit("pe", lambda e, n=n, ch=ch, pi=pi: e.matmul(out=pi[:, :], lhsT=wxT[:, n, :], rhs=xcb[:, ch * 512:(ch + 1) * 512],
                                                                        start=True, stop=True), reads=[b_wx, b_xcb], writes=[b_pi])
                    P.emit("act", lambda e, n=n, ch=ch, pr=pr: e.activation(out=t1[:, ch * 512:(ch + 1) * 512], in_=pr[:, :], func=AF.Sigmoid,
                                                                             bias=ba[:, n:n + 1], scale=1.0), reads=[b_pr, b_ba], writes=[b_t1])
                    P.emit("act", lambda e, n=n, ch=ch, pi=pi: e.activation(out=t2[:, ch * 512:(ch + 1) * 512], in_=pi[:, :], func=AF.Sigmoid,
                                                                             bias=bx[:, n:n + 1], scale=1.0), reads=[b_pi, b_bx], writes=[b_t2])

            def lru_back(n, xpad, b_xpad, xc, b_xc, xcb, b_xcb, t1, b_t1, t2, b_t2):
                wz_t, b_wzb = load_wblk(1024 + n * 128)
                P.emit("act", lambda e, n=n: e.activation(out=t3[:], in_=t1[:], func=AF.Exp, scale=cneg2[:, n:n + 1]), reads=[b_t1, b_cneg2], writes=[b_t3])
                P.emit("act", lambda e, n=n: e.activation(out=t1[:], in_=t1[:], func=AF.Exp, scale=cneg[:, n:n + 1]), reads=[b_t1, b_cneg, b_t3], writes=[b_t1])
                P.emit("act", lambda e: e.activation(out=t3[:], in_=t3[:], func=AF.Sqrt, bias=1.0, scale=-1.0), reads=[b_t3], writes=[b_t3])
                P.emit("dve", lambda e: e.tensor_tensor(out=t2[:], in0=t2[:], in1=xc[:], op=ALU.mult), reads=[b_t2, b_xc], writes=[b_t2])
                P.emit("dve", lambda e: e.tensor_tensor(out=t2[:], in0=t2[:], in1=t3[:], op=ALU.mult), reads=[b_t2, b_t3], writes=[b_t2])
                P.emit("dve", lambda e: e.tensor_tensor_scan(out=hh[:], data0=t1[:], data1=t2[:], initial=0.0, op0=ALU.mult, op1=ALU.add),
                       reads=[b_t1, b_t2], writes=[b_hh])

                def ev_z(ch, ps, b_ps):
                    sl = slice(ch * 512, (ch + 1) * 512)
                    if GELU_NATIVE:
                        P.emit("act", lambda e, ps=ps, sl=sl: e.activation(out=gz[:, sl], in_=ps[:, :], func=AF.Gelu_apprx_tanh),
                               reads=[b_ps], writes=[b_gz])
                    else:
                        P.emit("act", lambda e, ps=ps, sl=sl: e.activation(out=gz[:, sl], in_=ps[:, :], func=AF.Square), reads=[b_ps], writes=[b_gz])
                        P.emit("dve", lambda e, sl=sl: e.tensor_scalar(out=gz[:, sl], in0=gz[:, sl], scalar1=0.044715, scalar2=1.0, op0=ALU.mult, op1=ALU.add),
                               reads=[b_gz], writes=[b_gz])
                        P.emit("dve", lambda e, ps=ps, sl=sl: e.tensor_tensor(out=gz[:, sl], in0=ps[:, :], in1=gz[:, sl], op=ALU.mult),
                               reads=[b_gz, b_ps], writes=[b_gz])
                        P.emit("act", lambda e, sl=sl: e.activation(out=gz[:, sl], in_=gz[:, sl], func=AF.Sigmoid, scale=GELU_K), reads=[b_gz], writes=[b_gz])
                        P.emit("dve", lambda e, ps=ps, sl=sl: e.tensor_tensor(out=gz[:, sl], in0=ps[:, :], in1=gz[:, sl], op=ALU.mult),
                               reads=[b_gz, b_ps], writes=[b_gz])
                    P.emit("dve", lambda e, sl=sl: e.tensor_tensor(out=yb[:, sl], in0=hh[:, sl], in1=gz[:, sl], op=ALU.mult),
                           reads=[b_gz, b_hh], writes=[b_yb])
                proj_fm(P, hT, b_hT, wz_t, b_wzb, pss[2:4], ev_z)
                P.dma("sp", yscr[n * 128:(n + 1) * 128, :], yb[:], kyb, reads=[b_yb], writes=[P.dram_buf(yscr, 0)])
            lru_front(0, *sets[0])
            for n in range(8):
                if n + 1 < 8:
                    lru_front(n + 1, *sets[(n + 1) % 2])
                lru_back(n, *sets[n % 2])
            barrier(P)
            L.close()

        lru_phase()
        if STOP[0] == 2:
            O.close()
            return
        def mlstm_phase():
            M = Pool_(P, f"M{tag}")
            xpad, b_xpad = M.sb([128, TS + 4], F32, "xpad")
            xc, b_xc = M.sb([128, TS], F32, "xc")
            qT, b_qT = M.sb([128, 2, TS], BF16, "qT")
            kT, b_kT = M.sb([128, 2, TS], BF16, "kT")
            vext, b_v = M.sb([128, NT, 258], BF16, "vext")
            og, b_og = M.sb([128, NT, 256], BF16, "og")
            Fb, b_Fb = M.sb([128, TS], F32, "Fb")
            iT, b_iT = M.sb([4, TS], F32, "iT")
            fT, b_fT = M.sb([4, TS], F32, "fT")
            ones4, b_ones4 = M.sb([4, 1], F32, "ones4")
            biasT, b_bT = M.sb([128, NT, 4], F32, "biasT")
            wgi, b_wgi = M.sb([128, 16, 4], BF16, "wgi")
            wgf, b_wgf = M.sb([128, 16, 4], BF16, "wgf")
            ibias, b_ib = M.sb([4, 1], F32, "ibias")
            fbias, b_fb = M.sb([4, 1], F32, "fbias")
            selm, b_sel = M.sb([4, 4, 128], F32, "selm")
            id4, b_id4 = M.sb([4, 4], F32, "id4")
            tri, b_tri = M.sb([128, 128], F32, "tri")
            mcw, b_mcw = M.sb([128, 16, 4], F32, "mcw")
            mcb, b_mcb = M.sb([128, 16], F32, "mcb")
            mg, b_mg = M.sb([128, 1024], F32, "mg")
            dT = [M.sb([128, 512], F32, f"dT{i}") for i in range(2)]
            pT = [M.sb([128, 512], BF16, f"pT{i}") for i in range(2)]
            accS, _ = M.sb([128, 4, 258], F32, "accS")
            b_accS = [Buf(f"accS{q}") for q in range(4)]
            hm, b_hm = M.sb([128, 256], F32, "hm")
            ybh, b_ybh = M.sb([128, 256], BF16, "ybh")
            yTh, b_yTh = M.sb([128, 2, TS], BF16, "yTh")
            sml, b_sml = M.sb([128, 8], F32, "sml")
            epsc, b_epsc = M.sb([128, 1], F32, "epsc")
            P.emit("pool", lambda e: e.memset(epsc[:], EPS), writes=[b_epsc])
            wv = [M.sb([128, 16, 256], BF16, f"wv{i}") for i in range(2)]
            kwv = [P.key(f"wv_{i}") for i in range(2)]
            kyT = P.key(f"yT")
            kc2 = P.key(f"c2", group=True)
            kcp2 = P.key(f"cp2", group=True)
            psS = [M.ps([128, 512], F32, f"pS{i}") for i in range(2)]
            psA = [M.ps([128, 512], F32, f"pA{i}") for i in range(4)]
            psP = [M.ps([128, 512], F32, f"pP{i}") for i in range(2)]
            psB, b_psB = psP[0]
            psTt = psP[1][0][:, :].bitcast(BF16).rearrange("p (a b) -> p a b", a=8)
            b_psTt = psP[1][1]

            b_mcwj = [Buf(f"mcwj{j}") for j in range(4)]
            for j in range(4):
                P.dma("sp", mcw[:, :, j], W["m_conv_w"][j, :].rearrange("(n f) -> f n", f=128), kc2,
                      writes=[b_mcwj[j]], allow_slow_non_contiguous=True)
            load_cols(P, mcb[:], W["m_conv_b"], 16, kc2, b_mcb)
            P.dma("sp", mg[:], W["m_head_g"].partition_broadcast(128), kc2, writes=[b_mg])
            P.dma("sp", ibias[:], W["m_i_bias"].rearrange("o h -> h o"), kc2, writes=[b_ib], allow_slow_non_contiguous=True)
            P.dma("sp", fbias[:], W["m_f_bias"].rearrange("o h -> h o"), kc2, writes=[b_fb], allow_slow_non_contiguous=True)
            P.dma("pool", wgi[:], w_in[:, 6144:6148].rearrange("(kc p) n -> p kc n", p=128), kcp2, writes=[b_wgi])
            P.dma("pool", wgf[:], w_in[:, 6148:6152].rearrange("(kc p) n -> p kc n", p=128), kcp2, writes=[b_wgf])
            P.emit("pool", lambda e: e.memset(xpad[:, 0:3], 0.0), writes=[b_xpad])
            P.emit("pool", lambda e: e.memset(ones4[:], 1.0), writes=[b_ones4])
            P.emit("pool", lambda e: e.memset(vext[:, :, 256:258], 1.0), writes=[b_v])
            P.emit("pool", lambda e: e.memset(id4[:], 1.0), writes=[b_id4])
            P.emit("pool", lambda e: e.affine_select(out=id4[:], in_=id4[:], pattern=[[-1, 4]], compare_op=ALU.is_equal, fill=0.0,
                                                      base=0, channel_multiplier=1), reads=[b_id4], writes=[b_id4])
            P.emit("pool", lambda e: e.memset(selm[:], -1.0), writes=[b_sel])
            P.emit("pool", lambda e: e.affine_select(out=selm[:], in_=selm[:], pattern=[[-1, 4], [0, 128]], compare_op=ALU.is_equal, fill=0.0,
                                                      base=0, channel_multiplier=1), reads=[b_sel], writes=[b_sel])
            P.emit("pool", lambda e: e.memset(tri[:], 1.0), writes=[b_tri])
            P.emit("pool", lambda e: e.affine_select(out=tri[:], in_=tri[:], pattern=[[1, 128]], compare_op=ALU.is_ge, fill=0.0,
                                                      base=0, channel_multiplier=-1), reads=[b_tri], writes=[b_tri])
            for ch in range(4):
                sl = slice(ch * 512, (ch + 1) * 512)
                pi_, b_pi = psP[0]
                pf_, b_pf = psP[1]
                for kc in range(16):
                    P.emit("pe", lambda e, kc=kc, sl=sl, pi_=pi_: e.matmul(out=pi_[0:4, :], lhsT=wgi[:, kc, :], rhs=hT[:, kc, sl],
                                                                            start=(kc == 0), stop=(kc == 15)), reads=[b_hT, b_wgi], writes=[b_pi])
                for kc in range(16):
                    P.emit("pe", lambda e, kc=kc, sl=sl, pf_=pf_: e.matmul(out=pf_[0:4, :], lhsT=wgf[:, kc, :], rhs=hT[:, kc, sl],
                                                                            start=(kc == 0), stop=(kc == 15)), reads=[b_hT, b_wgf], writes=[b_pf])
                P.emit("act", lambda e, sl=sl, pi_=pi_: e.activation(out=iT[:, sl], in_=pi_[0:4, :], func=AF.Identity, bias=ibias[:, 0:1], scale=1.0),
                       reads=[b_pi, b_ib], writes=[b_iT])
                P.emit("act", lambda e, sl=sl, pf_=pf_: e.activation(out=fT[:, sl], in_=pf_[0:4, :], func=AF.Identity, bias=fbias[:, 0:1], scale=1.0),
                       reads=[b_pf, b_fb], writes=[b_fT])
            P.emit("act", lambda e: e.activation(out=fT[:], in_=fT[:], func=AF.Exp, scale=-1.0), reads=[b_fT], writes=[b_fT])
            P.emit("act", lambda e: e.activation(out=fT[:], in_=fT[:], func=AF.Ln, bias=1.0, scale=1.0), reads=[b_fT], writes=[b_fT])
            P.emit("dve", lambda e: e.tensor_tensor_scan(out=fT[:], data0=ones4[:, 0:1].to_broadcast([4, TS]), data1=fT[:], initial=0.0, op0=ALU.mult, op1=ALU.add),
                   reads=[b_fT, b_ones4], writes=[b_fT])
            P.emit("dve", lambda e: e.tensor_tensor(out=iT[:], in0=iT[:], in1=fT[:], op=ALU.add), reads=[b_iT, b_fT], writes=[b_iT])
            for jt in range(NT):
                P.emit("pe", lambda e, jt=jt: e.matmul(out=psB[:, jt * 4:(jt + 1) * 4], lhsT=iT[:, jt * 128:(jt + 1) * 128], rhs=id4[:],
                                                        start=True, stop=True), reads=[b_iT, b_id4], writes=[b_psB])
            P.emit("dve", lambda e: e.tensor_scalar(out=biasT[:].rearrange("p a b -> p (a b)"), in0=psB[:, 0:64], scalar1=-LN16, scalar2=None, op0=ALU.add),
                   reads=[b_psB], writes=[b_bT])
            wvc = {"n": 0}

            def load_wv(c0):
                i = wvc["n"] % 2
                wvc["n"] += 1
                t, b = wv[i]
                P.dma("pool", t[:], w_in[:, c0:c0 + 256].rearrange("(kc p) n -> p kc n", p=128), kwv[i], writes=[b])
                return t, b

            blk = 0
            for hd in range(4):
                for (dstT, b_dst, c0, t0) in ((qT, b_qT, 2048, 0), (kT, b_kT, 3072, 8)):
                    for c in range(2):
                        wt, b_wt = load_wblk(c0 + hd * 256 + c * 128)

                        def ev_q(ch, ps, b_ps):
                            P.emit("act", lambda e, ch=ch, ps=ps: e.copy(out=xpad[:, 3 + ch * 512:3 + (ch + 1) * 512], in_=ps[:, :]),
                                   reads=[b_ps], writes=[b_xpad])
                        proj_fm(P, hT, b_hT, wt, b_wt, psP, ev_q)
                        tcol = t0 + hd * 2 + c
                        conv4(P, xpad, b_xpad, mcw[:, tcol, :], mcb[:, tcol:tcol + 1], xc[:], b_xc, b_mcwj + [b_mcb])
                        P.emit("act", lambda e, dstT=dstT, c=c: e.activation(out=dstT[:, c, :], in_=xc[:], func=AF.Silu),
                               reads=[b_xc], writes=[b_dst])
                wv_t, b_wvt = load_wv(4096 + hd * 256)
                wo_t, b_wot = load_wv(5120 + hd * 256)
                for tt in range(NT):
                    for (w_t, b_w, isv) in ((wv_t, b_wvt, True), (wo_t, b_wot, False)):
                        ps, b_ps = psP[blk % 2]
                        blk += 1
                        for kc in range(16):
                            P.emit("pe", lambda e, kc=kc, tt=tt, ps=ps, w_t=w_t: e.matmul(
                                out=ps[:, 0:256], lhsT=hT[:, kc, tt * 128:(tt + 1) * 128], rhs=w_t[:, kc, :],
                                start=(kc == 0), stop=(kc == 15)), reads=[b_hT, b_w], writes=[b_ps])
                        if isv:
                            P.emit("dve", lambda e, tt=tt, ps=ps: e.tensor_copy(out=vext[:, tt, 0:256], in_=ps[:, 0:256]),
                                   reads=[b_ps], writes=[b_v])
                        else:
                            P.emit("act", lambda e, tt=tt, ps=ps: e.activation(out=og[:, tt, :], in_=ps[:, 0:256], func=AF.Sigmoid),
                                   reads=[b_ps], writes=[b_og])
                for ch in range(4):
                    ps, b_ps = psP[blk % 2]
                    blk += 1
                    P.emit("pe", lambda e, ch=ch, ps=ps, hd=hd: e.matmul(out=ps[:, :], lhsT=selm[:, hd, :], rhs=fT[:, ch * 512:(ch + 1) * 512],
                                                                          start=True, stop=True), reads=[b_sel, b_fT], writes=[b_ps])
                    P.emit("act", lambda e, ch=ch, ps=ps: e.copy(out=Fb[:, ch * 512:(ch + 1) * 512], in_=ps[:, :]), reads=[b_ps], writes=[b_Fb])
                def epilogue(tt, acc, b_acc, tsl):
                    dd, rec, ssq, rstd = sml[:, 0:1], sml[:, 1:2], sml[:, 2:3], sml[:, 3:4]
                    P.emit("dve", lambda e, acc=acc, dd=dd: e.tensor_scalar(out=dd, in0=acc[:, 256:257], scalar1=-1.0, scalar2=None, op0=ALU.mult),
                           reads=[b_acc], writes=[b_sml])
                    P.emit("dve", lambda e, acc=acc, dd=dd: e.tensor_tensor(out=dd, in0=dd, in1=acc[:, 256:257], op=ALU.max),
                           reads=[b_acc, b_sml], writes=[b_sml])
                    P.emit("dve", lambda e, dd=dd: e.tensor_scalar(out=dd, in0=dd, scalar1=1.0, scalar2=None, op0=ALU.max),
                           reads=[b_sml], writes=[b_sml])
                    P.emit("dve", lambda e, dd=dd, rec=rec: e.reciprocal(out=rec, in_=dd), reads=[b_sml], writes=[b_sml])
                    P.emit("dve", lambda e, acc=acc, rec=rec, tt=tt: e.scalar_tensor_tensor(out=hm[:], in0=acc[:, 0:256], scalar=rec, in1=og[:, tt, :],
                                                                                             op0=ALU.mult, op1=ALU.mult),
                           reads=[b_acc, b_sml, b_og], writes=[b_hm])
                    P.emit("dve", lambda e, ssq=ssq: e.scalar_tensor_tensor(out=ybh[:], in0=hm[:], scalar=1.0, in1=hm[:], op0=ALU.mult, op1=ALU.mult, accum_out=ssq),
                           reads=[b_hm], writes=[b_ybh, b_sml])
                    P.emit("act", lambda e, ssq=ssq, rstd=rstd: e.activation(out=rstd, in_=ssq, func=AF.Ln, bias=epsc[:, 0:1], scale=1.0 / 256), reads=[b_sml, b_epsc], writes=[b_sml])
                    P.emit("act", lambda e, rstd=rstd: e.activation(out=rstd, in_=rstd, func=AF.Exp, scale=-0.5), reads=[b_sml], writes=[b_sml])
                    P.emit("dve", lambda e, rstd=rstd, hd=hd: e.scalar_tensor_tensor(out=ybh[:], in0=hm[:], scalar=rstd, in1=mg[:, hd * 256:(hd + 1) * 256],
                                                                                      op0=ALU.mult, op1=ALU.mult),
                           reads=[b_hm, b_sml, b_mg], writes=[b_ybh])
                    for c in range(2):
                        P.emit("pe", lambda e, c=c: e.transpose(out=psTt[:, c, :], in_=ybh[:, c * 128:(c + 1) * 128], identity=ident[:]),
                               reads=[b_ybh, b_id], writes=[b_psTt])
                    P.emit("act", lambda e, tsl=tsl: e.copy(out=yTh[:, :, tsl], in_=psTt[:, 0:2, :]), reads=[b_psTt], writes=[b_yTh])
                cntS = 0
                pend = []
                for qg in range(4):
                    for jt in range(4 * qg + 4):
                        c0 = max(0, jt - 4 * qg)
                        ncol = (4 - c0) * 128
                        t0 = (4 * qg + c0) * 128
                        tsl = slice(t0, t0 + ncol)
                        jsl = slice(jt * 128, (jt + 1) * 128)
                        i2 = cntS % 2
                        cntS += 1
                        pS, b_pS = psS[i2]
                        d_t, b_d = dT[i2]
                        p_t, b_p = pT[i2]
                        for c in range(2):
                            P.emit("pe", lambda e, c=c, pS=pS, jsl=jsl, tsl=tsl, ncol=ncol: e.matmul(out=pS[:, 0:ncol], lhsT=kT[:, c, jsl], rhs=qT[:, c, tsl],
                                                                                                     start=(c == 0), stop=(c == 1)),
                                   reads=[b_kT, b_qT], writes=[b_pS])
                        P.emit("act", lambda e, d_t=d_t, tsl=tsl, jt=jt, hd=hd, ncol=ncol: e.activation(out=d_t[:, 0:ncol], in_=Fb[:, tsl], func=AF.Exp,
                                                                                                        bias=biasT[:, jt, hd:hd + 1], scale=1.0),
                               reads=[b_Fb, b_bT], writes=[b_d])
                        if jt >= 4 * qg:
                            P.emit("pool", lambda e, d_t=d_t: e.tensor_tensor(out=d_t[:, 0:128], in0=d_t[:, 0:128], in1=tri[:], op=ALU.mult),
                                   reads=[b_d, b_tri], writes=[b_d])
                        P.emit("dve", lambda e, d_t=d_t, p_t=p_t, pS=pS, ncol=ncol: e.tensor_tensor(out=p_t[:, 0:ncol], in0=pS[:, 0:ncol], in1=d_t[:, 0:ncol], op=ALU.mult),
                               reads=[b_pS, b_d], writes=[b_p])
                        for tq in range(c0, 4):
                            acc, b_acc = psA[tq]
                            tt = 4 * qg + tq
                            P.emit("pe", lambda e, p_t=p_t, jt=jt, acc=acc, tt=tt, tq=tq, c0=c0: e.matmul(
                                out=acc[:, 0:258], lhsT=p_t[:, (tq - c0) * 128:(tq - c0 + 1) * 128], rhs=vext[:, jt, :],
                                start=(jt == 0), stop=(jt == tt)), reads=[b_p, b_v], writes=[b_acc])
                        if pend:
                            epilogue(*pend.pop(0))
                    for tq in range(4):
                        acc, b_acc = psA[tq]
                        P.emit("act", lambda e, acc=acc, tq=tq: e.copy(out=accS[:, tq, :], in_=acc[:, 0:258]), reads=[b_acc], writes=[b_accS[tq]])
                    for tq in range(4):
                        tt = 4 * qg + tq
                        pend.append((tt, accS[:, tq, :], b_accS[tq], slice(tt * 128, (tt + 1) * 128)))
                    if qg == 3:
                        while pend:
                            epilogue(*pend.pop(0))
                for c in range(2):
                    r0 = 1024 + hd * 256 + c * 128
                    P.dma("sp", yscr[r0:r0 + 128, :], yTh[:, c, :], kyT, reads=[b_yTh], writes=[P.dram_buf(yscr, 0)])
            barrier(P)
            M.close()

        if STOP[0] != 3:
            mlstm_phase()
        O.close()
        if STOP[0] == 4:
            return
        def comb(P, cur, x_t, b_x, c, tmp):
            ps, b_ps = cur[0]
            P.emit("dve", lambda e, ps=ps, c=c, x_t=x_t: e.tensor_tensor(out=x_t[:, c * 512:(c + 1) * 512], in0=ps[:, :],
                                                                          in1=x_t[:, c * 512:(c + 1) * 512], op=ALU.add),
                   reads=[b_ps, b_x], writes=[b_x])
        outproj_phase(P, tag, xin, xout, base, yscr, [W["w_out"]], comb)
        barrier(P)

    for sq in range(NSEQ):
        seq_body(sq)


PI = 3.141592653589793
MAGIC = 12582912.0


def s5_stage(P, xin, xout, NSEQ, W, yscr_all):
    nc = P.nc
    u = P.uid()
    w_in = W["w_in"]

    def seq_body(sq):
        tag = f"s{u}q{sq}"
        base = sq * TS
        yscr = yscr_all[sq]
        O = Pool_(P, f"O{tag}")
        hT, b_hT = O.sb([128, 16, TS], BF16, "hT")
        ident, b_id = O.sb([128, 128], BF16, "ident")
        Gp = Pool_(P, f"G{tag}")
        gbc, b_g = Gp.sb([128, D], F32, "gbc")
        P.dma("sp", gbc[:], W["norm"].partition_broadcast(128), P.key(f"g"), writes=[b_g])
        make_ident(P, ident, b_id)
        build_hT(P, tag, xin, base, hT, b_hT, gbc, b_g, ident, b_id)
        barrier(P)
        Gp.close()

        def main_phase():
            M = Pool_(P, f"M{tag}")
            wq = [M.sb([128, 16, 128], BF16, f"wq{i}") for i in range(3)]
            kwq = [P.key(f"wq_{i}") for i in range(3)]
            rhoB, b_rhoB = M.sb([128, 64], F32, "rhoB")
            thB, b_thB = M.sb([128, 64], F32, "thB")
            bbr, b_bbr = M.sb([128, 64, 16], F32, "bbr")
            bbi, b_bbi = M.sb([128, 64, 16], F32, "bbi")
            dsk, b_dsk = M.sb([128, 16], F32, "dsk")
            iota, b_iota = M.sb([128, TS], F32, "iota")
            kc_ = P.key(f"c", group=True)
            psA = [M.ps([128, 512], F32, f"pa{i}") for i in range(4)]
            psY = [M.ps([128, 512], F32, f"py{i}") for i in range(4)]
            psTb = psA[3][0][:, :].bitcast(BF16).rearrange("p (a b) -> p a b", a=8)
            b_psTb = psA[3][1]

            def setup():
                S = Pool_(P, f"S{tag}")
                are, b_are = S.sb([128, 64], F32, "are")
                aim, b_aim = S.sb([128, 64], F32, "aim")
                ls, b_ls = S.sb([128, 1], F32, "ls")
                rho, b_rho = S.sb([128, 64], F32, "rho")
                th, b_th = S.sb([128, 64], F32, "th")
                tmp, b_tmp = S.sb([128, 64], F32, "tmp")
                sn, b_sn = S.sb([128, 64], F32, "sn")
                cs, b_cs = S.sb([128, 64], F32, "cs")
                den, b_den = S.sb([128, 64], F32, "den")
                fr, b_fr = S.sb([128, 64], F32, "fr")
                fi, b_fi = S.sb([128, 64], F32, "fi")
                frB, b_frB = S.sb([128, 64], F32, "frB")
                fiB, b_fiB = S.sb([128, 64], F32, "fiB")
                E0, b_E0 = S.sb([128, 64], F32, "E0")
                E1, b_E1 = S.sb([128, 64], F32, "E1")
                bre, b_bre = S.sb([128, 64, 16], F32, "bre")
                bim, b_bim = S.sb([128, 64, 16], F32, "bim")
                t16, b_t16 = S.sb([128, 64, 16], F32, "t16")
                P.dma("sp", are[:], W["a_re"], kc_, writes=[b_are])
                P.dma("sp", aim[:], W["a_im"], kc_, writes=[b_aim])
                P.dma("sp", ls[:], W["log_step"].rearrange("(g o) -> g o", o=1), kc_, writes=[b_ls])
                b_bre2, b_bim2 = Buf("bre2"), Buf("bim2")
                for two in range(2):
                    P.dma("sp", bre[two * 64:(two + 1) * 64, :, :], W["b_re"].rearrange("(gp two) p c -> two p gp c", two=2)[two], kc_,
                          writes=[(b_bre, b_bre2)[two]])
                    P.dma("sp", bim[two * 64:(two + 1) * 64, :, :], W["b_im"].rearrange("(gp two) p c -> two p gp c", two=2)[two], kc_,
                          writes=[(b_bim, b_bim2)[two]])
                load_cols(P, dsk[:], W["d"], 16, kc_, b_dsk)
                P.emit("pool", lambda e: e.iota(out=iota[:], pattern=[[1, TS]], base=0, channel_multiplier=0,
                                                allow_small_or_imprecise_dtypes=True), writes=[b_iota])
                for (E, b_E, bs) in ((E0, b_E0, 0), (E1, b_E1, -1)):
                    P.emit("pool", lambda e, E=E: e.memset(E[:], 1.0), writes=[b_E])
                    P.emit("pool", lambda e, E=E, bs=bs: e.affine_select(out=E[:], in_=E[:], pattern=[[-2, 64]], compare_op=ALU.is_equal,
                                                                          fill=0.0, base=bs, channel_multiplier=1), reads=[b_E], writes=[b_E])
                dv = lambda fn, r, w: P.emit("dve", fn, reads=r, writes=w)
                ac = lambda fn, r, w: P.emit("act", fn, reads=r, writes=w)
                ac(lambda e: e.activation(out=ls[:], in_=ls[:], func=AF.Exp), [b_ls], [b_ls])
                dv(lambda e: e.tensor_scalar(out=rho[:], in0=are[:], scalar1=ls[:, 0:1], scalar2=None, op0=ALU.mult), [b_are, b_ls], [b_rho])
                ac(lambda e: e.activation(out=rho[:], in_=rho[:], func=AF.Exp), [b_rho], [b_rho])
                dv(lambda e: e.tensor_scalar(out=th[:], in0=aim[:], scalar1=ls[:, 0:1], scalar2=None, op0=ALU.mult), [b_aim, b_ls], [b_th])
                dv(lambda e: e.tensor_scalar(out=tmp[:], in0=th[:], scalar1=1.0 / (2 * PI), scalar2=MAGIC, op0=ALU.mult, op1=ALU.add), [b_th], [b_tmp])
                dv(lambda e: e.tensor_scalar(out=tmp[:], in0=tmp[:], scalar1=-MAGIC, scalar2=-2 * PI, op0=ALU.add, op1=ALU.mult), [b_tmp], [b_tmp])
                dv(lambda e: e.tensor_tensor(out=tmp[:], in0=tmp[:], in1=th[:], op=ALU.add), [b_tmp, b_th], [b_tmp])
                dv(lambda e: e.tensor_scalar(out=tmp[:], in0=tmp[:], scalar1=-PI, scalar2=PI, op0=ALU.max, op1=ALU.min), [b_tmp], [b_tmp])
                ac(lambda e: e.activation(out=sn[:], in_=tmp[:], func=AF.Sin), [b_tmp], [b_sn])
                ac(lambda e: e.activation(out=cs[:], in_=tmp[:], func=AF.Sin, scale=0.5), [b_tmp], [b_cs])
                dv(lambda e: e.tensor_tensor(out=cs[:], in0=cs[:], in1=cs[:], op=ALU.mult), [b_cs], [b_cs])
                dv(lambda e: e.tensor_scalar(out=cs[:], in0=cs[:], scalar1=-2.0, scalar2=1.0, op0=ALU.mult, op1=ALU.add), [b_cs], [b_cs])
                dv(lambda e: e.tensor_tensor(out=cs[:], in0=cs[:], in1=rho[:], op=ALU.mult), [b_cs, b_rho], [b_cs])
                dv(lambda e: e.tensor_scalar(out=cs[:], in0=cs[:], scalar1=-1.0, scalar2=None, op0=ALU.add), [b_cs], [b_cs])
                dv(lambda e: e.tensor_tensor(out=sn[:], in0=sn[:], in1=rho[:], op=ALU.mult), [b_sn, b_rho], [b_sn])
                dv(lambda e: e.tensor_tensor(out=den[:], in0=are[:], in1=are[:], op=ALU.mult), [b_are], [b_den])
                dv(lambda e: e.tensor_tensor(out=tmp[:], in0=aim[:], in1=aim[:], op=ALU.mult), [b_aim, b_cs], [b_tmp])
                dv(lambda e: e.tensor_tensor(out=den[:], in0=den[:], in1=tmp[:], op=ALU.add), [b_den, b_tmp], [b_den])
                dv(lambda e: e.reciprocal(out=den[:], in_=den[:]), [b_den], [b_den])
                dv(lambda e: e.tensor_tensor(out=fr[:], in0=cs[:], in1=are[:], op=ALU.mult), [b_cs, b_are], [b_fr])
                dv(lambda e: e.tensor_tensor(out=tmp[:], in0=sn[:], in1=aim[:], op=ALU.mult), [b_sn, b_aim, b_den], [b_tmp])
                dv(lambda e: e.tensor_tensor(out=fr[:], in0=fr[:], in1=tmp[:], op=ALU.add), [b_fr, b_tmp], [b_fr])
                dv(lambda e: e.tensor_tensor(out=fr[:], in0=fr[:], in1=den[:], op=ALU.mult), [b_fr, b_den], [b_fr])
                dv(lambda e: e.tensor_tensor(out=fi[:], in0=sn[:], in1=are[:], op=ALU.mult), [b_sn, b_are], [b_fi])
                dv(lambda e: e.tensor_tensor(out=tmp[:], in0=cs[:], in1=aim[:], op=ALU.mult), [b_cs, b_aim, b_fr], [b_tmp])
                dv(lambda e: e.tensor_tensor(out=fi[:], in0=fi[:], in1=tmp[:], op=ALU.subtract), [b_fi, b_tmp], [b_fi])
                dv(lambda e: e.tensor_tensor(out=fi[:], in0=fi[:], in1=den[:], op=ALU.mult), [b_fi, b_den], [b_fi])
                mev, b_mev = S.sb([128, 2], F32, "mev")
                l2, b_l2 = S.sb([128, 128], F32, "l2")
                dv(lambda e: e.reduce_sum(out=mev[:, 0:1], in_=E0[:], axis=AX.X), [b_E0], [b_mev])
                dv(lambda e: e.reduce_sum(out=mev[:, 1:2], in_=E1[:], axis=AX.X), [b_E1, b_mev], [b_mev])
                dv(lambda e: e.tensor_tensor(out=E0[:], in0=E0[:], in1=E1[:], op=ALU.add), [b_E0, b_E1, b_mev], [b_E0])
                for qi, (src, b_src, dst, b_dst) in enumerate(((rho, b_rho, rhoB, b_rhoB), (th, b_th, thB, b_thB),
                                                               (fr, b_fr, frB, b_frB), (fi, b_fi, fiB, b_fiB))):
                    ps, b_ps = psA[qi % 3]
                    for two in range(2):
                        dv(lambda e, src=src, two=two: e.tensor_scalar(out=l2[:, two * 64:(two + 1) * 64], in0=src[:], scalar1=mev[:, two:two + 1],
                                                                       scalar2=None, op0=ALU.mult), [b_src, b_mev, b_l2], [b_l2])
                    P.emit("pe", lambda e, ps=ps: e.matmul(out=ps[:, 0:64], lhsT=l2[:], rhs=E0[:], start=True, stop=True),
                           reads=[b_l2, b_E0], writes=[b_ps])
                    P.emit("act", lambda e, dst=dst, ps=ps: e.copy(out=dst[:], in_=ps[:, 0:64]), reads=[b_ps], writes=[b_dst])
                frb = frB[:].unsqueeze(2).to_broadcast([128, 64, 16])
                fib = fiB[:].unsqueeze(2).to_broadcast([128, 64, 16])
                dv(lambda e: e.tensor_tensor(out=bbr[:], in0=bre[:], in1=frb, op=ALU.mult), [b_bre, b_bre2, b_frB], [b_bbr])
                dv(lambda e: e.tensor_tensor(out=t16[:], in0=bim[:], in1=fib, op=ALU.mult), [b_bim, b_bim2, b_fiB], [b_t16])
                dv(lambda e: e.tensor_tensor(out=bbr[:], in0=bbr[:], in1=t16[:], op=ALU.subtract), [b_bbr, b_t16], [b_bbr])
                dv(lambda e: e.tensor_tensor(out=bbi[:], in0=bim[:], in1=frb, op=ALU.mult), [b_bim, b_bim2, b_frB], [b_bbi])
                dv(lambda e: e.tensor_tensor(out=t16[:], in0=bre[:], in1=fib, op=ALU.mult), [b_bre, b_bre2, b_fiB, b_bbr], [b_t16])
                dv(lambda e: e.tensor_tensor(out=bbi[:], in0=bbi[:], in1=t16[:], op=ALU.add), [b_bbi, b_t16], [b_bbi])
                barrier(P)
                S.close()

            if STOP[0] != 11:
                setup()
            if STOP[0] in (11, 12):
                barrier(P)
                M.close()
                return
            thq, b_thq = M.sb([128, 64], F32, "thq")
            P.emit("dve", lambda e: e.tensor_scalar(out=thq[:], in0=thB[:], scalar1=1.0 / (2 * PI), scalar2=None, op0=ALU.mult), reads=[b_thB], writes=[b_thq])

            Bf = [[M.sb([128, 4, 128], BF16, f"Bf{k}_{i}") for i in range(2)] for k in range(2)]
            BT = [[M.sb([128, 4, 128], BF16, f"BT{k}_{i}") for i in range(2)] for k in range(2)]
            CT = [[M.sb([128, 4, 128], BF16, f"CT{k}_{i}") for i in range(2)] for k in range(2)]
            Dg = [M.sb([128, 128], BF16, f"Dg{k}") for k in range(2)]
            Cn = [M.sb([128, 64], F32, f"Cn{i}") for i in range(2)]
            Cnb = [M.sb([128, 128], BF16, f"Cnb{i}") for i in range(2)]
            kcn = [P.key(f"cn_{i}") for i in range(2)]
            ubs = [M.sb([128, TS], BF16, f"ub{k}") for k in range(2)]
            nSs = [M.sb([128, TS], F32, f"nS{i}") for i in range(2)]
            nCs = [M.sb([128, TS], F32, f"nC{i}") for i in range(2)]
            wr, b_wr = M.sb([128, TS], F32, "wr")
            wi, b_wi = M.sb([128, TS], F32, "wi")
            xrs = [M.sb([128, TS], BF16, f"xr{i}") for i in range(2)]
            xis = [M.sb([128, TS], BF16, f"xi{i}") for i in range(2)]
            tas = [M.sb([128, 1024], F32, f"ta{i}") for i in range(2)]
            tbs = [M.sb([128, 1024], F32, f"tb{i}") for i in range(2)]
            b_wrs = [Buf(f"wr{c}") for c in range(4)]
            b_wis = [Buf(f"wi{c}") for c in range(4)]
            b_xrs = [[Buf(f"xr{i}_{h}") for h in range(2)] for i in range(2)]
            b_xis = [[Buf(f"xi{i}_{h}") for h in range(2)] for i in range(2)]
            yb, b_yb = M.sb([128, TS], BF16, "yb")
            kyb = P.key(f"yb")
            wqc = {"n": 0}
            for k in range(2):
                for bf_, _b in Bf[k] + CT[k]:
                    P.emit("pool", lambda e, bf_=bf_: e.memset(bf_[:], 0.0), writes=[_b])

            def tables(gp):
                sl_ = gp % 2
                g1 = slice(gp, gp + 1)
                S_, b_S = nSs[sl_]
                C_, b_C = nCs[sl_]
                P.emit("dve", lambda e, g1=g1, C_=C_: e.tensor_scalar(out=C_[:], in0=iota[:], scalar1=thq[:, g1], scalar2=MAGIC, op0=ALU.mult, op1=ALU.add),
                       reads=[b_iota, b_thq], writes=[b_C])
                P.emit("dve", lambda e, C_=C_: e.tensor_scalar(out=C_[:], in0=C_[:], scalar1=-MAGIC, scalar2=-2 * PI, op0=ALU.add, op1=ALU.mult),
                       reads=[b_C], writes=[b_C])
                P.emit("dve", lambda e, g1=g1, C_=C_: e.scalar_tensor_tensor(out=C_[:], in0=iota[:], scalar=thB[:, g1], in1=C_[:], op0=ALU.mult, op1=ALU.add),
                       reads=[b_iota, b_thB, b_C], writes=[b_C])
                P.emit("dve", lambda e, C_=C_, S_=S_: e.tensor_scalar(out=S_[:], in0=C_[:], scalar1=-PI, scalar2=PI, op0=ALU.max, op1=ALU.min),
                       reads=[b_C], writes=[b_S])
                P.emit("act", lambda e, C_=C_, S_=S_: e.activation(out=C_[:], in_=S_[:], func=AF.Sin, scale=0.5), reads=[b_S, b_C], writes=[b_C])
                P.emit("act", lambda e, C_=C_: e.activation(out=C_[:], in_=C_[:], func=AF.Square), reads=[b_C], writes=[b_C])
                P.emit("act", lambda e, S_=S_: e.activation(out=S_[:], in_=S_[:], func=AF.Sin), reads=[b_S, b_C], writes=[b_S])
                P.emit("act", lambda e, C_=C_: e.activation(out=C_[:], in_=C_[:], func=AF.Identity, bias=1.0, scale=-2.0),
                       reads=[b_C], writes=[b_C])

            def rotate(gp, r):
                sl_ = gp % 2
                k = (gp // 4) % 2
                g1 = slice(gp, gp + 1)
                nS, b_nS = nSs[sl_]
                nC, b_nC = nCs[sl_]
                xr, b_xr = xrs[sl_]
                xi, b_xi = xis[sl_]
                ub, b_ub = ubs[k]
                rho_bc = rhoB[:, g1].to_broadcast([128, TS])
                dv = lambda fn, r_, w_: P.emit("dve", fn, reads=r_, writes=w_)
                for pair in range(2):
                    cs = (2 * pair, 2 * pair + 1)
                    sls = [slice(ch * 512, (ch + 1) * 512) for ch in cs]
                    pRs = [psA[0], psA[2]]
                    pIs = [psA[1], psA[3]]
                    for j, ch in enumerate(cs):
                        pR, b_pR = pRs[j]
                        pI, b_pI = pIs[j]
                        sl = sls[j]
                        P.emit("pe", lambda e, r=r, sl=sl, pR=pR, k=k, ub=ub: e.matmul(out=pR[:, :], lhsT=BT[k][0][0][:, r, :], rhs=ub[:, sl], start=True, stop=True),
                               reads=[BT[k][0][1], b_ub], writes=[b_pR])
                        P.emit("pe", lambda e, r=r, sl=sl, pI=pI, k=k, ub=ub: e.matmul(out=pI[:, :], lhsT=BT[k][1][0][:, r, :], rhs=ub[:, sl], start=True, stop=True),
                               reads=[BT[k][1][1], b_ub], writes=[b_pI])
                    tq = [(tas[j][0][:, 0:512], tas[j][1], tbs[j][0][:, 0:512], tbs[j][1]) for j in range(2)]
                    for j in range(2):
                        dv(lambda e, sl=sls[j], pR=pRs[j][0]: e.tensor_tensor(out=wr[:, sl], in0=pR[:, :], in1=nC[:, sl], op=ALU.mult), [pRs[j][1], b_nC], [b_wrs[cs[j]]])
                    for j in range(2):
                        dv(lambda e, sl=sls[j], pI=pIs[j][0], ta=tq[j][0]: e.tensor_tensor(out=ta, in0=pI[:, :], in1=nS[:, sl], op=ALU.mult), [pIs[j][1], b_nS], [tq[j][1]])
                    for j in range(2):
                        dv(lambda e, sl=sls[j], ta=tq[j][0]: e.tensor_tensor(out=wr[:, sl], in0=wr[:, sl], in1=ta, op=ALU.add), [b_wrs[cs[j]], tq[j][1]], [b_wrs[cs[j]]])
                    for j in range(2):
                        dv(lambda e, sl=sls[j], pI=pIs[j][0]: e.tensor_tensor(out=wi[:, sl], in0=pI[:, :], in1=nC[:, sl], op=ALU.mult), [pIs[j][1], b_nC], [b_wis[cs[j]]])
                    for j in range(2):
                        dv(lambda e, sl=sls[j], pR=pRs[j][0], tb=tq[j][2]: e.tensor_tensor(out=tb, in0=pR[:, :], in1=nS[:, sl], op=ALU.mult), [pRs[j][1], b_nS], [tq[j][3]])
                    for j in range(2):
                        dv(lambda e, sl=sls[j], tb=tq[j][2]: e.tensor_tensor(out=wi[:, sl], in0=wi[:, sl], in1=tb, op=ALU.subtract), [b_wis[cs[j]], tq[j][3]], [b_wis[cs[j]]])
                dv(lambda e: e.tensor_tensor_scan(out=wr[:], data0=rho_bc, data1=wr[:], initial=0.0, op0=ALU.mult, op1=ALU.add), [b_rhoB] + b_wrs, b_wrs)
                dv(lambda e: e.tensor_tensor_scan(out=wi[:], data0=rho_bc, data1=wi[:], initial=0.0, op0=ALU.mult, op1=ALU.add), [b_rhoB] + b_wis, b_wis)
                hs = [slice(0, 1024), slice(1024, 2048)]
                TA = [(tas[j][0], tas[j][1]) for j in range(2)]
                TB = [(tbs[j][0], tbs[j][1]) for j in range(2)]
                bw = [b_wrs[0:2], b_wrs[2:4]]
                bi = [b_wis[0:2], b_wis[2:4]]
                for j in range(2):
                    dv(lambda e, h=hs[j], ta=TA[j][0]: e.tensor_tensor(out=ta[:], in0=wr[:, h], in1=nC[:, h], op=ALU.mult), bw[j] + [b_nC], [TA[j][1]])
                for j in range(2):
                    dv(lambda e, h=hs[j], tb=TB[j][0]: e.tensor_tensor(out=tb[:], in0=wi[:, h], in1=nS[:, h], op=ALU.mult), bi[j] + [b_nS], [TB[j][1]])
                for j in range(2):
                    dv(lambda e, h=hs[j], ta=TA[j][0], tb=TB[j][0]: e.tensor_tensor(out=xr[:, h], in0=ta[:], in1=tb[:], op=ALU.subtract), [TA[j][1], TB[j][1]], [b_xrs[sl_][j]])
                for j in range(2):
                    dv(lambda e, h=hs[j], ta=TA[j][0]: e.tensor_tensor(out=ta[:], in0=wr[:, h], in1=nS[:, h], op=ALU.mult), bw[j] + [b_nS], [TA[j][1]])
                for j in range(2):
                    dv(lambda e, h=hs[j], tb=TB[j][0]: e.tensor_tensor(out=tb[:], in0=wi[:, h], in1=nC[:, h], op=ALU.mult), bi[j] + [b_nC], [TB[j][1]])
                for j in range(2):
                    dv(lambda e, h=hs[j], ta=TA[j][0], tb=TB[j][0]: e.tensor_tensor(out=xi[:, h], in0=ta[:], in1=tb[:], op=ALU.add), [TA[j][1], TB[j][1]], [b_xis[sl_][j]])
                for ch in range(4):
                    sl = slice(ch * 512, (ch + 1) * 512)
                    pY, b_pY = psY[ch]
                    if r == 0:
                        P.emit("pe", lambda e, sl=sl, pY=pY, k=k, ub=ub: e.matmul(out=pY[:, :], lhsT=Dg[k][0][:], rhs=ub[:, sl], start=True, stop=False),
                               reads=[Dg[k][1], b_ub], writes=[b_pY])
                    P.emit("pe", lambda e, r=r, sl=sl, pY=pY, k=k, xr=xr: e.matmul(out=pY[:, :], lhsT=CT[k][0][0][:, r, :], rhs=xr[:, sl], start=False, stop=False),
                           reads=[CT[k][0][1], b_xrs[sl_][ch // 2]], writes=[b_pY])
                    P.emit("pe", lambda e, r=r, sl=sl, pY=pY, k=k, xi=xi: e.matmul(out=pY[:, :], lhsT=CT[k][1][0][:, r, :], rhs=xi[:, sl], start=False, stop=(r == 3)),
                           reads=[CT[k][1][1], b_xis[sl_][ch // 2]], writes=[b_pY])

            def prep(ct):
                k = ct % 2
                i3 = wqc["n"] % 3
                wqc["n"] += 1
                wt, b_wt = wq[i3]
                ub, b_ub = ubs[k]
                P.dma("pool", wt[:], w_in[:, ct * 128:(ct + 1) * 128].rearrange("(kc p) n -> p kc n", p=128), kwq[i3], writes=[b_wt])

                def ev_u(ch, ps, b_ps):
                    sl = slice(ch * 512, (ch + 1) * 512)
                    P.emit("act", lambda e, ps=ps, sl=sl, ub=ub: e.copy(out=ub[:, sl], in_=ps[:, :]), reads=[b_ps], writes=[b_ub])
                proj_fm(P, hT, b_hT, wt, b_wt, psA[0:2], ev_u)
                dg_, b_dg = Dg[k]
                P.emit("dve", lambda e, dg_=dg_, ct=ct: e.tensor_scalar(out=dg_[:], in0=ident[:], scalar1=dsk[:, ct:ct + 1], scalar2=None, op0=ALU.mult),
                       reads=[b_id, b_dsk], writes=[b_dg])
                for ri in range(2):
                    src = (bbr, bbi)[ri]
                    b_src = (b_bbr, b_bbi)[ri]
                    bf_, b_bf = Bf[k][ri]
                    for r in range(4):
                        gp = 4 * ct + r
                        for two in range(2):
                            col0 = (2 * r + two) * 16
                            P.emit("pool", lambda e, bf_=bf_, r=r, two=two, col0=col0, gp=gp, src=src: e.tensor_copy(
                                out=bf_[two * 64:(two + 1) * 64, r, col0:col0 + 16], in_=src[two * 64:(two + 1) * 64, gp, :]),
                                reads=[b_src, b_bf], writes=[b_bf])
                    for r in range(4):
                        P.emit("pe", lambda e, bf_=bf_, r=r: e.transpose(out=psTb[:, r, :], in_=bf_[:, r, :], identity=ident[:]),
                               reads=[b_bf, b_id], writes=[b_psTb])
                    bt_, b_bt = BT[k][ri]
                    P.emit("act", lambda e, bt_=bt_: e.copy(out=bt_[:], in_=psTb[:, 0:4, :]), reads=[b_psTb], writes=[b_bt])
                    cn_, b_cn = Cn[ri]
                    cnb_, b_cnb = Cnb[ri]
                    csrc = W["c_re"] if ri == 0 else W["c_im"]
                    P.dma("sp", cn_[:], csrc[ct * 8:(ct + 1) * 8].rearrange("g c p -> (g c) p"), kcn[ri], writes=[b_cn])
                    for two in range(2):
                        P.emit("act", lambda e, cn_=cn_, cnb_=cnb_, ri=ri, two=two: e.activation(out=cnb_[:, two * 64:(two + 1) * 64], in_=cn_[:], func=AF.Identity,
                                                                                                 scale=(1.0 if ri == 0 else -1.0)),
                               reads=[b_cn, b_cnb], writes=[b_cnb])
                    P.emit("pe", lambda e, cnb_=cnb_, ri=ri: e.transpose(out=psTb[:, 4 + ri, :], in_=cnb_[:, :], identity=ident[:]),
                           reads=[b_cnb, b_id], writes=[b_psTb])
                    ct_, b_ctt = CT[k][ri]
                    for r in range(4):
                        for two in range(2):
                            col0 = (2 * r + two) * 16
                            P.emit("act", lambda e, ct_=ct_, r=r, two=two, col0=col0, ri=ri: e.copy(
                                out=ct_[two * 64:(two + 1) * 64, r, col0:col0 + 16], in_=psTb[two * 64:(two + 1) * 64, 4 + ri, col0:col0 + 16]),
                                reads=[b_psTb, b_ctt], writes=[b_ctt])

            def gelu_out(ct):
                for ch in range(4):
                    sl = slice(ch * 512, (ch + 1) * 512)
                    pY, b_pY = psY[ch]
                    tb, b_tb = tbs[ch % 2][0][:, 0:512], tbs[ch % 2][1]
                    P.emit("act", lambda e, pY=pY, tb=tb: e.activation(out=tb, in_=pY[:, :], func=AF.Square), reads=[b_pY], writes=[b_tb])
                    P.emit("dve", lambda e, tb=tb: e.tensor_scalar(out=tb, in0=tb, scalar1=0.044715, scalar2=1.0, op0=ALU.mult, op1=ALU.add), reads=[b_tb], writes=[b_tb])
                    P.emit("dve", lambda e, tb=tb, pY=pY: e.tensor_tensor(out=tb, in0=pY[:, :], in1=tb, op=ALU.mult), reads=[b_tb, b_pY], writes=[b_tb])
                    P.emit("act", lambda e, tb=tb: e.activation(out=tb, in_=tb, func=AF.Sigmoid, scale=GELU_K), reads=[b_tb], writes=[b_tb])
                    P.emit("dve", lambda e, sl=sl, tb=tb, pY=pY: e.tensor_tensor(out=yb[:, sl], in0=pY[:, :], in1=tb, op=ALU.mult), reads=[b_tb, b_pY], writes=[b_yb])
                P.dma("sp", yscr[ct * 128:(ct + 1) * 128, :], yb[:], kyb, reads=[b_yb], writes=[P.dram_buf(yscr, 0)])

            prep(0)
            tables(0)
            for ct in range(16):
                for r in range(4):
                    gp = 4 * ct + r
                    if r == 1 and ct + 1 < 16:
                        prep(ct + 1)
                    if gp + 1 < 64:
                        tables(gp + 1)
                    rotate(gp, r)
                gelu_out(ct)
            barrier(P)
            M.close()

        main_phase()
        O.close()
        if STOP[0] >= 11:
            return

        def comb(P, cur, x_t, b_x, c, tmp):
            (pv, b_pv), (pg, b_pg) = cur
            tm, b_tm = tmp
            csl = slice(c * 512, (c + 1) * 512)
            P.emit("act", lambda e, pg=pg, tm=tm: e.activation(out=tm[:], in_=pg[:, :], func=AF.Sigmoid), reads=[b_pg], writes=[b_tm])
            P.emit("dve", lambda e, pv=pv, tm=tm: e.tensor_tensor(out=tm[:], in0=pv[:, :], in1=tm[:], op=ALU.mult), reads=[b_pv, b_tm], writes=[b_tm])
            P.emit("dve", lambda e, tm=tm, x_t=x_t, csl=csl: e.tensor_tensor(out=x_t[:, csl], in0=x_t[:, csl], in1=tm[:], op=ALU.add),
                   reads=[b_tm, b_x], writes=[b_x])
        outproj_phase(P, tag, xin, xout, base, yscr, [W["w_glu_v"], W["w_glu_g"]], comb)
        barrier(P)

    for sq in range(NSEQ):
        seq_body(sq)


NSEQ_CORE = 2
T_CORE = NSEQ_CORE * TS
N_CORES = 8

_SPECS = [
    ("x", [T_CORE, D]), ("norm_mix", [2, D]), ("norm_ffn", [2, D]), ("norm_final", [1, D]),
    ("ab_w_in", [D, 6152]), ("lru_conv_w", [4, 1024]), ("lru_conv_b", [1024]), ("lru_w_a", [8, 128, 128]),
    ("lru_b_a", [1024]), ("lru_w_x", [8, 128, 128]), ("lru_b_x", [1024]), ("lru_lam", [1024]),
    ("m_conv_w", [4, 2048]), ("m_conv_b", [2048]), ("m_i_bias", [1, 4]), ("m_f_bias", [1, 4]), ("m_head_g", [1, 1024]),
    ("ab_w_out", [D, D]), ("s5_w_in", [D, D]), ("s5_a_re", [128, 64]), ("s5_a_im", [128, 64]), ("s5_log_step", [128]),
    ("s5_b_re", [128, 64, 16]), ("s5_b_im", [128, 64, 16]), ("s5_c_re", [128, 16, 64]), ("s5_c_im", [128, 16, 64]),
    ("s5_d", [2048]), ("s5_w_glu_v", [D, D]), ("s5_w_glu_g", [D, D]),
    ("moe_w_coarse", [2, D, 4]), ("moe_b_coarse", [2, 4]), ("moe_w_fine", [2, D, 16]), ("moe_b_fine", [2, 16]),
    ("moe_w_gate", [2, NEXP, D, FF]), ("moe_w_up", [2, NEXP, D, FF]), ("moe_w_down", [2, NEXP, FF, D]),
]


def build_program():
    nc = bass.Bass("TRN2", target_bir_lowering=False)
    A = {n: nc.dram_tensor(n, list(shp), F32, kind="ExternalInput").ap() for n, shp in _SPECS}
    out = nc.dram_tensor("out", [T_CORE, D], F32, kind="ExternalOutput").ap()
    xs = [nc.dram_tensor(f"xs{i}", [T_CORE, D], F32, kind="Internal").ap() for i in range(3)]
    yscr = [nc.dram_tensor(f"yscr{i}", [D, TS], BF16, kind="Internal").ap() for i in range(NSEQ_CORE)]
    P = Prog(nc)
    WA = {"norm": A["norm_mix"][0:1, :], "w_in": A["ab_w_in"], "lru_conv_w": A["lru_conv_w"], "lru_conv_b": A["lru_conv_b"],
          "lru_w_a": A["lru_w_a"], "lru_b_a": A["lru_b_a"], "lru_w_x": A["lru_w_x"], "lru_b_x": A["lru_b_x"],
          "lru_lam": A["lru_lam"], "m_conv_w": A["m_conv_w"], "m_conv_b": A["m_conv_b"], "m_i_bias": A["m_i_bias"],
          "m_f_bias": A["m_f_bias"], "m_head_g": A["m_head_g"], "w_out": A["ab_w_out"]}
    WS = {"norm": A["norm_mix"][1:2, :], "w_in": A["s5_w_in"], "a_re": A["s5_a_re"], "a_im": A["s5_a_im"],
          "log_step": A["s5_log_step"], "b_re": A["s5_b_re"], "b_im": A["s5_b_im"], "c_re": A["s5_c_re"],
          "c_im": A["s5_c_im"], "d": A["s5_d"], "w_glu_v": A["s5_w_glu_v"], "w_glu_g": A["s5_w_glu_g"]}

    def moe(layer, xi, xo, gfin):
        moe_stage(P, xi, xo, T_CORE, A["norm_ffn"][layer:layer + 1, :], A["moe_w_coarse"][layer],
                  A["moe_b_coarse"][layer:layer + 1, :], A["moe_w_fine"][layer], A["moe_b_fine"][layer:layer + 1, :],
                  A["moe_w_gate"][layer], A["moe_w_up"][layer], A["moe_w_down"][layer], g_final=gfin)
        barrier(P)

    mixer_a_stage(P, A["x"], xs[0], NSEQ_CORE, WA, yscr)
    moe(0, xs[0], xs[1], None)
    s5_stage(P, xs[1], xs[2], NSEQ_CORE, WS, yscr)
    moe(1, xs[2], out, A["norm_final"])
    P.finish()
    P.build()
    P.close()
    return nc


def kernel(**inputs):
    f = lambda a: np.ascontiguousarray(np.asarray(a, dtype=np.float32))
    x = f(inputs["x"])
    B = x.shape[0]
    shared = {}
    for n, shp in _SPECS:
        if n == "x":
            continue
        shared[n] = f(inputs[n]).reshape(shp)
    nc = build_program()
    in_maps = []
    for c in range(N_CORES):
        m = dict(shared)
        m["x"] = x[c * NSEQ_CORE:(c + 1) * NSEQ_CORE].reshape(T_CORE, D)
        in_maps.append(m)
    res = run_bass_kernel_spmd(nc, in_maps, core_ids=list(range(N_CORES)))
    outs = [np.asarray(r["out"], dtype=np.float32).reshape(NSEQ_CORE, TS, D) for r in res.results]
    return np.concatenate(outs, axis=0)
```

```python
import numpy as np
from contextlib import ExitStack
import concourse.bass as bass
import concourse.mybir as mybir
from concourse.bass_utils import run_bass_kernel_spmd

F32 = mybir.dt.float32
BF16 = mybir.dt.bfloat16
ALU = mybir.AluOpType
AF = mybir.ActivationFunctionType
AX = mybir.AxisListType


class Buf:
    __slots__ = ("name", "lw", "rd")

    def __init__(self, name):
        self.name = name
        self.lw = None
        self.rd = []


class SemKey:
    __slots__ = ("name", "sem", "count", "group", "epoch", "totals")

    def __init__(self, name, group=False):
        self.name = name
        self.sem = None
        self.count = 0
        self.group = group


class Op:
    __slots__ = ("eng", "fn", "deps", "sig", "signo", "key", "idx", "ep")

    def __init__(self, eng, fn, key=None):
        self.eng = eng
        self.fn = fn
        self.deps = []
        self.sig = False
        self.signo = 0
        self.key = key
        self.idx = 0


ENGS = ("pe", "act", "dve", "pool", "sp")


class Prog:
    def __init__(self, nc):
        self.nc = nc
        self.stack = ExitStack()
        self.ops = {e: [] for e in ENGS}
        self.nops = 0
        self.keys = []
        self.ntile = 0
        self.bar_idx = 0
        self.out_ops = []
        self.stage_stacks = []
        self._uid = 0
        self._dbufs = {}
        self._keyreg = {}

    def uid(self):
        self._uid += 1
        return self._uid

    def dram_buf(self, ap, r0):
        k = (ap.tensor.name, r0)
        if k not in self._dbufs:
            self._dbufs[k] = Buf(f"d_{k}")
        return self._dbufs[k]

    def finish(self):
        op = Op("sp", None)
        op.idx = self.nops
        self.nops += 1
        op.deps = list(self.out_ops)
        self.ops["sp"].append(op)

    def sb(self, shape, dtype, name=None):
        self.ntile += 1
        name = name or f"t{self.ntile}"
        return self.stack.enter_context(self.nc.sbuf_tensor(name, list(shape), dtype))

    def ps(self, shape, dtype, name=None):
        self.ntile += 1
        name = name or f"p{self.ntile}"
        return self.stack.enter_context(self.nc.psum_tensor(name, list(shape), dtype))

    def key(self, name, group=False):
        if name in self._keyreg:
            k = self._keyreg[name]
            assert k.group == group
            return k
        k = SemKey(name, group)
        k.epoch = 0
        k.totals = {}
        self._keyreg[name] = k
        self.keys.append(k)
        return k

    def close_epochs(self):
        for k in self.keys:
            if k.group:
                k.totals[k.epoch] = k.count
                k.epoch += 1

    def _deps(self, op, reads, writes):
        deps = []
        for b in reads:
            if b.lw is not None:
                deps.append(b.lw)
        for b in writes:
            if b.lw is not None:
                deps.append(b.lw)
            deps.extend(b.rd)
        for b in reads:
            b.rd.append(op)
        for b in writes:
            b.lw = op
            b.rd = []
        seen = set()
        for d in deps:
            if d is op or id(d) in seen:
                continue
            seen.add(id(d))
            if d.key is None and d.eng == "pe" and op.eng == "pe" and op.key is None:
                continue
            op.deps.append(d)
            d.sig = True

    def emit(self, eng, fn, reads=(), writes=()):
        op = Op(eng, fn)
        op.idx = self.nops
        self.nops += 1
        self._deps(op, reads, writes)
        self.ops[eng].append(op)
        return op

    def dma(self, eng, out, in_, key, reads=(), writes=(), **kw):
        def fn(e, out=out, in_=in_, kw=kw):
            return e.dma_start(out=out, in_=in_, **kw)
        op = Op(eng, fn, key=key)
        op.idx = self.nops
        self.nops += 1
        self._deps(op, reads, writes)
        key.count += 16
        op.signo = key.count
        op.ep = key.epoch
        op.sig = True
        self.ops[eng].append(op)
        return op

    def build(self):
        nc = self.nc
        st = self.stack
        self.close_epochs()
        esem = {e: st.enter_context(nc.semaphore(f"s_{e}")) for e in ENGS}
        for k in self.keys:
            if k.count > 0:
                k.sem = st.enter_context(nc.semaphore(f"k_{k.name}"))
        for e in ENGS:
            c = 0
            for op in self.ops[e]:
                if op.key is None and op.sig:
                    c += 1
                    op.signo = c
        ops = self.ops

        def run(ename, eng):
            waited = {}
            for op in ops[ename]:
                need = {}
                for d in op.deps:
                    if d.key is not None:
                        s = d.key.sem
                        v = d.key.totals[d.ep] if d.key.group else d.signo
                    else:
                        s = esem[d.eng]
                        v = d.signo
                    sid = id(s)
                    if v > need.get(sid, (None, 0))[1]:
                        need[sid] = (s, v)
                for sid, (s, v) in need.items():
                    if waited.get(sid, 0) >= v:
                        continue
                    waited[sid] = v
                    eng.wait_ge(s, v)
                if op.fn is None:
                    continue
                ins = op.fn(eng)
                if op.key is not None:
                    ins.then_inc(op.key.sem, 16)
                elif op.sig:
                    ins.then_inc(esem[ename], 1)

        block = st.enter_context(nc.Block())

        @block.tensor
        def _(e):
            run("pe", e)

        @block.scalar
        def _(e):
            run("act", e)

        @block.vector
        def _(e):
            run("dve", e)

        @block.gpsimd
        def _(e):
            run("pool", e)

        @block.sync
        def _(e):
            run("sp", e)

    def close(self):
        self.stack.close()
        for st in reversed(self.stage_stacks):
            st.close()


D = 2048
EPS = 1e-6
NEXP = 16
FF = 512


class Ctx:
    pass


def barrier(P):
    lasts = []
    for e in ENGS:
        if P.ops[e]:
            for op in reversed(P.ops[e]):
                if op.fn is not None and op.key is None:
                    lasts.append(op)
                    op.sig = True
                    break
    dmas = [op for e in ENGS for op in P.ops[e] if op.key is not None and op.idx >= P.bar_idx]
    for e in ENGS:
        op = Op(e, None)
        op.idx = P.nops
        P.nops += 1
        op.deps = [d for d in lasts] + dmas
        P.ops[e].append(op)
    P.bar_idx = P.nops
    P.close_epochs()


def rmsnorm_rows(P, src, dst, gbc, b_src, b_dst, b_g, scr, tag):
    junk, ssq, rstd, b_junk, b_ssq, b_rstd = scr
    P.emit("act", lambda e: e.activation(out=junk, in_=src, func=AF.Square, accum_out=ssq),
           reads=[b_src], writes=[b_junk, b_ssq])
    P.emit("dve", lambda e: e.tensor_scalar(out=rstd, in0=ssq, scalar1=1.0 / D, scalar2=EPS,
                                            op0=ALU.mult, op1=ALU.add), reads=[b_ssq], writes=[b_rstd])
    P.emit("act", lambda e: e.activation(out=rstd, in_=rstd, func=AF.Sqrt), reads=[b_rstd], writes=[b_rstd])
    P.emit("dve", lambda e: e.reciprocal(out=rstd, in_=rstd), reads=[b_rstd], writes=[b_rstd])
    P.emit("dve", lambda e: e.scalar_tensor_tensor(out=dst, in0=src, scalar=rstd, in1=gbc,
                                                   op0=ALU.mult, op1=ALU.mult),
           reads=[b_src, b_rstd, b_g], writes=[b_dst])


def moe_stage(P, xin, xout, T, g_ffn, w_coarse, b_coarse, w_fine, b_fine, w_gate, w_up, w_down,
              g_final=None, nexp=NEXP, TT=1024):
    nc = P.nc
    NS = TT // 128
    NH = TT // 512
    ntiles = T // TT
    st = ExitStack()
    sbuf = lambda shape, dt, name: st.enter_context(nc.sbuf_tensor(name, list(shape), dt))
    psum = lambda shape, dt, name: st.enter_context(nc.psum_tensor(name, list(shape), dt))
    u = P.uid()

    yacc = sbuf([128, NS, D], F32, f"yacc{u}")
    hT = sbuf([128, 16, TT], BF16, f"hT{u}")
    hns = [sbuf([128, D], BF16, f"hn{u}_{i}") for i in range(2)]
    hn = hns[0]
    gbc = sbuf([128, D], F32, f"gbc{u}")
    NSLOT = 5 if g_final is None else 4
    ring = [sbuf([128, 16 * 512], BF16, f"ring{u}_{i}") for i in range(NSLOT)]
    sg = [sbuf([128, 512], F32, f"sg{u}_{i}") for i in range(2)]
    he = sbuf([128, 4, TT], BF16, f"he{u}")
    wr = sbuf([128, 16, 20], BF16, f"wr{u}")
    brc = sbuf([128, 20], F32, f"brc{u}")
    ident = sbuf([128, 128], BF16, f"ident{u}")
    gates = sbuf([128, NS, 16], F32, f"gates{u}")
    sm = sbuf([128, NS, 64], F32, f"sm{u}")
    ssq = sbuf([128, NS], F32, f"ssq{u}")
    rstd = sbuf([128, NS], F32, f"rstd{u}")
    if g_final is not None:
        gfin = sbuf([128, D], F32, f"gfin{u}")

    psG = [psum([128, 512], F32, f"psG{u}_{i}") for i in range(2)]
    psU = [psum([128, 512], F32, f"psU{u}_{i}") for i in range(2)]
    psY = [psum([128, 512], F32, f"psY{u}_{i}") for i in range(2)]
    psT = [psum([128, 8, 128], BF16, f"psT{u}_{i}") for i in range(2)]

    B = lambda n: Buf(f"{n}{u}")
    b_yacc = [B(f"yacc{s}") for s in range(NS)]
    b_hT = [B(f"hT{s}") for s in range(NS)]
    b_hn, b_g, b_wr, b_br, b_id, b_he = B("hn"), B("g"), B("wr"), B("br"), B("id"), B("he")
    b_hns = [b_hn, B("hn1")]
    b_ring = [B(f"ring{i}") for i in range(NSLOT)]
    b_sg = [B("sg0"), B("sg1")]
    b_gates = [B(f"gates{s}") for s in range(NS)]
    b_sm = [B(f"sm{s}") for s in range(NS)]
    b_ssq = [B(f"ssq{s}") for s in range(NS)]
    b_rstd = [B(f"rstd{s}") for s in range(NS)]
    b_psG, b_psU, b_psY, b_psT = [B("pg0"), B("pg1")], [B("pu0"), B("pu1")], [B("py0"), B("py1")], [B("pt0"), B("pt1")]
    k_c = P.key(f"mc", group=True)
    k_cp = P.key(f"mcp", group=True)
    k_x = [P.key(f"mx_{s}") for s in range(NS)]
    k_ring = [P.key(f"mr_{i}") for i in range(NSLOT)]

    P.dma("sp", gbc[:], g_ffn.partition_broadcast(128), k_c, writes=[b_g])
    b_br2, b_wr2 = B("br2"), B("wr2")
    P.dma("sp", brc[:, 0:4], b_coarse.partition_broadcast(128), k_c, writes=[b_br])
    P.dma("sp", brc[:, 4:20], b_fine.partition_broadcast(128), k_c, writes=[b_br2])
    P.dma("pool", wr[:, :, 0:4], w_coarse.rearrange("(kc p) n -> p kc n", p=128), k_cp, writes=[b_wr])
    P.dma("pool", wr[:, :, 4:20], w_fine.rearrange("(kc p) n -> p kc n", p=128), k_cp, writes=[b_wr2])
    if g_final is not None:
        b_gf = B("gf")
        P.dma("sp", gfin[:], g_final.partition_broadcast(128), k_c, writes=[b_gf])
    P.emit("pool", lambda e: e.memset(ident[:], 1.0), writes=[b_id])
    P.emit("pool", lambda e: e.affine_select(out=ident[:], in_=ident[:], pattern=[[-1, 128]],
                                              compare_op=ALU.is_equal, fill=0.0, base=0,
                                              channel_multiplier=1), reads=[b_id], writes=[b_id])

    wseq = []
    for t in range(ntiles):
        for ex in range(nexp):
            for which in range(3):
                wseq.append((t, ex, which))
    wslot = {}
    state = {"next": 0}

    def issue_w(upto):
        while state["next"] < min(upto, len(wseq)):
            i = state["next"]
            t, ex, which = wseq[i]
            sl = i % NSLOT
            wslot[(t, ex, which)] = sl
            if which < 2:
                src = (w_gate if which == 0 else w_up)[ex].rearrange("(kc p) n -> p kc n", p=128)
                dst = ring[sl][:, :].rearrange("p (kc n) -> p kc n", kc=16)
            else:
                src = w_down[ex].rearrange("(f p) n -> p f n", p=128)
                dst = ring[sl][:, :].rearrange("p (f n) -> p f n", f=4)
            P.dma("pool", dst, src, k_ring[sl], writes=[b_ring[sl]])
            state["next"] += 1

    issue_w(NSLOT)
    gcount = 0
    ycount = 0
    for t in range(ntiles):
        for s in range(NS):
            r0 = t * TT + s * 128
            P.dma("sp", yacc[:, s, :], xin[r0:r0 + 128, :], k_x[s], reads=[P.dram_buf(xin, r0)], writes=[b_yacc[s]])
        for s in range(NS):
            hn_s, b_hn_s = hns[s % 2], b_hns[s % 2]
            rmsnorm_rows(P, yacc[:, s, :], hn_s[:], gbc[:], b_yacc[s], b_hn_s, b_g,
                         (hn_s[:], ssq[:, s:s + 1], rstd[:, s:s + 1], b_hn_s, b_ssq[s], b_rstd[s]), "m")
            for half in range(2):
                pt = psT[half]
                for j in range(8):
                    kc = half * 8 + j
                    P.emit("pe", lambda e, pt=pt, j=j, kc=kc, hn_s=hn_s: e.transpose(
                        out=pt[:, j, :], in_=hn_s[:, kc * 128:(kc + 1) * 128], identity=ident[:]),
                        reads=[b_hn_s, b_id], writes=[b_psT[half]])
                eng = "act" if half == 0 else "dve"
                if eng == "act":
                    P.emit("act", lambda e, pt=pt, half=half, s=s: e.copy(
                        out=hT[:, half * 8:half * 8 + 8, s * 128:(s + 1) * 128], in_=pt[:, :, :]),
                        reads=[b_psT[half]], writes=[b_hT[s]])
                else:
                    P.emit("dve", lambda e, pt=pt, half=half, s=s: e.tensor_copy(
                        out=hT[:, half * 8:half * 8 + 8, s * 128:(s + 1) * 128], in_=pt[:, :, :]),
                        reads=[b_psT[half]], writes=[b_hT[s]])
            pr = psY[1]
            for kc in range(16):
                P.emit("pe", lambda e, kc=kc, s=s, pr=pr: e.matmul(
                    out=pr[:, 0:20], lhsT=hT[:, kc, s * 128:(s + 1) * 128], rhs=wr[:, kc, :],
                    start=(kc == 0), stop=(kc == 15)),
                    reads=[b_hT[s], b_wr, b_wr2], writes=[b_psY[1]])
            S = sm[:, s, :]
            lg, gmax, ohg, ngmax, ex4, sume, pg = S[:, 0:20], S[:, 20:21], S[:, 21:25], S[:, 25:26], S[:, 26:30], S[:, 30:31], S[:, 31:32]
            lfs, m1, mk1, lf2, m2, mk2 = S[:, 32:36], S[:, 36:37], S[:, 37:41], S[:, 41:45], S[:, 45:46], S[:, 46:50]
            d21, e2, den, wa, wb, gsel = S[:, 50:51], S[:, 51:52], S[:, 52:53], S[:, 53:54], S[:, 54:55], S[:, 55:59]
            bs = b_sm[s]

            def dv(fn, extra_r=(), extra_w=()):
                P.emit("dve", fn, reads=[bs] + list(extra_r), writes=[bs] + list(extra_w))

            def ac(fn):
                P.emit("act", fn, reads=[bs], writes=[bs])
            dv(lambda e, lg=lg, pr=pr: e.tensor_tensor(out=lg, in0=pr[:, 0:20], in1=brc[:], op=ALU.add),
               extra_r=[b_psY[1], b_br, b_br2])
        for s in range(NS):
            pr = psY[1]
            S = sm[:, s, :]
            lg, gmax, ohg, ngmax, ex4, sume, pg = S[:, 0:20], S[:, 20:21], S[:, 21:25], S[:, 25:26], S[:, 26:30], S[:, 30:31], S[:, 31:32]
            lfs, m1, mk1, lf2, m2, mk2 = S[:, 32:36], S[:, 36:37], S[:, 37:41], S[:, 41:45], S[:, 45:46], S[:, 46:50]
            d21, e2, den, wa, wb, gsel = S[:, 50:51], S[:, 51:52], S[:, 52:53], S[:, 53:54], S[:, 54:55], S[:, 55:59]
            bs = b_sm[s]

            def dv(fn, extra_r=(), extra_w=()):
                P.emit("dve", fn, reads=[bs] + list(extra_r), writes=[bs] + list(extra_w))

            def ac(fn):
                P.emit("act", fn, reads=[bs], writes=[bs])
            dv(lambda e, lg=lg, gmax=gmax: e.reduce_max(out=gmax, in_=lg[:, 0:4], axis=AX.X))
            dv(lambda e, lg=lg, gmax=gmax, ohg=ohg: e.tensor_scalar(out=ohg, in0=lg[:, 0:4], scalar1=gmax, scalar2=None, op0=ALU.is_ge))
            dv(lambda e, gmax=gmax, ngmax=ngmax: e.tensor_scalar(out=ngmax, in0=gmax, scalar1=-1.0, scalar2=None, op0=ALU.mult))
            ac(lambda e, lg=lg, ngmax=ngmax, ex4=ex4, sume=sume: e.activation(out=ex4, in_=lg[:, 0:4], func=AF.Exp, bias=ngmax, scale=1.0, accum_out=sume))
            dv(lambda e, pg=pg, sume=sume: e.reciprocal(out=pg, in_=sume))
            dv(lambda e, lg=lg, lfs=lfs, ohg=ohg: e.tensor_scalar(out=lfs, in0=lg[:, 4:8], scalar1=ohg[:, 0:1], scalar2=None, op0=ALU.mult))
            for g in range(1, 4):
                dv(lambda e, lg=lg, lfs=lfs, ohg=ohg, g=g: e.scalar_tensor_tensor(
                    out=lfs, in0=lg[:, 4 + 4 * g:8 + 4 * g], scalar=ohg[:, g:g + 1], in1=lfs, op0=ALU.mult, op1=ALU.add))
            dv(lambda e, lfs=lfs, m1=m1: e.reduce_max(out=m1, in_=lfs, axis=AX.X))
            dv(lambda e, lfs=lfs, m1=m1, mk1=mk1: e.tensor_scalar(out=mk1, in0=lfs, scalar1=m1, scalar2=None, op0=ALU.is_ge))
            dv(lambda e, lfs=lfs, lf2=lf2, mk1=mk1: e.scalar_tensor_tensor(out=lf2, in0=mk1, scalar=-1e30, in1=lfs, op0=ALU.mult, op1=ALU.add))
            dv(lambda e, lf2=lf2, m2=m2: e.reduce_max(out=m2, in_=lf2, axis=AX.X))
            dv(lambda e, lf2=lf2, m2=m2, mk2=mk2: e.tensor_scalar(out=mk2, in0=lf2, scalar1=m2, scalar2=None, op0=ALU.is_ge))
            dv(lambda e, d21=d21, m1=m1, m2=m2: e.tensor_tensor(out=d21, in0=m2, in1=m1, op=ALU.subtract))
            ac(lambda e, d21=d21, e2=e2: e.activation(out=e2, in_=d21, func=AF.Exp))
            dv(lambda e, e2=e2, den=den: e.tensor_scalar(out=den, in0=e2, scalar1=1.0, scalar2=None, op0=ALU.add))
            dv(lambda e, den=den: e.reciprocal(out=den, in_=den))
            dv(lambda e, den=den, pg=pg, wa=wa: e.tensor_tensor(out=wa, in0=den, in1=pg, op=ALU.mult))
            dv(lambda e, wa=wa, wb=wb, e2=e2: e.tensor_tensor(out=wb, in0=wa, in1=e2, op=ALU.mult))
            dv(lambda e, gsel=gsel, mk1=mk1, wa=wa: e.tensor_scalar(out=gsel, in0=mk1, scalar1=wa, scalar2=None, op0=ALU.mult))
            dv(lambda e, gsel=gsel, mk2=mk2, wb=wb: e.scalar_tensor_tensor(out=gsel, in0=mk2, scalar=wb, in1=gsel, op0=ALU.mult, op1=ALU.add))
            for g in range(4):
                dv(lambda e, gsel=gsel, ohg=ohg, g=g, s=s: e.tensor_scalar(
                    out=gates[:, s, 4 * g:4 * g + 4], in0=gsel, scalar1=ohg[:, g:g + 1], scalar2=None, op0=ALU.mult),
                    extra_w=[b_gates[s]])

        for ex in range(nexp):
            wi = (t * nexp + ex) * 3
            issue_w(wi + NSLOT)
            sg_, su_, sd_ = wslot[(t, ex, 0)], wslot[(t, ex, 1)], wslot[(t, ex, 2)]
            Wg = ring[sg_][:, :].rearrange("p (kc n) -> p kc n", kc=16)
            Wu = ring[su_][:, :].rearrange("p (kc n) -> p kc n", kc=16)
            Wd = ring[sd_][:, :].rearrange("p (f n) -> p f n", f=4)
            for f in range(4):
                for h in range(NH):
                    gi = gcount % 2
                    gcount += 1
                    for kc in range(16):
                        P.emit("pe", lambda e, kc=kc, f=f, h=h, gi=gi, Wg=Wg: e.matmul(
                            out=psG[gi][:, :], lhsT=Wg[:, kc, f * 128:(f + 1) * 128],
                            rhs=hT[:, kc, h * 512:(h + 1) * 512], start=(kc == 0), stop=(kc == 15)),
                            reads=[b_ring[sg_]] + b_hT[h * 4:(h + 1) * 4], writes=[b_psG[gi]])
                    for kc in range(16):
                        P.emit("pe", lambda e, kc=kc, f=f, h=h, gi=gi, Wu=Wu: e.matmul(
                            out=psU[gi][:, :], lhsT=Wu[:, kc, f * 128:(f + 1) * 128],
                            rhs=hT[:, kc, h * 512:(h + 1) * 512], start=(kc == 0), stop=(kc == 15)),
                            reads=[b_ring[su_]] + b_hT[h * 4:(h + 1) * 4], writes=[b_psU[gi]])
                    P.emit("act", lambda e, gi=gi: e.activation(out=sg[gi][:], in_=psG[gi][:], func=AF.Silu),
                           reads=[b_psG[gi]], writes=[b_sg[gi]])
                    P.emit("dve", lambda e, gi=gi, f=f, h=h: e.tensor_tensor(
                        out=he[:, f, h * 512:(h + 1) * 512], in0=psU[gi][:], in1=sg[gi][:], op=ALU.mult),
                        reads=[b_psU[gi], b_sg[gi]], writes=[b_he])
            for s in range(NS):
                for c in range(4):
                    yi = ycount % 2
                    ycount += 1
                    for f in range(4):
                        P.emit("pe", lambda e, f=f, s=s, c=c, yi=yi, Wd=Wd: e.matmul(
                            out=psY[yi][:, :], lhsT=he[:, f, s * 128:(s + 1) * 128],
                            rhs=Wd[:, f, c * 512:(c + 1) * 512], start=(f == 0), stop=(f == 3)),
                            reads=[b_he, b_ring[sd_]], writes=[b_psY[yi]])
                    P.emit("dve", lambda e, s=s, c=c, yi=yi, ex=ex: e.scalar_tensor_tensor(
                        out=yacc[:, s, c * 512:(c + 1) * 512], in0=psY[yi][:], scalar=gates[:, s, ex:ex + 1],
                        in1=yacc[:, s, c * 512:(c + 1) * 512], op0=ALU.mult, op1=ALU.add),
                        reads=[b_psY[yi], b_gates[s], b_yacc[s]], writes=[b_yacc[s]])
        for s in range(NS):
            r0 = t * TT + s * 128
            if g_final is not None:
                rmsnorm_rows(P, yacc[:, s, :], yacc[:, s, :], gfin[:], b_yacc[s], b_yacc[s], b_gf,
                             (hn[:], ssq[:, s:s + 1], rstd[:, s:s + 1], b_hn, b_ssq[s], b_rstd[s]), "f")
            P.out_ops.append(P.dma("sp", xout[r0:r0 + 128, :], yacc[:, s, :], k_x[s],
                                   reads=[b_yacc[s]], writes=[P.dram_buf(xout, r0)]))
    st.close()


TS = 2048
NT = 16
GELU_K = 1.5957691216057308


class Pool_:
    def __init__(self, P, tag):
        self.P, self.nc, self.tag = P, P.nc, tag
        self.st = ExitStack()

    def sb(self, shape, dt, name):
        t = self.st.enter_context(self.nc.sbuf_tensor(f"{name}_{self.tag}", list(shape), dt))
        return t, Buf(f"{name}_{self.tag}")

    def ps(self, shape, dt, name):
        t = self.st.enter_context(self.nc.psum_tensor(f"{name}_{self.tag}", list(shape), dt))
        return t, Buf(f"{name}_{self.tag}")

    def close(self):
        self.st.close()


def make_ident(P, ident, b_id):
    P.emit("pool", lambda e: e.memset(ident[:], 1.0), writes=[b_id])
    P.emit("pool", lambda e: e.affine_select(out=ident[:], in_=ident[:], pattern=[[-1, 128]],
                                              compare_op=ALU.is_equal, fill=0.0, base=0,
                                              channel_multiplier=1), reads=[b_id], writes=[b_id])


def build_hT(P, tag, xin, base, hT, b_hT, gbc, b_g, ident, b_id):
    A = Pool_(P, f"h{tag}")
    xt = [A.sb([128, D], F32, f"xt{i}") for i in range(2)]
    hn, b_hn = A.sb([128, D], BF16, "hn")
    ssq, b_ssq = A.sb([128, 2], F32, "ssq")
    rstd, b_rstd = A.sb([128, 2], F32, "rstd")
    psT = [A.ps([128, 8, 128], BF16, f"psT{i}") for i in range(2)]
    kx = [P.key(f"hx_{i}") for i in range(2)]
    for tt in range(NT):
        i = tt % 2
        x_t, b_x = xt[i]
        r0 = base + tt * 128
        P.dma("sp", x_t[:], xin[r0:r0 + 128, :], kx[i], reads=[P.dram_buf(xin, r0)], writes=[b_x])
        rmsnorm_rows(P, x_t[:], hn[:], gbc[:], b_x, b_hn, b_g,
                     (hn[:], ssq[:, 0:1], rstd[:, 0:1], b_hn, b_ssq, b_rstd), "h")
        for half in range(2):
            pt, b_pt = psT[half]
            for j in range(8):
                kc = half * 8 + j
                P.emit("pe", lambda e, pt=pt, j=j, kc=kc: e.transpose(
                    out=pt[:, j, :], in_=hn[:, kc * 128:(kc + 1) * 128], identity=ident[:]),
                    reads=[b_hn, b_id], writes=[b_pt])
            if half == 0:
                P.emit("act", lambda e, pt=pt, tt=tt: e.copy(out=hT[:, 0:8, tt * 128:(tt + 1) * 128], in_=pt[:, :, :]),
                       reads=[b_pt], writes=[b_hT])
            else:
                P.emit("dve", lambda e, pt=pt, tt=tt: e.tensor_copy(out=hT[:, 8:16, tt * 128:(tt + 1) * 128], in_=pt[:, :, :]),
                       reads=[b_pt], writes=[b_hT])
    A.close()


def load_cols(P, dst, src1d, n, key, b):
    P.dma("sp", dst, src1d.rearrange("(n f) -> f n", f=128), key, writes=[b], allow_slow_non_contiguous=True)


def conv4(P, src, b_src, cw, cb, out, b_out, b_c):
    P.emit("dve", lambda e: e.tensor_scalar(out=out, in0=src[:, 3:3 + TS], scalar1=cw[:, 3:4], scalar2=cb,
                                            op0=ALU.mult, op1=ALU.add), reads=[b_src] + list(b_c), writes=[b_out])
    for j in (2, 1, 0):
        P.emit("dve", lambda e, j=j: e.scalar_tensor_tensor(out=out, in0=src[:, j:j + TS], scalar=cw[:, j:j + 1],
                                                          in1=out, op0=ALU.mult, op1=ALU.add),
               reads=[b_src, b_out] + list(b_c), writes=[b_out])


def proj_fm(P, hT, b_hT, wblk, b_w, pss, evac):
    for ch in range(4):
        ps, b_ps = pss[ch % len(pss)]
        for kc in range(16):
            P.emit("pe", lambda e, kc=kc, ch=ch, ps=ps: e.matmul(
                out=ps[:, :], lhsT=wblk[:, kc, :], rhs=hT[:, kc, ch * 512:(ch + 1) * 512],
                start=(kc == 0), stop=(kc == 15)), reads=[b_hT, b_w], writes=[b_ps])
        evac(ch, ps, b_ps)


def outproj_phase(P, tag, xin, xout, base, yscr, Wlist, combine):
    A = Pool_(P, f"o{tag}")
    nW = len(Wlist)
    Wsb = [A.sb([128, 16, D], BF16, f"W{i}") for i in range(nW)]
    kW = P.key(f"oW", group=True)
    b_wc = [[Buf(f"W{tag}_{w}_{c}") for c in range(4)] for w in range(nW)]
    for w, ((w_t, b_w), wd) in enumerate(zip(Wsb, Wlist)):
        for c in range(4):
            P.dma("pool", w_t[:, :, c * 512:(c + 1) * 512],
                  wd[:, c * 512:(c + 1) * 512].rearrange("(kc p) n -> p kc n", p=128), kW, writes=[b_wc[w][c]])
    ys = [A.sb([128, 16, 128], BF16, f"ys{i}") for i in range(2)]
    xt = [A.sb([128, D], F32, f"xo{i}") for i in range(2)]
    tmp = [A.sb([128, 512], F32, f"tmp{i}") for i in range(2)]
    pss = [[A.ps([128, 512], F32, f"po{w}_{i}") for i in range(2)] for w in range(nW)]
    ky = [P.key(f"oy_{i}") for i in range(2)]
    kx = [P.key(f"ox_{i}") for i in range(2)]
    cnt = 0
    for tt in range(NT):
        i = tt % 2
        y_t, b_y = ys[i]
        x_t, b_x = xt[i]
        r0 = base + tt * 128
        P.dma("sp", y_t[:], yscr.rearrange("(kc p) t -> p kc t", p=128)[:, :, tt * 128:(tt + 1) * 128], ky[i],
              reads=[P.dram_buf(yscr, 0)], writes=[b_y])
        P.dma("sp", x_t[:], xin[r0:r0 + 128, :], kx[i], reads=[P.dram_buf(xin, r0)], writes=[b_x])
        for c in range(4):
            cur = []
            for w in range(nW):
                ps, b_ps = pss[w][cnt % 2]
                w_t, b_w = Wsb[w]
                for kc in range(16):
                    P.emit("pe", lambda e, kc=kc, c=c, ps=ps, w_t=w_t, y_t=y_t: e.matmul(
                        out=ps[:, :], lhsT=y_t[:, kc, :], rhs=w_t[:, kc, c * 512:(c + 1) * 512],
                        start=(kc == 0), stop=(kc == 15)), reads=[b_y, b_wc[w][c]], writes=[b_ps])
                cur.append((ps, b_ps))
            combine(P, cur, x_t, b_x, c, tmp[cnt % 2])
            cnt += 1
        P.out_ops.append(P.dma("sp", xout[r0:r0 + 128, :], x_t[:], kx[i], reads=[b_x],
                               writes=[P.dram_buf(xout, r0)]))
    A.close()


LN16 = 2.772588722239781
STOP = [0]


def mixer_a_stage(P, xin, xout, NSEQ, W, yscr_all, GELU_NATIVE=False):
    nc = P.nc
    u = P.uid()
    w_in = W["w_in"]
    def seq_body(sq):
        tag = f"a{u}s{sq}"
        base = sq * TS
        yscr = yscr_all[sq]
        O = Pool_(P, f"O{tag}")
        hT, b_hT = O.sb([128, 16, TS], BF16, "hT")
        ident, b_id = O.sb([128, 128], BF16, "ident")
        Gp = Pool_(P, f"G{tag}")
        gbc, b_g = Gp.sb([128, D], F32, "gbc")
        kc_ = P.key(f"c", group=True)
        kcp = P.key(f"cp", group=True)
        P.dma("sp", gbc[:], W["norm"].partition_broadcast(128), P.key(f"g"), writes=[b_g])
        make_ident(P, ident, b_id)
        build_hT(P, tag, xin, base, hT, b_hT, gbc, b_g, ident, b_id)
        barrier(P)
        Gp.close()
        if STOP[0] == 1:
            O.close()
            return
        wq = [O.sb([128, 16, 128], BF16, f"wq{i}") for i in range(3)]
        kwq = [P.key(f"wq_{i}") for i in range(3)]
        wqc = {"n": 0}

        def load_wblk(c0, width=128):
            i = wqc["n"] % 3
            wqc["n"] += 1
            t, b = wq[i]
            P.dma("pool", t[:, :, 0:width], w_in[:, c0:c0 + width].rearrange("(kc p) n -> p kc n", p=128),
                  kwq[i], writes=[b])
            return t, b

        def lru_phase():
            L = Pool_(P, f"L{tag}")
            sets = []
            for i_ in range(2):
                sets.append(L.sb([128, TS + 4], F32, f"xpad{i_}") + L.sb([128, TS], F32, f"xc{i_}") + L.sb([128, TS], BF16, f"xcb{i_}")
                            + L.sb([128, TS], F32, f"t1_{i_}") + L.sb([128, TS], F32, f"t2_{i_}"))
            t3, b_t3 = L.sb([128, TS], F32, "t3")
            hh, b_hh = L.sb([128, TS], F32, "hh")
            gz, b_gz = L.sb([128, TS], F32, "gz")
            yb, b_yb = L.sb([128, TS], BF16, "yb")
            cw, b_cw = L.sb([128, 8, 4], F32, "cw")
            cb, b_cb = L.sb([128, 8], F32, "cb")
            ba, b_ba = L.sb([128, 8], F32, "ba")
            bx, b_bx = L.sb([128, 8], F32, "bx")
            lam, b_lam = L.sb([128, 8], F32, "lam")
            cneg, b_cneg = L.sb([128, 8], F32, "cneg")
            cneg2, b_cneg2 = L.sb([128, 8], F32, "cneg2")
            waT, b_wa = L.sb([128, 8, 128], BF16, "waT")
            wxT, b_wx = L.sb([128, 8, 128], BF16, "wxT")
            pss = [L.ps([128, 512], F32, f"pl{i}") for i in range(4)]
            kyb = P.key(f"yb")
            b_cwj = [Buf(f"cwj{j}") for j in range(4)]
            for j in range(4):
                P.dma("sp", cw[:, :, j], W["lru_conv_w"][j, :].rearrange("(n f) -> f n", f=128), kc_,
                      writes=[b_cwj[j]], allow_slow_non_contiguous=True)
            load_cols(P, cb[:], W["lru_conv_b"], 8, kc_, b_cb)
            load_cols(P, ba[:], W["lru_b_a"], 8, kc_, b_ba)
            load_cols(P, bx[:], W["lru_b_x"], 8, kc_, b_bx)
            load_cols(P, lam[:], W["lru_lam"], 8, kc_, b_lam)
            P.dma("pool", waT[:], W["lru_w_a"].rearrange("n i j -> i n j"), kcp, writes=[b_wa])
            P.dma("pool", wxT[:], W["lru_w_x"].rearrange("n i j -> i n j"), kcp, writes=[b_wx])
            P.emit("act", lambda e: e.activation(out=cneg[:], in_=lam[:], func=AF.Exp, scale=-1.0), reads=[b_lam], writes=[b_cneg])
            P.emit("act", lambda e: e.activation(out=cneg[:], in_=cneg[:], func=AF.Ln, bias=1.0, scale=1.0), reads=[b_cneg], writes=[b_cneg])
            P.emit("dve", lambda e: e.tensor_scalar(out=cneg2[:], in0=cneg[:], scalar1=-16.0, scalar2=None, op0=ALU.mult), reads=[b_cneg], writes=[b_cneg2])
            P.emit("dve", lambda e: e.tensor_scalar(out=cneg[:], in0=cneg[:], scalar1=-8.0, scalar2=None, op0=ALU.mult), reads=[b_cneg, b_cneg2], writes=[b_cneg])
            for st_ in sets:
                P.emit("pool", lambda e, xp=st_[0]: e.memset(xp[:, 0:3], 0.0), writes=[st_[1]])

            def lru_front(n, xpad, b_xpad, xc, b_xc, xcb, b_xcb, t1, b_t1, t2, b_t2):
                wx_t, b_wxb = load_wblk(n * 128)

                def ev_x(ch, ps, b_ps):
                    P.emit("act", lambda e, ch=ch, ps=ps: e.copy(out=xpad[:, 3 + ch * 512:3 + (ch + 1) * 512], in_=ps[:, :]),
                           reads=[b_ps], writes=[b_xpad])
                proj_fm(P, hT, b_hT, wx_t, b_wxb, pss[0:2], ev_x)
                conv4(P, xpad, b_xpad, cw[:, n, :], cb[:, n:n + 1], xc[:], b_xc, b_cwj + [b_cb])
                P.emit("pool", lambda e: e.tensor_copy(out=xcb[:], in_=xc[:]), reads=[b_xc], writes=[b_xcb])
                for ch in range(4):
                    pr, b_pr = pss[ch % 2]
                    pi, b_pi = pss[2 + ch % 2]
                    P.emit("pe", lambda e, n=n, ch=ch, pr=pr: e.matmul(out=pr[:, :], lhsT=waT[:, n, :], rhs=xcb[:, ch * 512:(ch + 1) * 512],
                                                                        start=True, stop=True), reads=[b_wa, b_xcb], writes=[b_pr])
                    P.emit("pe", lambda e, n=n, ch=ch, pi=pi: e.matmul(out=pi[:, :], lhsT=wxT[:, n, :], rhs=xcb[:, ch * 512:(ch + 1) * 512],
                                                                        start=True, stop=True), reads=[b_wx, b_xcb], writes=[b_pi])
                    P.emit("act", lambda e, n=n, ch=ch, pr=pr: e.activation(out=t1[:, ch * 512:(ch + 1) * 512], in_=pr[:, :], func=AF.Sigmoid,
                                                                             bias=ba[:, n:n + 1], scale=1.0), reads=[b_pr, b_ba], writes=[b_t1])
                    P.emit("act", lambda e, n=n, ch=ch, pi=pi: e.activation(out=t2[:, ch * 512:(ch + 1) * 512], in_=pi[:, :], func=AF.Sigmoid,
                                                                             bias=bx[:, n:n + 1], scale=1.0), reads=[b_pi, b_bx], writes=[b_t2])

            def lru_back(n, xpad, b_xpad, xc, b_xc, xcb, b_xcb, t1, b_t1, t2, b_t2):
                wz_t, b_wzb = load_wblk(1024 + n * 128)
                P.emit("act", lambda e, n=n: e.activation(out=t3[:], in_=t1[:], func=AF.Exp, scale=cneg2[:, n:n + 1]), reads=[b_t1, b_cneg2], writes=[b_t3])
                P.emit("act", lambda e, n=n: e.activation(out=t1[:], in_=t1[:], func=AF.Exp, scale=cneg[:, n:n + 1]), reads=[b_t1, b_cneg, b_t3], writes=[b_t1])
                P.emit("act", lambda e: e.activation(out=t3[:], in_=t3[:], func=AF.Sqrt, bias=1.0, scale=-1.0), reads=[b_t3], writes=[b_t3])
                P.emit("dve", lambda e: e.tensor_tensor(out=t2[:], in0=t2[:], in1=xc[:], op=ALU.mult), reads=[b_t2, b_xc], writes=[b_t2])
                P.emit("dve", lambda e: e.tensor_tensor(out=t2[:], in0=t2[:], in1=t3[:], op=ALU.mult), reads=[b_t2, b_t3], writes=[b_t2])
                P.emit("dve", lambda e: e.tensor_tensor_scan(out=hh[:], data0=t1[:], data1=t2[:], initial=0.0, op0=ALU.mult, op1=ALU.add),
                       reads=[b_t1, b_t2], writes=[b_hh])

                def ev_z(ch, ps, b_ps):
                    sl = slice(ch * 512, (ch + 1) * 512)
                    if GELU_NATIVE:
                        P.emit("act", lambda e, ps=ps, sl=sl: e.activation(out=gz[:, sl], in_=ps[:, :], func=AF.Gelu_apprx_tanh),
                               reads=[b_ps], writes=[b_gz])
                    else:
                        P.emit("act", lambda e, ps=ps, sl=sl: e.activation(out=gz[:, sl], in_=ps[:, :], func=AF.Square), reads=[b_ps], writes=[b_gz])
                        P.emit("dve", lambda e, sl=sl: e.tensor_scalar(out=gz[:, sl], in0=gz[:, sl], scalar1=0.044715, scalar2=1.0, op0=ALU.mult, op1=ALU.add),
                               reads=[b_gz], writes=[b_gz])
                        P.emit("dve", lambda e, ps=ps, sl=sl: e.tensor_tensor(out=gz[:, sl], in0=ps[:, :], in1=gz[:, sl], op=ALU.mult),
                               reads=[b_gz, b_ps], writes=[b_gz])
                        P.emit("act", lambda e, sl=sl: e.activation(out=gz[:, sl], in_=gz[:, sl], func=AF.Sigmoid, scale=GELU_K), reads=[b_gz], writes=[b_gz])
                        P.emit("dve", lambda e, ps=ps, sl=sl: e.tensor_tensor(out=gz[:, sl], in0=ps[:, :], in1=gz[:, sl], op=ALU.mult),
                               reads=[b_gz, b_ps], writes=[b_gz])
                    P.emit("dve", lambda e, sl=sl: e.tensor_tensor(out=yb[:, sl], in0=hh[:, sl], in1=gz[:, sl], op=ALU.mult),
                           reads=[b_gz, b_hh], writes=[b_yb])
                proj_fm(P, hT, b_hT, wz_t, b_wzb, pss[2:4], ev_z)
                P.dma("sp", yscr[n * 128:(n + 1) * 128, :], yb[:], kyb, reads=[b_yb], writes=[P.dram_buf(yscr, 0)])
            lru_front(0, *sets[0])
            for n in range(8):
                if n + 1 < 8:
                    lru_front(n + 1, *sets[(n + 1) % 2])
                lru_back(n, *sets[n % 2])
            barrier(P)
            L.close()

        lru_phase()
        if STOP[0] == 2:
            O.close()
            return
        def mlstm_phase():
            M = Pool_(P, f"M{tag}")
            xpad, b_xpad = M.sb([128, TS + 4], F32, "xpad")
            xc, b_xc = M.sb([128, TS], F32, "xc")
            qT, b_qT = M.sb([128, 2, TS], BF16, "qT")
            kT, b_kT = M.sb([128, 2, TS], BF16, "kT")
            vext, b_v = M.sb([128, NT, 258], BF16, "vext")
            og, b_og = M.sb([128, NT, 256], BF16, "og")
            Fb, b_Fb = M.sb([128, TS], F32, "Fb")
            iT, b_iT = M.sb([4, TS], F32, "iT")
            fT, b_fT = M.sb([4, TS], F32, "fT")
            ones4, b_ones4 = M.sb([4, 1], F32, "ones4")
            biasT, b_bT = M.sb([128, NT, 4], F32, "biasT")
            wgi, b_wgi = M.sb([128, 16, 4], BF16, "wgi")
            wgf, b_wgf = M.sb([128, 16, 4], BF16, "wgf")
            ibias, b_ib = M.sb([4, 1], F32, "ibias")
            fbias, b_fb = M.sb([4, 1], F32, "fbias")
            selm, b_sel = M.sb([4, 4, 128], F32, "selm")
            id4, b_id4 = M.sb([4, 4], F32, "id4")
            tri, b_tri = M.sb([128, 128], F32, "tri")
            mcw, b_mcw = M.sb([128, 16, 4], F32, "mcw")
            mcb, b_mcb = M.sb([128, 16], F32, "mcb")
            mg, b_mg = M.sb([128, 1024], F32, "mg")
            dT = [M.sb([128, 512], F32, f"dT{i}") for i in range(2)]
            pT = [M.sb([128, 512], BF16, f"pT{i}") for i in range(2)]
            accS, _ = M.sb([128, 4, 258], F32, "accS")
            b_accS = [Buf(f"accS{q}") for q in range(4)]
            hm, b_hm = M.sb([128, 256], F32, "hm")
            ybh, b_ybh = M.sb([128, 256], BF16, "ybh")
            yTh, b_yTh = M.sb([128, 2, TS], BF16, "yTh")
            sml, b_sml = M.sb([128, 8], F32, "sml")
            epsc, b_epsc = M.sb([128, 1], F32, "epsc")
            P.emit("pool", lambda e: e.memset(epsc[:], EPS), writes=[b_epsc])
            wv = [M.sb([128, 16, 256], BF16, f"wv{i}") for i in range(2)]
            kwv = [P.key(f"wv_{i}") for i in range(2)]
            kyT = P.key(f"yT")
            kc2 = P.key(f"c2", group=True)
            kcp2 = P.key(f"cp2", group=True)
            psS = [M.ps([128, 512], F32, f"pS{i}") for i in range(2)]
            psA = [M.ps([128, 512], F32, f"pA{i}") for i in range(4)]
            psP = [M.ps([128, 512], F32, f"pP{i}") for i in range(2)]
            psB, b_psB = psP[0]
            psTt = psP[1][0][:, :].bitcast(BF16).rearrange("p (a b) -> p a b", a=8)
            b_psTt = psP[1][1]

            b_mcwj = [Buf(f"mcwj{j}") for j in range(4)]
            for j in range(4):
                P.dma("sp", mcw[:, :, j], W["m_conv_w"][j, :].rearrange("(n f) -> f n", f=128), kc2,
                      writes=[b_mcwj[j]], allow_slow_non_contiguous=True)
            load_cols(P, mcb[:], W["m_conv_b"], 16, kc2, b_mcb)
            P.dma("sp", mg[:], W["m_head_g"].partition_broadcast(128), kc2, writes=[b_mg])
            P.dma("sp", ibias[:], W["m_i_bias"].rearrange("o h -> h o"), kc2, writes=[b_ib], allow_slow_non_contiguous=True)
            P.dma("sp", fbias[:], W["m_f_bias"].rearrange("o h -> h o"), kc2, writes=[b_fb], allow_slow_non_contiguous=True)
            P.dma("pool", wgi[:], w_in[:, 6144:6148].rearrange("(kc p) n -> p kc n", p=128), kcp2, writes=[b_wgi])
            P.dma("pool", wgf[:], w_in[:, 6148:6152].rearrange("(kc p) n -> p kc n", p=128), kcp2, writes=[b_wgf])
            P.emit("pool", lambda e: e.memset(xpad[:, 0:3], 0.0), writes=[b_xpad])
            P.emit("pool", lambda e: e.memset(ones4[:], 1.0), writes=[b_ones4])
            P.emit("pool", lambda e: e.memset(vext[:, :, 256:258], 1.0), writes=[b_v])
            P.emit("pool", lambda e: e.memset(id4[:], 1.0), writes=[b_id4])
            P.emit("pool", lambda e: e.affine_select(out=id4[:], in_=id4[:], pattern=[[-1, 4]], compare_op=ALU.is_equal, fill=0.0,
                                                      base=0, channel_multiplier=1), reads=[b_id4], writes=[b_id4])
            P.emit("pool", lambda e: e.memset(selm[:], -1.0), writes=[b_sel])
            P.emit("pool", lambda e: e.affine_select(out=selm[:], in_=selm[:], pattern=[[-1, 4], [0, 128]], compare_op=ALU.is_equal, fill=0.0,
                                                      base=0, channel_multiplier=1), reads=[b_sel], writes=[b_sel])
            P.emit("pool", lambda e: e.memset(tri[:], 1.0), writes=[b_tri])
            P.emit("pool", lambda e: e.affine_select(out=tri[:], in_=tri[:], pattern=[[1, 128]], compare_op=ALU.is_ge, fill=0.0,
                                                      base=0, channel_multiplier=-1), reads=[b_tri], writes=[b_tri])
            for ch in range(4):
                sl = slice(ch * 512, (ch + 1) * 512)
                pi_, b_pi = psP[0]
                pf_, b_pf = psP[1]
                for kc in range(16):
                    P.emit("pe", lambda e, kc=kc, sl=sl, pi_=pi_: e.matmul(out=pi_[0:4, :], lhsT=wgi[:, kc, :], rhs=hT[:, kc, sl],
                                                                            start=(kc == 0), stop=(kc == 15)), reads=[b_hT, b_wgi], writes=[b_pi])
                for kc in range(16):
                    P.emit("pe", lambda e, kc=kc, sl=sl, pf_=pf_: e.matmul(out=pf_[0:4, :], lhsT=wgf[:, kc, :], rhs=hT[:, kc, sl],
                                                                            start=(kc == 0), stop=(kc == 15)), reads=[b_hT, b_wgf], writes=[b_pf])
                P.emit("act", lambda e, sl=sl, pi_=pi_: e.activation(out=iT[:, sl], in_=pi_[0:4, :], func=AF.Identity, bias=ibias[:, 0:1], scale=1.0),
                       reads=[b_pi, b_ib], writes=[b_iT])
                P.emit("act", lambda e, sl=sl, pf_=pf_: e.activation(out=fT[:, sl], in_=pf_[0:4, :], func=AF.Identity, bias=fbias[:, 0:1], scale=1.0),
                       reads=[b_pf, b_fb], writes=[b_fT])
            P.emit("act", lambda e: e.activation(out=fT[:], in_=fT[:], func=AF.Exp, scale=-1.0), reads=[b_fT], writes=[b_fT])
            P.emit("act", lambda e: e.activation(out=fT[:], in_=fT[:], func=AF.Ln, bias=1.0, scale=1.0), reads=[b_fT], writes=[b_fT])
            P.emit("dve", lambda e: e.tensor_tensor_scan(out=fT[:], data0=ones4[:, 0:1].to_broadcast([4, TS]), data1=fT[:], initial=0.0, op0=ALU.mult, op1=ALU.add),
                   reads=[b_fT, b_ones4], writes=[b_fT])
            P.emit("dve", lambda e: e.tensor_tensor(out=iT[:], in0=iT[:], in1=fT[:], op=ALU.add), reads=[b_iT, b_fT], writes=[b_iT])
            for jt in range(NT):
                P.emit("pe", lambda e, jt=jt: e.matmul(out=psB[:, jt * 4:(jt + 1) * 4], lhsT=iT[:, jt * 128:(jt + 1) * 128], rhs=id4[:],
                                                        start=True, stop=True), reads=[b_iT, b_id4], writes=[b_psB])
            P.emit("dve", lambda e: e.tensor_scalar(out=biasT[:].rearrange("p a b -> p (a b)"), in0=psB[:, 0:64], scalar1=-LN16, scalar2=None, op0=ALU.add),
                   reads=[b_psB], writes=[b_bT])
            wvc = {"n": 0}

            def load_wv(c0):
                i = wvc["n"] % 2
                wvc["n"] += 1
                t, b = wv[i]
                P.dma("pool", t[:], w_in[:, c0:c0 + 256].rearrange("(kc p) n -> p kc n", p=128), kwv[i], writes=[b])
                return t, b

            blk = 0
            for hd in range(4):
                for (dstT, b_dst, c0, t0) in ((qT, b_qT, 2048, 0), (kT, b_kT, 3072, 8)):
                    for c in range(2):
                        wt, b_wt = load_wblk(c0 + hd * 256 + c * 128)

                        def ev_q(ch, ps, b_ps):
                            P.emit("act", lambda e, ch=ch, ps=ps: e.copy(out=xpad[:, 3 + ch * 512:3 + (ch + 1) * 512], in_=ps[:, :]),
                                   reads=[b_ps], writes=[b_xpad])
                        proj_fm(P, hT, b_hT, wt, b_wt, psP, ev_q)
                        tcol = t0 + hd * 2 + c
                        conv4(P, xpad, b_xpad, mcw[:, tcol, :], mcb[:, tcol:tcol + 1], xc[:], b_xc, b_mcwj + [b_mcb])
                        P.emit("act", lambda e, dstT=dstT, c=c: e.activation(out=dstT[:, c, :], in_=xc[:], func=AF.Silu),
                               reads=[b_xc], writes=[b_dst])
                wv_t, b_wvt = load_wv(4096 + hd * 256)
                wo_t, b_wot = load_wv(5120 + hd * 256)
                for tt in range(NT):
                    for (w_t, b_w, isv) in ((wv_t, b_wvt, True), (wo_t, b_wot, False)):
                        ps, b_ps = psP[blk % 2]
                        blk += 1
                        for kc in range(16):
                            P.emit("pe", lambda e, kc=kc, tt=tt, ps=ps, w_t=w_t: e.matmul(
                                out=ps[:, 0:256], lhsT=hT[:, kc, tt * 128:(tt + 1) * 128], rhs=w_t[:, kc, :],
                                start=(kc == 0), stop=(kc == 15)), reads=[b_hT, b_w], writes=[b_ps])
                        if isv:
                            P.emit("dve", lambda e, tt=tt, ps=ps: e.tensor_copy(out=vext[:, tt, 0:256], in_=ps[:, 0:256]),
                                   reads=[b_ps], writes=[b_v])
                        else:
                            P.emit("act", lambda e, tt=tt, ps=ps: e.activation(out=og[:, tt, :], in_=ps[:, 0:256], func=AF.Sigmoid),
                                   reads=[b_ps], writes=[b_og])
                for ch in range(4):
                    ps, b_ps = psP[blk % 2]
                    blk += 1
                    P.emit("pe", lambda e, ch=ch, ps=ps, hd=hd: e.matmul(out=ps[:, :], lhsT=selm[:, hd, :], rhs=fT[:, ch * 512:(ch + 1) * 512],
                                                                          start=True, stop=True), reads=[b_sel, b_fT], writes=[b_ps])
                    P.emit("act", lambda e, ch=ch, ps=ps: e.copy(out=Fb[:, ch * 512:(ch + 1) * 512], in_=ps[:, :]), reads=[b_ps], writes=[b_Fb])
                def epilogue(tt, acc, b_acc, tsl):
                    dd, rec, ssq, rstd = sml[:, 0:1], sml[:, 1:2], sml[:, 2:3], sml[:, 3:4]
                    P.emit("dve", lambda e, acc=acc, dd=dd: e.tensor_scalar(out=dd, in0=acc[:, 256:257], scalar1=-1.0, scalar2=None, op0=ALU.mult),
                           reads=[b_acc], writes=[b_sml])
                    P.emit("dve", lambda e, acc=acc, dd=dd: e.tensor_tensor(out=dd, in0=dd, in1=acc[:, 256:257], op=ALU.max),
                           reads=[b_acc, b_sml], writes=[b_sml])
                    P.emit("dve", lambda e, dd=dd: e.tensor_scalar(out=dd, in0=dd, scalar1=1.0, scalar2=None, op0=ALU.max),
                           reads=[b_sml], writes=[b_sml])
                    P.emit("dve", lambda e, dd=dd, rec=rec: e.reciprocal(out=rec, in_=dd), reads=[b_sml], writes=[b_sml])
                    P.emit("dve", lambda e, acc=acc, rec=rec, tt=tt: e.scalar_tensor_tensor(out=hm[:], in0=acc[:, 0:256], scalar=rec, in1=og[:, tt, :],
                                                                                             op0=ALU.mult, op1=ALU.mult),
                           reads=[b_acc, b_sml, b_og], writes=[b_hm])
                    P.emit("dve", lambda e, ssq=ssq: e.scalar_tensor_tensor(out=ybh[:], in0=hm[:], scalar=1.0, in1=hm[:], op0=ALU.mult, op1=ALU.mult, accum_out=ssq),
                           reads=[b_hm], writes=[b_ybh, b_sml])
                    P.emit("act", lambda e, ssq=ssq, rstd=rstd: e.activation(out=rstd, in_=ssq, func=AF.Ln, bias=epsc[:, 0:1], scale=1.0 / 256), reads=[b_sml, b_epsc], writes=[b_sml])
                    P.emit("act", lambda e, rstd=rstd: e.activation(out=rstd, in_=rstd, func=AF.Exp, scale=-0.5), reads=[b_sml], writes=[b_sml])
                    P.emit("dve", lambda e, rstd=rstd, hd=hd: e.scalar_tensor_tensor(out=ybh[:], in0=hm[:], scalar=rstd, in1=mg[:, hd * 256:(hd + 1) * 256],
                                                                                      op0=ALU.mult, op1=ALU.mult),
                           reads=[b_hm, b_sml, b_mg], writes=[b_ybh])
                    for c in range(2):
                        P.emit("pe", lambda e, c=c: e.transpose(out=psTt[:, c, :], in_=ybh[:, c * 128:(c + 1) * 128], identity=ident[:]),
                               reads=[b_ybh, b_id], writes=[b_psTt])
                    P.emit("act", lambda e, tsl=tsl: e.copy(out=yTh[:, :, tsl], in_=psTt[:, 0:2, :]), reads=[b_psTt], writes=[b_yTh])
                cntS = 0
                pend = []
                for qg in range(4):
                    for jt in range(4 * qg + 4):
                        c0 = max(0, jt - 4 * qg)
                        ncol = (4 - c0) * 128
                        t0 = (4 * qg + c0) * 128
                        tsl = slice(t0, t0 + ncol)
                        jsl = slice(jt * 128, (jt + 1) * 128)
                        i2 = cntS % 2
                        cntS += 1
                        pS, b_pS = psS[i2]
                        d_t, b_d = dT[i2]
                        p_t, b_p = pT[i2]
                        for c in range(2):
                            P.emit("pe", lambda e, c=c, pS=pS, jsl=jsl, tsl=tsl, ncol=ncol: e.matmul(out=pS[:, 0:ncol], lhsT=kT[:, c, jsl], rhs=qT[:, c, tsl],
                                                                                                     start=(c == 0), stop=(c == 1)),
                                   reads=[b_kT, b_qT], writes=[b_pS])
                        P.emit("act", lambda e, d_t=d_t, tsl=tsl, jt=jt, hd=hd, ncol=ncol: e.activation(out=d_t[:, 0:ncol], in_=Fb[:, tsl], func=AF.Exp,
                                                                                                        bias=biasT[:, jt, hd:hd + 1], scale=1.0),
                               reads=[b_Fb, b_bT], writes=[b_d])
                        if jt >= 4 * qg:
                            P.emit("pool", lambda e, d_t=d_t: e.tensor_tensor(out=d_t[:, 0:128], in0=d_t[:, 0:128], in1=tri[:], op=ALU.mult),
                                   reads=[b_d, b_tri], writes=[b_d])
                        P.emit("dve", lambda e, d_t=d_t, p_t=p_t, pS=pS, ncol=ncol: e.tensor_tensor(out=p_t[:, 0:ncol], in0=pS[:, 0:ncol], in1=d_t[:, 0:ncol], op=ALU.mult),
                               reads=[b_pS, b_d], writes=[b_p])
                        for tq in range(c0, 4):
                            acc, b_acc = psA[tq]
                            tt = 4 * qg + tq
                            P.emit("pe", lambda e, p_t=p_t, jt=jt, acc=acc, tt=tt, tq=tq, c0=c0: e.matmul(
                                out=acc[:, 0:258], lhsT=p_t[:, (tq - c0) * 128:(tq - c0 + 1) * 128], rhs=vext[:, jt, :],
                                start=(jt == 0), stop=(jt == tt)), reads=[b_p, b_v], writes=[b_acc])
                        if pend:
                            epilogue(*pend.pop(0))
                    for tq in range(4):
                        acc, b_acc = psA[tq]
                        P.emit("act", lambda e, acc=acc, tq=tq: e.copy(out=accS[:, tq, :], in_=acc[:, 0:258]), reads=[b_acc], writes=[b_accS[tq]])
                    for tq in range(4):
                        tt = 4 * qg + tq
                        pend.append((tt, accS[:, tq, :], b_accS[tq], slice(tt * 128, (tt + 1) * 128)))
                    if qg == 3:
                        while pend:
                            epilogue(*pend.pop(0))
                for c in range(2):
                    r0 = 1024 + hd * 256 + c * 128
                    P.dma("sp", yscr[r0:r0 + 128, :], yTh[:, c, :], kyT, reads=[b_yTh], writes=[P.dram_buf(yscr, 0)])
            barrier(P)
            M.close()

        if STOP[0] != 3:
            mlstm_phase()
        O.close()
        if STOP[0] == 4:
            return
        def comb(P, cur, x_t, b_x, c, tmp):
            ps, b_ps = cur[0]
            P.emit("dve", lambda e, ps=ps, c=c, x_t=x_t: e.tensor_tensor(out=x_t[:, c * 512:(c + 1) * 512], in0=ps[:, :],
                                                                          in1=x_t[:, c * 512:(c + 1) * 512], op=ALU.add),
                   reads=[b_ps, b_x], writes=[b_x])
        outproj_phase(P, tag, xin, xout, base, yscr, [W["w_out"]], comb)
        barrier(P)

    for sq in range(NSEQ):
        seq_body(sq)


PI = 3.141592653589793
MAGIC = 12582912.0


def s5_stage(P, xin, xout, NSEQ, W, yscr_all):
    nc = P.nc
    u = P.uid()
    w_in = W["w_in"]

    def seq_body(sq):
        tag = f"s{u}q{sq}"
        base = sq * TS
        yscr = yscr_all[sq]
        O = Pool_(P, f"O{tag}")
        hT, b_hT = O.sb([128, 16, TS], BF16, "hT")
        ident, b_id = O.sb([128, 128], BF16, "ident")
        Gp = Pool_(P, f"G{tag}")
        gbc, b_g = Gp.sb([128, D], F32, "gbc")
        P.dma("sp", gbc[:], W["norm"].partition_broadcast(128), P.key(f"g"), writes=[b_g])
        make_ident(P, ident, b_id)
        build_hT(P, tag, xin, base, hT, b_hT, gbc, b_g, ident, b_id)
        barrier(P)
        Gp.close()

        def main_phase():
            M = Pool_(P, f"M{tag}")
            wq = [M.sb([128, 16, 128], BF16, f"wq{i}") for i in range(3)]
            kwq = [P.key(f"wq_{i}") for i in range(3)]
            rhoB, b_rhoB = M.sb([128, 64], F32, "rhoB")
            thB, b_thB = M.sb([128, 64], F32, "thB")
            bbr, b_bbr = M.sb([128, 64, 16], F32, "bbr")
            bbi, b_bbi = M.sb([128, 64, 16], F32, "bbi")
            dsk, b_dsk = M.sb([128, 16], F32, "dsk")
            iota, b_iota = M.sb([128, TS], F32, "iota")
            kc_ = P.key(f"c", group=True)
            psA = [M.ps([128, 512], F32, f"pa{i}") for i in range(4)]
            psY = [M.ps([128, 512], F32, f"py{i}") for i in range(4)]
            psTb = psA[3][0][:, :].bitcast(BF16).rearrange("p (a b) -> p a b", a=8)
            b_psTb = psA[3][1]

            def setup():
                S = Pool_(P, f"S{tag}")
                are, b_are = S.sb([128, 64], F32, "are")
                aim, b_aim = S.sb([128, 64], F32, "aim")
                ls, b_ls = S.sb([128, 1], F32, "ls")
                rho, b_rho = S.sb([128, 64], F32, "rho")
                th, b_th = S.sb([128, 64], F32, "th")
                tmp, b_tmp = S.sb([128, 64], F32, "tmp")
                sn, b_sn = S.sb([128, 64], F32, "sn")
                cs, b_cs = S.sb([128, 64], F32, "cs")
                den, b_den = S.sb([128, 64], F32, "den")
                fr, b_fr = S.sb([128, 64], F32, "fr")
                fi, b_fi = S.sb([128, 64], F32, "fi")
                frB, b_frB = S.sb([128, 64], F32, "frB")
                fiB, b_fiB = S.sb([128, 64], F32, "fiB")
                E0, b_E0 = S.sb([128, 64], F32, "E0")
                E1, b_E1 = S.sb([128, 64], F32, "E1")
                bre, b_bre = S.sb([128, 64, 16], F32, "bre")
                bim, b_bim = S.sb([128, 64, 16], F32, "bim")
                t16, b_t16 = S.sb([128, 64, 16], F32, "t16")
                P.dma("sp", are[:], W["a_re"], kc_, writes=[b_are])
                P.dma("sp", aim[:], W["a_im"], kc_, writes=[b_aim])
                P.dma("sp", ls[:], W["log_step"].rearrange("(g o) -> g o", o=1), kc_, writes=[b_ls])
                b_bre2, b_bim2 = Buf("bre2"), Buf("bim2")
                for two in range(2):
                    P.dma("sp", bre[two * 64:(two + 1) * 64, :, :], W["b_re"].rearrange("(gp two) p c -> two p gp c", two=2)[two], kc_,
                          writes=[(b_bre, b_bre2)[two]])
                    P.dma("sp", bim[two * 64:(two + 1) * 64, :, :], W["b_im"].rearrange("(gp two) p c -> two p gp c", two=2)[two], kc_,
                          writes=[(b_bim, b_bim2)[two]])
                load_cols(P, dsk[:], W["d"], 16, kc_, b_dsk)
                P.emit("pool", lambda e: e.iota(out=iota[:], pattern=[[1, TS]], base=0, channel_multiplier=0,
                                                allow_small_or_imprecise_dtypes=True), writes=[b_iota])
                for (E, b_E, bs) in ((E0, b_E0, 0), (E1, b_E1, -1)):
                    P.emit("pool", lambda e, E=E: e.memset(E[:], 1.0), writes=[b_E])
                    P.emit("pool", lambda e, E=E, bs=bs: e.affine_select(out=E[:], in_=E[:], pattern=[[-2, 64]], compare_op=ALU.is_equal,
                                                                          fill=0.0, base=bs, channel_multiplier=1), reads=[b_E], writes=[b_E])
                dv = lambda fn, r, w: P.emit("dve", fn, reads=r, writes=w)
                ac = lambda fn, r, w: P.emit("act", fn, reads=r, writes=w)
                ac(lambda e: e.activation(out=ls[:], in_=ls[:], func=AF.Exp), [b_ls], [b_ls])
                dv(lambda e: e.tensor_scalar(out=rho[:], in0=are[:], scalar1=ls[:, 0:1], scalar2=None, op0=ALU.mult), [b_are, b_ls], [b_rho])
                ac(lambda e: e.activation(out=rho[:], in_=rho[:], func=AF.Exp), [b_rho], [b_rho])
                dv(lambda e: e.tensor_scalar(out=th[:], in0=aim[:], scalar1=ls[:, 0:1], scalar2=None, op0=ALU.mult), [b_aim, b_ls], [b_th])
                dv(lambda e: e.tensor_scalar(out=tmp[:], in0=th[:], scalar1=1.0 / (2 * PI), scalar2=MAGIC, op0=ALU.mult, op1=ALU.add), [b_th], [b_tmp])
                dv(lambda e: e.tensor_scalar(out=tmp[:], in0=tmp[:], scalar1=-MAGIC, scalar2=-2 * PI, op0=ALU.add, op1=ALU.mult), [b_tmp], [b_tmp])
                dv(lambda e: e.tensor_tensor(out=tmp[:], in0=tmp[:], in1=th[:], op=ALU.add), [b_tmp, b_th], [b_tmp])
                dv(lambda e: e.tensor_scalar(out=tmp[:], in0=tmp[:], scalar1=-PI, scalar2=PI, op0=ALU.max, op1=ALU.min), [b_tmp], [b_tmp])
                ac(lambda e: e.activation(out=sn[:], in_=tmp[:], func=AF.Sin), [b_tmp], [b_sn])
                ac(lambda e: e.activation(out=cs[:], in_=tmp[:], func=AF.Sin, scale=0.5), [b_tmp], [b_cs])
                dv(lambda e: e.tensor_tensor(out=cs[:], in0=cs[:], in1=cs[:], op=ALU.mult), [b_cs], [b_cs])
                dv(lambda e: e.tensor_scalar(out=cs[:], in0=cs[:], scalar1=-2.0, scalar2=1.0, op0=ALU.mult, op1=ALU.add), [b_cs], [b_cs])
                dv(lambda e: e.tensor_tensor(out=cs[:], in0=cs[:], in1=rho[:], op=ALU.mult), [b_cs, b_rho], [b_cs])
                dv(lambda e: e.tensor_scalar(out=cs[:], in0=cs[:], scalar1=-1.0, scalar2=None, op0=ALU.add), [b_cs], [b_cs])
                dv(lambda e: e.tensor_tensor(out=sn[:], in0=sn[:], in1=rho[:], op=ALU.mult), [b_sn, b_rho], [b_sn])
                dv(lambda e: e.tensor_tensor(out=den[:], in0=are[:], in1=are[:], op=ALU.mult), [b_are], [b_den])
                dv(lambda e: e.tensor_tensor(out=tmp[:], in0=aim[:], in1=aim[:], op=ALU.mult), [b_aim, b_cs], [b_tmp])
                dv(lambda e: e.tensor_tensor(out=den[:], in0=den[:], in1=tmp[:], op=ALU.add), [b_den, b_tmp], [b_den])
                dv(lambda e: e.reciprocal(out=den[:], in_=den[:]), [b_den], [b_den])
                dv(lambda e: e.tensor_tensor(out=fr[:], in0=cs[:], in1=are[:], op=ALU.mult), [b_cs, b_are], [b_fr])
                dv(lambda e: e.tensor_tensor(out=tmp[:], in0=sn[:], in1=aim[:], op=ALU.mult), [b_sn, b_aim, b_den], [b_tmp])
                dv(lambda e: e.tensor_tensor(out=fr[:], in0=fr[:], in1=tmp[:], op=ALU.add), [b_fr, b_tmp], [b_fr])
                dv(lambda e: e.tensor_tensor(out=fr[:], in0=fr[:], in1=den[:], op=ALU.mult), [b_fr, b_den], [b_fr])
                dv(lambda e: e.tensor_tensor(out=fi[:], in0=sn[:], in1=are[:], op=ALU.mult), [b_sn, b_are], [b_fi])
                dv(lambda e: e.tensor_tensor(out=tmp[:], in0=cs[:], in1=aim[:], op=ALU.mult), [b_cs, b_aim, b_fr], [b_tmp])
                dv(lambda e: e.tensor_tensor(out=fi[:], in0=fi[:], in1=tmp[:], op=ALU.subtract), [b_fi, b_tmp], [b_fi])
                dv(lambda e: e.tensor_tensor(out=fi[:], in0=fi[:], in1=den[:], op=ALU.mult), [b_fi, b_den], [b_fi])
                mev, b_mev = S.sb([128, 2], F32, "mev")
                l2, b_l2 = S.sb([128, 128], F32, "l2")
                dv(lambda e: e.reduce_sum(out=mev[:, 0:1], in_=E0[:], axis=AX.X), [b_E0], [b_mev])
                dv(lambda e: e.reduce_sum(out=mev[:, 1:2], in_=E1[:], axis=AX.X), [b_E1, b_mev], [b_mev])
                dv(lambda e: e.tensor_tensor(out=E0[:], in0=E0[:], in1=E1[:], op=ALU.add), [b_E0, b_E1, b_mev], [b_E0])
                for qi, (src, b_src, dst, b_dst) in enumerate(((rho, b_rho, rhoB, b_rhoB), (th, b_th, thB, b_thB),
                                                               (fr, b_fr, frB, b_frB), (fi, b_fi, fiB, b_fiB))):
                    ps, b_ps = psA[qi % 3]
                    for two in range(2):
                        dv(lambda e, src=src, two=two: e.tensor_scalar(out=l2[:, two * 64:(two + 1) * 64], in0=src[:], scalar1=mev[:, two:two + 1],
                                                                       scalar2=None, op0=ALU.mult), [b_src, b_mev, b_l2], [b_l2])
                    P.emit("pe", lambda e, ps=ps: e.matmul(out=ps[:, 0:64], lhsT=l2[:], rhs=E0[:], start=True, stop=True),
                           reads=[b_l2, b_E0], writes=[b_ps])
                    P.emit("act", lambda e, dst=dst, ps=ps: e.copy(out=dst[:], in_=ps[:, 0:64]), reads=[b_ps], writes=[b_dst])
                frb = frB[:].unsqueeze(2).to_broadcast([128, 64, 16])
                fib = fiB[:].unsqueeze(2).to_broadcast([128, 64, 16])
                dv(lambda e: e.tensor_tensor(out=bbr[:], in0=bre[:], in1=frb, op=ALU.mult), [b_bre, b_bre2, b_frB], [b_bbr])
                dv(lambda e: e.tensor_tensor(out=t16[:], in0=bim[:], in1=fib, op=ALU.mult), [b_bim, b_bim2, b_fiB], [b_t16])
                dv(lambda e: e.tensor_tensor(out=bbr[:], in0=bbr[:], in1=t16[:], op=ALU.subtract), [b_bbr, b_t16], [b_bbr])
                dv(lambda e: e.tensor_tensor(out=bbi[:], in0=bim[:], in1=frb, op=ALU.mult), [b_bim, b_bim2, b_frB], [b_bbi])
                dv(lambda e: e.tensor_tensor(out=t16[:], in0=bre[:], in1=fib, op=ALU.mult), [b_bre, b_bre2, b_fiB, b_bbr], [b_t16])
                dv(lambda e: e.tensor_tensor(out=bbi[:], in0=bbi[:], in1=t16[:], op=ALU.add), [b_bbi, b_t16], [b_bbi])
                barrier(P)
                S.close()

            if STOP[0] != 11:
                setup()
            if STOP[0] in (11, 12):
                barrier(P)
                M.close()
                return
            thq, b_thq = M.sb([128, 64], F32, "thq")
            P.emit("dve", lambda e: e.tensor_scalar(out=thq[:], in0=thB[:], scalar1=1.0 / (2 * PI), scalar2=None, op0=ALU.mult), reads=[b_thB], writes=[b_thq])

            Bf = [[M.sb([128, 4, 128], BF16, f"Bf{k}_{i}") for i in range(2)] for k in range(2)]
            BT = [[M.sb([128, 4, 128], BF16, f"BT{k}_{i}") for i in range(2)] for k in range(2)]
            CT = [[M.sb([128, 4, 128], BF16, f"CT{k}_{i}") for i in range(2)] for k in range(2)]
            Dg = [M.sb([128, 128], BF16, f"Dg{k}") for k in range(2)]
            Cn = [M.sb([128, 64], F32, f"Cn{i}") for i in range(2)]
            Cnb = [M.sb([128, 128], BF16, f"Cnb{i}") for i in range(2)]
            kcn = [P.key(f"cn_{i}") for i in range(2)]
            ubs = [M.sb([128, TS], BF16, f"ub{k}") for k in range(2)]
            nSs = [M.sb([128, TS], F32, f"nS{i}") for i in range(2)]
            nCs = [M.sb([128, TS], F32, f"nC{i}") for i in range(2)]
            wr, b_wr = M.sb([128, TS], F32, "wr")
            wi, b_wi = M.sb([128, TS], F32, "wi")
            xrs = [M.sb([128, TS], BF16, f"xr{i}") for i in range(2)]
            xis = [M.sb([128, TS], BF16, f"xi{i}") for i in range(2)]
            tas = [M.sb([128, 1024], F32, f"ta{i}") for i in range(2)]
            tbs = [M.sb([128, 1024], F32, f"tb{i}") for i in range(2)]
            b_wrs = [Buf(f"wr{c}") for c in range(4)]
            b_wis = [Buf(f"wi{c}") for c in range(4)]
            b_xrs = [[Buf(f"xr{i}_{h}") for h in range(2)] for i in range(2)]
            b_xis = [[Buf(f"xi{i}_{h}") for h in range(2)] for i in range(2)]
            yb, b_yb = M.sb([128, TS], BF16, "yb")
            kyb = P.key(f"yb")
            wqc = {"n": 0}
            for k in range(2):
                for bf_, _b in Bf[k] + CT[k]:
                    P.emit("pool", lambda e, bf_=bf_: e.memset(bf_[:], 0.0), writes=[_b])

            def tables(gp):
                sl_ = gp % 2
                g1 = slice(gp, gp + 1)
                S_, b_S = nSs[sl_]
                C_, b_C = nCs[sl_]
                P.emit("dve", lambda e, g1=g1, C_=C_: e.tensor_scalar(out=C_[:], in0=iota[:], scalar1=thq[:, g1], scalar2=MAGIC, op0=ALU.mult, op1=ALU.add),
                       reads=[b_iota, b_thq], writes=[b_C])
                P.emit("dve", lambda e, C_=C_: e.tensor_scalar(out=C_[:], in0=C_[:], scalar1=-MAGIC, scalar2=-2 * PI, op0=ALU.add, op1=ALU.mult),
                       reads=[b_C], writes=[b_C])
                P.emit("dve", lambda e, g1=g1, C_=C_: e.scalar_tensor_tensor(out=C_[:], in0=iota[:], scalar=thB[:, g1], in1=C_[:], op0=ALU.mult, op1=ALU.add),
                       reads=[b_iota, b_thB, b_C], writes=[b_C])
                P.emit("dve", lambda e, C_=C_, S_=S_: e.tensor_scalar(out=S_[:], in0=C_[:], scalar1=-PI, scalar2=PI, op0=ALU.max, op1=ALU.min),
                       reads=[b_C], writes=[b_S])
                P.emit("act", lambda e, C_=C_, S_=S_: e.activation(out=C_[:], in_=S_[:], func=AF.Sin, scale=0.5), reads=[b_S, b_C], writes=[b_C])
                P.emit("act", lambda e, C_=C_: e.activation(out=C_[:], in_=C_[:], func=AF.Square), reads=[b_C], writes=[b_C])
                P.emit("act", lambda e, S_=S_: e.activation(out=S_[:], in_=S_[:], func=AF.Sin), reads=[b_S, b_C], writes=[b_S])
                P.emit("act", lambda e, C_=C_: e.activation(out=C_[:], in_=C_[:], func=AF.Identity, bias=1.0, scale=-2.0),
                       reads=[b_C], writes=[b_C])

            def rotate(gp, r):
                sl_ = gp % 2
                k = (gp // 4) % 2
                g1 = slice(gp, gp + 1)
                nS, b_nS = nSs[sl_]
                nC, b_nC = nCs[sl_]
                xr, b_xr = xrs[sl_]
                xi, b_xi = xis[sl_]
                ub, b_ub = ubs[k]
                rho_bc = rhoB[:, g1].to_broadcast([128, TS])
                dv = lambda fn, r_, w_: P.emit("dve", fn, reads=r_, writes=w_)
                for pair in range(2):
                    cs = (2 * pair, 2 * pair + 1)
                    sls = [slice(ch * 512, (ch + 1) * 512) for ch in cs]
                    pRs = [psA[0], psA[2]]
                    pIs = [psA[1], psA[3]]
                    for j, ch in enumerate(cs):
                        pR, b_pR = pRs[j]
                        pI, b_pI = pIs[j]
                        sl = sls[j]
                        P.emit("pe", lambda e, r=r, sl=sl, pR=pR, k=k, ub=ub: e.matmul(out=pR[:, :], lhsT=BT[k][0][0][:, r, :], rhs=ub[:, sl], start=True, stop=True),
                               reads=[BT[k][0][1], b_ub], writes=[b_pR])
                        P.emit("pe", lambda e, r=r, sl=sl, pI=pI, k=k, ub=ub: e.matmul(out=pI[:, :], lhsT=BT[k][1][0][:, r, :], rhs=ub[:, sl], start=True, stop=True),
                               reads=[BT[k][1][1], b_ub], writes=[b_pI])
                    tq = [(tas[j][0][:, 0:512], tas[j][1], tbs[j][0][:, 0:512], tbs[j][1]) for j in range(2)]
                    for j in range(2):
                        dv(lambda e, sl=sls[j], pR=pRs[j][0]: e.tensor_tensor(out=wr[:, sl], in0=pR[:, :], in1=nC[:, sl], op=ALU.mult), [pRs[j][1], b_nC], [b_wrs[cs[j]]])
                    for j in range(2):
                        dv(lambda e, sl=sls[j], pI=pIs[j][0], ta=tq[j][0]: e.tensor_tensor(out=ta, in0=pI[:, :], in1=nS[:, sl], op=ALU.mult), [pIs[j][1], b_nS], [tq[j][1]])
                    for j in range(2):
                        dv(lambda e, sl=sls[j], ta=tq[j][0]: e.tensor_tensor(out=wr[:, sl], in0=wr[:, sl], in1=ta, op=ALU.add), [b_wrs[cs[j]], tq[j][1]], [b_wrs[cs[j]]])
                    for j in range(2):
                        dv(lambda e, sl=sls[j], pI=pIs[j][0]: e.tensor_tensor(out=wi[:, sl], in0=pI[:, :], in1=nC[:, sl], op=ALU.mult), [pIs[j][1], b_nC], [b_wis[cs[j]]])
                    for j in range(2):
                        dv(lambda e, sl=sls[j], pR=pRs[j][0], tb=tq[j][2]: e.tensor_tensor(out=tb, in0=pR[:, :], in1=nS[:, sl], op=ALU.mult), [pRs[j][1], b_nS], [tq[j][3]])
                    for j in range(2):
                        dv(lambda e, sl=sls[j], tb=tq[j][2]: e.tensor_tensor(out=wi[:, sl], in0=wi[:, sl], in1=tb, op=ALU.subtract), [b_wis[cs[j]], tq[j][3]], [b_wis[cs[j]]])
                dv(lambda e: e.tensor_tensor_scan(out=wr[:], data0=rho_bc, data1=wr[:], initial=0.0, op0=ALU.mult, op1=ALU.add), [b_rhoB] + b_wrs, b_wrs)
                dv(lambda e: e.tensor_tensor_scan(out=wi[:], data0=rho_bc, data1=wi[:], initial=0.0, op0=ALU.mult, op1=ALU.add), [b_rhoB] + b_wis, b_wis)
                hs = [slice(0, 1024), slice(1024, 2048)]
                TA = [(tas[j][0], tas[j][1]) for j in range(2)]
                TB = [(tbs[j][0], tbs[j][1]) for j in range(2)]
                bw = [b_wrs[0:2], b_wrs[2:4]]
                bi = [b_wis[0:2], b_wis[2:4]]
                for j in range(2):
                    dv(lambda e, h=hs[j], ta=TA[j][0]: e.tensor_tensor(out=ta[:], in0=wr[:, h], in1=nC[:, h], op=ALU.mult), bw[j] + [b_nC], [TA[j][1]])
                for j in range(2):
                    dv(lambda e, h=hs[j], tb=TB[j][0]: e.tensor_tensor(out=tb[:], in0=wi[:, h], in1=nS[:, h], op=ALU.mult), bi[j] + [b_nS], [TB[j][1]])
                for j in range(2):
                    dv(lambda e, h=hs[j], ta=TA[j][0], tb=TB[j][0]: e.tensor_tensor(out=xr[:, h], in0=ta[:], in1=tb[:], op=ALU.subtract), [TA[j][1], TB[j][1]], [b_xrs[sl_][j]])
                for j in range(2):
                    dv(lambda e, h=hs[j], ta=TA[j][0]: e.tensor_tensor(out=ta[:], in0=wr[:, h], in1=nS[:, h], op=ALU.mult), bw[j] + [b_nS], [TA[j][1]])
                for j in range(2):
                    dv(lambda e, h=hs[j], tb=TB[j][0]: e.tensor_tensor(out=tb[:], in0=wi[:, h], in1=nC[:, h], op=ALU.mult), bi[j] + [b_nC], [TB[j][1]])
                for j in range(2):
                    dv(lambda e, h=hs[j], ta=TA[j][0], tb=TB[j][0]: e.tensor_tensor(out=xi[:, h], in0=ta[:], in1=tb[:], op=ALU.add), [TA[j][1], TB[j][1]], [b_xis[sl_][j]])
                for ch in range(4):
                    sl = slice(ch * 512, (ch + 1) * 512)
                    pY, b_pY = psY[ch]
                    if r == 0:
                        P.emit("pe", lambda e, sl=sl, pY=pY, k=k, ub=ub: e.matmul(out=pY[:, :], lhsT=Dg[k][0][:], rhs=ub[:, sl], start=True, stop=False),
                               reads=[Dg[k][1], b_ub], writes=[b_pY])
                    P.emit("pe", lambda e, r=r, sl=sl, pY=pY, k=k, xr=xr: e.matmul(out=pY[:, :], lhsT=CT[k][0][0][:, r, :], rhs=xr[:, sl], start=False, stop=False),
                           reads=[CT[k][0][1], b_xrs[sl_][ch // 2]], writes=[b_pY])
                    P.emit("pe", lambda e, r=r, sl=sl, pY=pY, k=k, xi=xi: e.matmul(out=pY[:, :], lhsT=CT[k][1][0][:, r, :], rhs=xi[:, sl], start=False, stop=(r == 3)),
                           reads=[CT[k][1][1], b_xis[sl_][ch // 2]], writes=[b_pY])

            def prep(ct):
                k = ct % 2
                i3 = wqc["n"] % 3
                wqc["n"] += 1
                wt, b_wt = wq[i3]
                ub, b_ub = ubs[k]
                P.dma("pool", wt[:], w_in[:, ct * 128:(ct + 1) * 128].rearrange("(kc p) n -> p kc n", p=128), kwq[i3], writes=[b_wt])

                def ev_u(ch, ps, b_ps):
                    sl = slice(ch * 512, (ch + 1) * 512)
                    P.emit("act", lambda e, ps=ps, sl=sl, ub=ub: e.copy(out=ub[:, sl], in_=ps[:, :]), reads=[b_ps], writes=[b_ub])
                proj_fm(P, hT, b_hT, wt, b_wt, psA[0:2], ev_u)
                dg_, b_dg = Dg[k]
                P.emit("dve", lambda e, dg_=dg_, ct=ct: e.tensor_scalar(out=dg_[:], in0=ident[:], scalar1=dsk[:, ct:ct + 1], scalar2=None, op0=ALU.mult),
                       reads=[b_id, b_dsk], writes=[b_dg])
                for ri in range(2):
                    src = (bbr, bbi)[ri]
                    b_src = (b_bbr, b_bbi)[ri]
                    bf_, b_bf = Bf[k][ri]
                    for r in range(4):
                        gp = 4 * ct + r
                        for two in range(2):
                            col0 = (2 * r + two) * 16
                            P.emit("pool", lambda e, bf_=bf_, r=r, two=two, col0=col0, gp=gp, src=src: e.tensor_copy(
                                out=bf_[two * 64:(two + 1) * 64, r, col0:col0 + 16], in_=src[two * 64:(two + 1) * 64, gp, :]),
                                reads=[b_src, b_bf], writes=[b_bf])
                    for r in range(4):
                        P.emit("pe", lambda e, bf_=bf_, r=r: e.transpose(out=psTb[:, r, :], in_=bf_[:, r, :], identity=ident[:]),
                               reads=[b_bf, b_id], writes=[b_psTb])
                    bt_, b_bt = BT[k][ri]
                    P.emit("act", lambda e, bt_=bt_: e.copy(out=bt_[:], in_=psTb[:, 0:4, :]), reads=[b_psTb], writes=[b_bt])
                    cn_, b_cn = Cn[ri]
                    cnb_, b_cnb = Cnb[ri]
                    csrc = W["c_re"] if ri == 0 else W["c_im"]
                    P.dma("sp", cn_[:], csrc[ct * 8:(ct + 1) * 8].rearrange("g c p -> (g c) p"), kcn[ri], writes=[b_cn])
                    for two in range(2):
                        P.emit("act", lambda e, cn_=cn_, cnb_=cnb_, ri=ri, two=two: e.activation(out=cnb_[:, two * 64:(two + 1) * 64], in_=cn_[:], func=AF.Identity,
                                                                                                 scale=(1.0 if ri == 0 else -1.0)),
                               reads=[b_cn, b_cnb], writes=[b_cnb])
                    P.emit("pe", lambda e, cnb_=cnb_, ri=ri: e.transpose(out=psTb[:, 4 + ri, :], in_=cnb_[:, :], identity=ident[:]),
                           reads=[b_cnb, b_id], writes=[b_psTb])
                    ct_, b_ctt = CT[k][ri]
                    for r in range(4):
                        for two in range(2):
                            col0 = (2 * r + two) * 16
                            P.emit("act", lambda e, ct_=ct_, r=r, two=two, col0=col0, ri=ri: e.copy(
                                out=ct_[two * 64:(two + 1) * 64, r, col0:col0 + 16], in_=psTb[two * 64:(two + 1) * 64, 4 + ri, col0:col0 + 16]),
                                reads=[b_psTb, b_ctt], writes=[b_ctt])

            def gelu_out(ct):
                for ch in range(4):
                    sl = slice(ch * 512, (ch + 1) * 512)
                    pY, b_pY = psY[ch]
                    tb, b_tb = tbs[ch % 2][0][:, 0:512], tbs[ch % 2][1]
                    P.emit("act", lambda e, pY=pY, tb=tb: e.activation(out=tb, in_=pY[:, :], func=AF.Square), reads=[b_pY], writes=[b_tb])
                    P.emit("dve", lambda e, tb=tb: e.tensor_scalar(out=tb, in0=tb, scalar1=0.044715, scalar2=1.0, op0=ALU.mult, op1=ALU.add), reads=[b_tb], writes=[b_tb])
                    P.emit("dve", lambda e, tb=tb, pY=pY: e.tensor_tensor(out=tb, in0=pY[:, :], in1=tb, op=ALU.mult), reads=[b_tb, b_pY], writes=[b_tb])
                    P.emit("act", lambda e, tb=tb: e.activation(out=tb, in_=tb, func=AF.Sigmoid, scale=GELU_K), reads=[b_tb], writes=[b_tb])
                    P.emit("dve", lambda e, sl=sl, tb=tb, pY=pY: e.tensor_tensor(out=yb[:, sl], in0=pY[:, :], in1=tb, op=ALU.mult), reads=[b_tb, b_pY], writes=[b_yb])
                P.dma("sp", yscr[ct * 128:(ct + 1) * 128, :], yb[:], kyb, reads=[b_yb], writes=[P.dram_buf(yscr, 0)])

            prep(0)
            tables(0)
            for ct in range(16):
                for r in range(4):
                    gp = 4 * ct + r
                    if r == 1 and ct + 1 < 16:
                        prep(ct + 1)
                    if gp + 1 < 64:
                        tables(gp + 1)
                    rotate(gp, r)
                gelu_out(ct)
            barrier(P)
            M.close()

        main_phase()
        O.close()
        if STOP[0] >= 11:
            return

        def comb(P, cur, x_t, b_x, c, tmp):
            (pv, b_pv), (pg, b_pg) = cur
            tm, b_tm = tmp
            csl = slice(c * 512, (c + 1) * 512)
            P.emit("act", lambda e, pg=pg, tm=tm: e.activation(out=tm[:], in_=pg[:, :], func=AF.Sigmoid), reads=[b_pg], writes=[b_tm])
            P.emit("dve", lambda e, pv=pv, tm=tm: e.tensor_tensor(out=tm[:], in0=pv[:, :], in1=tm[:], op=ALU.mult), reads=[b_pv, b_tm], writes=[b_tm])
            P.emit("dve", lambda e, tm=tm, x_t=x_t, csl=csl: e.tensor_tensor(out=x_t[:, csl], in0=x_t[:, csl], in1=tm[:], op=ALU.add),
                   reads=[b_tm, b_x], writes=[b_x])
        outproj_phase(P, tag, xin, xout, base, yscr, [W["w_glu_v"], W["w_glu_g"]], comb)
        barrier(P)

    for sq in range(NSEQ):
        seq_body(sq)


NSEQ_CORE = 2
T_CORE = NSEQ_CORE * TS
N_CORES = 8

_SPECS = [
    ("x", [T_CORE, D]), ("norm_mix", [2, D]), ("norm_ffn", [2, D]), ("norm_final", [1, D]),
    ("ab_w_in", [D, 6152]), ("lru_conv_w", [4, 1024]), ("lru_conv_b", [1024]), ("lru_w_a", [8, 128, 128]),
    ("lru_b_a", [1024]), ("lru_w_x", [8, 128, 128]), ("lru_b_x", [1024]), ("lru_lam", [1024]),
    ("m_conv_w", [4, 2048]), ("m_conv_b", [2048]), ("m_i_bias", [1, 4]), ("m_f_bias", [1, 4]), ("m_head_g", [1, 1024]),
    ("ab_w_out", [D, D]), ("s5_w_in", [D, D]), ("s5_a_re", [128, 64]), ("s5_a_im", [128, 64]), ("s5_log_step", [128]),
    ("s5_b_re", [128, 64, 16]), ("s5_b_im", [128, 64, 16]), ("s5_c_re", [128, 16, 64]), ("s5_c_im", [128, 16, 64]),
    ("s5_d", [2048]), ("s5_w_glu_v", [D, D]), ("s5_w_glu_g", [D, D]),
    ("moe_w_coarse", [2, D, 4]), ("moe_b_coarse", [2, 4]), ("moe_w_fine", [2, D, 16]), ("moe_b_fine", [2, 16]),
    ("moe_w_gate", [2, NEXP, D, FF]), ("moe_w_up", [2, NEXP, D, FF]), ("moe_w_down", [2, NEXP, FF, D]),
]


def build_program():
    nc = bass.Bass("TRN2", target_bir_lowering=False)
    A = {n: nc.dram_tensor(n, list(shp), F32, kind="ExternalInput").ap() for n, shp in _SPECS}
    out = nc.dram_tensor("out", [T_CORE, D], F32, kind="ExternalOutput").ap()
    xs = [nc.dram_tensor(f"xs{i}", [T_CORE, D], F32, kind="Internal").ap() for i in range(3)]
    yscr = [nc.dram_tensor(f"yscr{i}", [D, TS], BF16, kind="Internal").ap() for i in range(NSEQ_CORE)]
    P = Prog(nc)
    WA = {"norm": A["norm_mix"][0:1, :], "w_in": A["ab_w_in"], "lru_conv_w": A["lru_conv_w"], "lru_conv_b": A["lru_conv_b"],
          "lru_w_a": A["lru_w_a"], "lru_b_a": A["lru_b_a"], "lru_w_x": A["lru_w_x"], "lru_b_x": A["lru_b_x"],
          "lru_lam": A["lru_lam"], "m_conv_w": A["m_conv_w"], "m_conv_b": A["m_conv_b"], "m_i_bias": A["m_i_bias"],
          "m_f_bias": A["m_f_bias"], "m_head_g": A["m_head_g"], "w_out": A["ab_w_out"]}
    WS = {"norm": A["norm_mix"][1:2, :], "w_in": A["s5_w_in"], "a_re": A["s5_a_re"], "a_im": A["s5_a_im"],
          "log_step": A["s5_log_step"], "b_re": A["s5_b_re"], "b_im": A["s5_b_im"], "c_re": A["s5_c_re"],
          "c_im": A["s5_c_im"], "d": A["s5_d"], "w_glu_v": A["s5_w_glu_v"], "w_glu_g": A["s5_w_glu_g"]}

    def moe(layer, xi, xo, gfin):
        moe_stage(P, xi, xo, T_CORE, A["norm_ffn"][layer:layer + 1, :], A["moe_w_coarse"][layer],
                  A["moe_b_coarse"][layer:layer + 1, :], A["moe_w_fine"][layer], A["moe_b_fine"][layer:layer + 1, :],
                  A["moe_w_gate"][layer], A["moe_w_up"][layer], A["moe_w_down"][layer], g_final=gfin)
        barrier(P)

    mixer_a_stage(P, A["x"], xs[0], NSEQ_CORE, WA, yscr)
    moe(0, xs[0], xs[1], None)
    s5_stage(P, xs[1], xs[2], NSEQ_CORE, WS, yscr)
    moe(1, xs[2], out, A["norm_final"])
    P.finish()
    P.build()
    P.close()
    return nc


def kernel(**inputs):
    f = lambda a: np.ascontiguousarray(np.asarray(a, dtype=np.float32))
    x = f(inputs["x"])
    B = x.shape[0]
    shared = {}
    for n, shp in _SPECS:
        if n == "x":
            continue
        shared[n] = f(inputs[n]).reshape(shp)
    nc = build_program()
    in_maps = []
    for c in range(N_CORES):
        m = dict(shared)
        m["x"] = x[c * NSEQ_CORE:(c + 1) * NSEQ_CORE].reshape(T_CORE, D)
        in_maps.append(m)
    res = run_bass_kernel_spmd(nc, in_maps, core_ids=list(range(N_CORES)))
    outs = [np.asarray(r["out"], dtype=np.float32).reshape(NSEQ_CORE, TS, D) for r in res.results]
    return np.concatenate(outs, axis=0)
```

```python
import numpy as np
from contextlib import ExitStack
import concourse.bass as bass
import concourse.mybir as mybir
from concourse.bass_utils import run_bass_kernel_spmd

F32 = mybir.dt.float32
BF16 = mybir.dt.bfloat16
ALU = mybir.AluOpType
AF = mybir.ActivationFunctionType
AX = mybir.AxisListType


class Buf:
    __slots__ = ("name", "lw", "rd")

    def __init__(self, name):
        self.name = name
        self.lw = None
        self.rd = []


class SemKey:
    __slots__ = ("name", "sem", "count", "group", "epoch", "totals")

    def __init__(self, name, group=False):
        self.name = name
        self.sem = None
        self.count = 0
        self.group = group


class Op:
    __slots__ = ("eng", "fn", "deps", "sig", "signo", "key", "idx", "ep")

    def __init__(self, eng, fn, key=None):
        self.eng = eng
        self.fn = fn
        self.deps = []
        self.sig = False
        self.signo = 0
        self.key = key
        self.idx = 0


ENGS = ("pe", "act", "dve", "pool", "sp")


class Prog:
    def __init__(self, nc):
        self.nc = nc
        self.stack = ExitStack()
        self.ops = {e: [] for e in ENGS}
        self.nops = 0
        self.keys = []
        self.ntile = 0
        self.bar_idx = 0
        self.out_ops = []
        self.stage_stacks = []
        self._uid = 0
        self._dbufs = {}
        self._keyreg = {}

    def uid(self):
        self._uid += 1
        return self._uid

    def dram_buf(self, ap, r0):
        k = (ap.tensor.name, r0)
        if k not in self._dbufs:
            self._dbufs[k] = Buf(f"d_{k}")
        return self._dbufs[k]

    def finish(self):
        op = Op("sp", None)
        op.idx = self.nops
        self.nops += 1
        op.deps = list(self.out_ops)
        self.ops["sp"].append(op)

    def sb(self, shape, dtype, name=None):
        self.ntile += 1
        name = name or f"t{self.ntile}"
        return self.stack.enter_context(self.nc.sbuf_tensor(name, list(shape), dtype))

    def ps(self, shape, dtype, name=None):
        self.ntile += 1
        name = name or f"p{self.ntile}"
        return self.stack.enter_context(self.nc.psum_tensor(name, list(shape), dtype))

    def key(self, name, group=False):
        if name in self._keyreg:
            k = self._keyreg[name]
            assert k.group == group
            return k
        k = SemKey(name, group)
        k.epoch = 0
        k.totals = {}
        self._keyreg[name] = k
        self.keys.append(k)
        return k

    def close_epochs(self):
        for k in self.keys:
            if k.group:
                k.totals[k.epoch] = k.count
                k.epoch += 1

    def _deps(self, op, reads, writes):
        deps = []
        for b in reads:
            if b.lw is not None:
                deps.append(b.lw)
        for b in writes:
            if b.lw is not None:
                deps.append(b.lw)
            deps.extend(b.rd)
        for b in reads:
            b.rd.append(op)
        for b in writes:
            b.lw = op
            b.rd = []
        seen = set()
        for d in deps:
            if d is op or id(d) in seen:
                continue
            seen.add(id(d))
            if d.key is None and d.eng == "pe" and op.eng == "pe" and op.key is None:
                continue
            op.deps.append(d)
            d.sig = True

    def emit(self, eng, fn, reads=(), writes=()):
        op = Op(eng, fn)
        op.idx = self.nops
        self.nops += 1
        self._deps(op, reads, writes)
        self.ops[eng].append(op)
        return op

    def dma(self, eng, out, in_, key, reads=(), writes=(), **kw):
        def fn(e, out=out, in_=in_, kw=kw):
            return e.dma_start(out=out, in_=in_, **kw)
        op = Op(eng, fn, key=key)
        op.idx = self.nops
        self.nops += 1
        self._deps(op, reads, writes)
        key.count += 16
        op.signo = key.count
        op.ep = key.epoch
        op.sig = True
        self.ops[eng].append(op)
        return op

    def build(self):
        nc = self.nc
        st = self.stack
        self.close_epochs()
        esem = {e: st.enter_context(nc.semaphore(f"s_{e}")) for e in ENGS}
        for k in self.keys:
            if k.count > 0:
                k.sem = st.enter_context(nc.semaphore(f"k_{k.name}"))
        for e in ENGS:
            c = 0
            for op in self.ops[e]:
                if op.key is None and op.sig:
                    c += 1
                    op.signo = c
        ops = self.ops

        def run(ename, eng):
            waited = {}
            for op in ops[ename]:
                need = {}
                for d in op.deps:
                    if d.key is not None:
                        s = d.key.sem
                        v = d.key.totals[d.ep] if d.key.group else d.signo
                    else:
                        s = esem[d.eng]
                        v = d.signo
                    sid = id(s)
                    if v > need.get(sid, (None, 0))[1]:
                        need[sid] = (s, v)
                for sid, (s, v) in need.items():
                    if waited.get(sid, 0) >= v:
                        continue
                    waited[sid] = v
                    eng.wait_ge(s, v)
                if op.fn is None:
                    continue
                ins = op.fn(eng)
                if op.key is not None:
                    ins.then_inc(op.key.sem, 16)
                elif op.sig:
                    ins.then_inc(esem[ename], 1)

        block = st.enter_context(nc.Block())

        @block.tensor
        def _(e):
            run("pe", e)

        @block.scalar
        def _(e):
            run("act", e)

        @block.vector
        def _(e):
            run("dve", e)

        @block.gpsimd
        def _(e):
            run("pool", e)

        @block.sync
        def _(e):
            run("sp", e)

    def close(self):
        self.stack.close()
        for st in reversed(self.stage_stacks):
            st.close()


D = 2048
EPS = 1e-6
NEXP = 16
FF = 512


class Ctx:
    pass


def barrier(P):
    lasts = []
    for e in ENGS:
        if P.ops[e]:
            for op in reversed(P.ops[e]):
                if op.fn is not None and op.key is None:
                    lasts.append(op)
                    op.sig = True
                    break
    dmas = [op for e in ENGS for op in P.ops[e] if op.key is not None and op.idx >= P.bar_idx]
    for e in ENGS:
        op = Op(e, None)
        op.idx = P.nops
        P.nops += 1
        op.deps = [d for d in lasts] + dmas
        P.ops[e].append(op)
    P.bar_idx = P.nops
    P.close_epochs()


def rmsnorm_rows(P, src, dst, gbc, b_src, b_dst, b_g, scr, tag):
    junk, ssq, rstd, b_junk, b_ssq, b_rstd = scr
    P.emit("act", lambda e: e.activation(out=junk, in_=src, func=AF.Square, accum_out=ssq),
           reads=[b_src], writes=[b_junk, b_ssq])
    P.emit("dve", lambda e: e.tensor_scalar(out=rstd, in0=ssq, scalar1=1.0 / D, scalar2=EPS,
                                            op0=ALU.mult, op1=ALU.add), reads=[b_ssq], writes=[b_rstd])
    P.emit("act", lambda e: e.activation(out=rstd, in_=rstd, func=AF.Sqrt), reads=[b_rstd], writes=[b_rstd])
    P.emit("dve", lambda e: e.reciprocal(out=rstd, in_=rstd), reads=[b_rstd], writes=[b_rstd])
    P.emit("dve", lambda e: e.scalar_tensor_tensor(out=dst, in0=src, scalar=rstd, in1=gbc,
                                                   op0=ALU.mult, op1=ALU.mult),
           reads=[b_src, b_rstd, b_g], writes=[b_dst])


def moe_stage(P, xin, xout, T, g_ffn, w_coarse, b_coarse, w_fine, b_fine, w_gate, w_up, w_down,
              g_final=None, nexp=NEXP, TT=1024):
    nc = P.nc
    NS = TT // 128
    NH = TT // 512
    ntiles = T // TT
    st = ExitStack()
    sbuf = lambda shape, dt, name: st.enter_context(nc.sbuf_tensor(name, list(shape), dt))
    psum = lambda shape, dt, name: st.enter_context(nc.psum_tensor(name, list(shape), dt))
    u = P.uid()

    yacc = sbuf([128, NS, D], F32, f"yacc{u}")
    hT = sbuf([128, 16, TT], BF16, f"hT{u}")
    hns = [sbuf([128, D], BF16, f"hn{u}_{i}") for i in range(2)]
    hn = hns[0]
    gbc = sbuf([128, D], F32, f"gbc{u}")
    NSLOT = 5 if g_final is None else 4
    ring = [sbuf([128, 16 * 512], BF16, f"ring{u}_{i}") for i in range(NSLOT)]
    sg = [sbuf([128, 512], F32, f"sg{u}_{i}") for i in range(2)]
    he = sbuf([128, 4, TT], BF16, f"he{u}")
    wr = sbuf([128, 16, 20], BF16, f"wr{u}")
    brc = sbuf([128, 20], F32, f"brc{u}")
    ident = sbuf([128, 128], BF16, f"ident{u}")
    gates = sbuf([128, NS, 16], F32, f"gates{u}")
    sm = sbuf([128, NS, 64], F32, f"sm{u}")
    ssq = sbuf([128, NS], F32, f"ssq{u}")
    rstd = sbuf([128, NS], F32, f"rstd{u}")
    if g_final is not None:
        gfin = sbuf([128, D], F32, f"gfin{u}")

    psG = [psum([128, 512], F32, f"psG{u}_{i}") for i in range(2)]
    psU = [psum([128, 512], F32, f"psU{u}_{i}") for i in range(2)]
    psY = [psum([128, 512], F32, f"psY{u}_{i}") for i in range(2)]
    psT = [psum([128, 8, 128], BF16, f"psT{u}_{i}") for i in range(2)]

    B = lambda n: Buf(f"{n}{u}")
    b_yacc = [B(f"yacc{s}") for s in range(NS)]
    b_hT = [B(f"hT{s}") for s in range(NS)]
    b_hn, b_g, b_wr, b_br, b_id, b_he = B("hn"), B("g"), B("wr"), B("br"), B("id"), B("he")
    b_hns = [b_hn, B("hn1")]
    b_ring = [B(f"ring{i}") for i in range(NSLOT)]
    b_sg = [B("sg0"), B("sg1")]
    b_gates = [B(f"gates{s}") for s in range(NS)]
    b_sm = [B(f"sm{s}") for s in range(NS)]
    b_ssq = [B(f"ssq{s}") for s in range(NS)]
    b_rstd = [B(f"rstd{s}") for s in range(NS)]
    b_psG, b_psU, b_psY, b_psT = [B("pg0"), B("pg1")], [B("pu0"), B("pu1")], [B("py0"), B("py1")], [B("pt0"), B("pt1")]
    k_c = P.key(f"mc", group=True)
    k_cp = P.key(f"mcp", group=True)
    k_x = [P.key(f"mx_{s}") for s in range(NS)]
    k_ring = [P.key(f"mr_{i}") for i in range(NSLOT)]

    P.dma("sp", gbc[:], g_ffn.partition_broadcast(128), k_c, writes=[b_g])
    b_br2, b_wr2 = B("br2"), B("wr2")
    P.dma("sp", brc[:, 0:4], b_coarse.partition_broadcast(128), k_c, writes=[b_br])
    P.dma("sp", brc[:, 4:20], b_fine.partition_broadcast(128), k_c, writes=[b_br2])
    P.dma("pool", wr[:, :, 0:4], w_coarse.rearrange("(kc p) n -> p kc n", p=128), k_cp, writes=[b_wr])
    P.dma("pool", wr[:, :, 4:20], w_fine.rearrange("(kc p) n -> p kc n", p=128), k_cp, writes=[b_wr2])
    if g_final is not None:
        b_gf = B("gf")
        P.dma("sp", gfin[:], g_final.partition_broadcast(128), k_c, writes=[b_gf])
    P.emit("pool", lambda e: e.memset(ident[:], 1.0), writes=[b_id])
    P.emit("pool", lambda e: e.affine_select(out=ident[:], in_=ident[:], pattern=[[-1, 128]],
                                              compare_op=ALU.is_equal, fill=0.0, base=0,
                                              channel_multiplier=1), reads=[b_id], writes=[b_id])

    wseq = []
    for t in range(ntiles):
        for ex in range(nexp):
            for which in range(3):
                wseq.append((t, ex, which))
    wslot = {}
    state = {"next": 0}

    def issue_w(upto):
        while state["next"] < min(upto, len(wseq)):
            i = state["next"]
            t, ex, which = wseq[i]
            sl = i % NSLOT
            wslot[(t, ex, which)] = sl
            if which < 2:
                src = (w_gate if which == 0 else w_up)[ex].rearrange("(kc p) n -> p kc n", p=128)
                dst = ring[sl][:, :].rearrange("p (kc n) -> p kc n", kc=16)
            else:
                src = w_down[ex].rearrange("(f p) n -> p f n", p=128)
                dst = ring[sl][:, :].rearrange("p (f n) -> p f n", f=4)
            P.dma("pool", dst, src, k_ring[sl], writes=[b_ring[sl]])
            state["next"] += 1

    issue_w(NSLOT)
    gcount = 0
    ycount = 0
    for t in range(ntiles):
        for s in range(NS):
            r0 = t * TT + s * 128
            P.dma("sp", yacc[:, s, :], xin[r0:r0 + 128, :], k_x[s], reads=[P.dram_buf(xin, r0)], writes=[b_yacc[s]])
        for s in range(NS):
            hn_s, b_hn_s = hns[s % 2], b_hns[s % 2]
            rmsnorm_rows(P, yacc[:, s, :], hn_s[:], gbc[:], b_yacc[s], b_hn_s, b_g,
                         (hn_s[:], ssq[:, s:s + 1], rstd[:, s:s + 1], b_hn_s, b_ssq[s], b_rstd[s]), "m")
            for half in range(2):
                pt = psT[half]
                for j in range(8):
                    kc = half * 8 + j
                    P.emit("pe", lambda e, pt=pt, j=j, kc=kc, hn_s=hn_s: e.transpose(
                        out=pt[:, j, :], in_=hn_s[:, kc * 128:(kc + 1) * 128], identity=ident[:]),
                        reads=[b_hn_s, b_id], writes=[b_psT[half]])
                eng = "act" if half == 0 else "dve"
                if eng == "act":
                    P.emit("act", lambda e, pt=pt, half=half, s=s: e.copy(
                        out=hT[:, half * 8:half * 8 + 8, s * 128:(s + 1) * 128], in_=pt[:, :, :]),
                        reads=[b_psT[half]], writes=[b_hT[s]])
                else:
                    P.emit("dve", lambda e, pt=pt, half=half, s=s: e.tensor_copy(
                        out=hT[:, half * 8:half * 8 + 8, s * 128:(s + 1) * 128], in_=pt[:, :, :]),
                        reads=[b_psT[half]], writes=[b_hT[s]])
            pr = psY[1]
            for kc in range(16):
                P.emit("pe", lambda e, kc=kc, s=s, pr=pr: e.matmul(
                    out=pr[:, 0:20], lhsT=hT[:, kc, s * 128:(s + 1) * 128], rhs=wr[:, kc, :],
                    start=(kc == 0), stop=(kc == 15)),
                    reads=[b_hT[s], b_wr, b_wr2], writes=[b_psY[1]])
            S = sm[:, s, :]
            lg, gmax, ohg, ngmax, ex4, sume, pg = S[:, 0:20], S[:, 20:21], S[:, 21:25], S[:, 25:26], S[:, 26:30], S[:, 30:31], S[:, 31:32]
            lfs, m1, mk1, lf2, m2, mk2 = S[:, 32:36], S[:, 36:37], S[:, 37:41], S[:, 41:45], S[:, 45:46], S[:, 46:50]
            d21, e2, den, wa, wb, gsel = S[:, 50:51], S[:, 51:52], S[:, 52:53], S[:, 53:54], S[:, 54:55], S[:, 55:59]
            bs = b_sm[s]

            def dv(fn, extra_r=(), extra_w=()):
                P.emit("dve", fn, reads=[bs] + list(extra_r), writes=[bs] + list(extra_w))

            def ac(fn):
                P.emit("act", fn, reads=[bs], writes=[bs])
            dv(lambda e, lg=lg, pr=pr: e.tensor_tensor(out=lg, in0=pr[:, 0:20], in1=brc[:], op=ALU.add),
               extra_r=[b_psY[1], b_br, b_br2])
        for s in range(NS):
            pr = psY[1]
            S = sm[:, s, :]
            lg, gmax, ohg, ngmax, ex4, sume, pg = S[:, 0:20], S[:, 20:21], S[:, 21:25], S[:, 25:26], S[:, 26:30], S[:, 30:31], S[:, 31:32]
            lfs, m1, mk1, lf2, m2, mk2 = S[:, 32:36], S[:, 36:37], S[:, 37:41], S[:, 41:45], S[:, 45:46], S[:, 46:50]
            d21, e2, den, wa, wb, gsel = S[:, 50:51], S[:, 51:52], S[:, 52:53], S[:, 53:54], S[:, 54:55], S[:, 55:59]
            bs = b_sm[s]

            def dv(fn, extra_r=(), extra_w=()):
                P.emit("dve", fn, reads=[bs] + list(extra_r), writes=[bs] + list(extra_w))

            def ac(fn):
                P.emit("act", fn, reads=[bs], writes=[bs])
            dv(lambda e, lg=lg, gmax=gmax: e.reduce_max(out=gmax, in_=lg[:, 0:4], axis=AX.X))
            dv(lambda e, lg=lg, gmax=gmax, ohg=ohg: e.tensor_scalar(out=ohg, in0=lg[:, 0:4], scalar1=gmax, scalar2=None, op0=ALU.is_ge))
            dv(lambda e, gmax=gmax, ngmax=ngmax: e.tensor_scalar(out=ngmax, in0=gmax, scalar1=-1.0, scalar2=None, op0=ALU.mult))
            ac(lambda e, lg=lg, ngmax=ngmax, ex4=ex4, sume=sume: e.activation(out=ex4, in_=lg[:, 0:4], func=AF.Exp, bias=ngmax, scale=1.0, accum_out=sume))
            dv(lambda e, pg=pg, sume=sume: e.reciprocal(out=pg, in_=sume))
            dv(lambda e, lg=lg, lfs=lfs, ohg=ohg: e.tensor_scalar(out=lfs, in0=lg[:, 4:8], scalar1=ohg[:, 0:1], scalar2=None, op0=ALU.mult))
            for g in range(1, 4):
                dv(lambda e, lg=lg, lfs=lfs, ohg=ohg, g=g: e.scalar_tensor_tensor(
                    out=lfs, in0=lg[:, 4 + 4 * g:8 + 4 * g], scalar=ohg[:, g:g + 1], in1=lfs, op0=ALU.mult, op1=ALU.add))
            dv(lambda e, lfs=lfs, m1=m1: e.reduce_max(out=m1, in_=lfs, axis=AX.X))
            dv(lambda e, lfs=lfs, m1=m1, mk1=mk1: e.tensor_scalar(out=mk1, in0=lfs, scalar1=m1, scalar2=None, op0=ALU.is_ge))
            dv(lambda e, lfs=lfs, lf2=lf2, mk1=mk1: e.scalar_tensor_tensor(out=lf2, in0=mk1, scalar=-1e30, in1=lfs, op0=ALU.mult, op1=ALU.add))
            dv(lambda e, lf2=lf2, m2=m2: e.reduce_max(out=m2, in_=lf2, axis=AX.X))
            dv(lambda e, lf2=lf2, m2=m2, mk2=mk2: e.tensor_scalar(out=mk2, in0=lf2, scalar1=m2, scalar2=None, op0=ALU.is_ge))
            dv(lambda e, d21=d21, m1=m1, m2=m2: e.tensor_tensor(out=d21, in0=m2, in1=m1, op=ALU.subtract))
            ac(lambda e, d21=d21, e2=e2: e.activation(out=e2, in_=d21, func=AF.Exp))
            dv(lambda e, e2=e2, den=den: e.tensor_scalar(out=den, in0=e2, scalar1=1.0, scalar2=None, op0=ALU.add))
            dv(lambda e, den=den: e.reciprocal(out=den, in_=den))
            dv(lambda e, den=den, pg=pg, wa=wa: e.tensor_tensor(out=wa, in0=den, in1=pg, op=ALU.mult))
            dv(lambda e, wa=wa, wb=wb, e2=e2: e.tensor_tensor(out=wb, in0=wa, in1=e2, op=ALU.mult))
            dv(lambda e, gsel=gsel, mk1=mk1, wa=wa: e.tensor_scalar(out=gsel, in0=mk1, scalar1=wa, scalar2=None, op0=ALU.mult))
            dv(lambda e, gsel=gsel, mk2=mk2, wb=wb: e.scalar_tensor_tensor(out=gsel, in0=mk2, scalar=wb, in1=gsel, op0=ALU.mult, op1=ALU.add))
            for g in range(4):
                dv(lambda e, gsel=gsel, ohg=ohg, g=g, s=s: e.tensor_scalar(
                    out=gates[:, s, 4 * g:4 * g + 4], in0=gsel, scalar1=ohg[:, g:g + 1], scalar2=None, op0=ALU.mult),
                    extra_w=[b_gates[s]])

        for ex in range(nexp):
            wi = (t * nexp + ex) * 3
            issue_w(wi + NSLOT)
            sg_, su_, sd_ = wslot[(t, ex, 0)], wslot[(t, ex, 1)], wslot[(t, ex, 2)]
            Wg = ring[sg_][:, :].rearrange("p (kc n) -> p kc n", kc=16)
            Wu = ring[su_][:, :].rearrange("p (kc n) -> p kc n", kc=16)
            Wd = ring[sd_][:, :].rearrange("p (f n) -> p f n", f=4)
            for f in range(4):
                for h in range(NH):
                    gi = gcount % 2
                    gcount += 1
                    for kc in range(16):
                        P.emit("pe", lambda e, kc=kc, f=f, h=h, gi=gi, Wg=Wg: e.matmul(
                            out=psG[gi][:, :], lhsT=Wg[:, kc, f * 128:(f + 1) * 128],
                            rhs=hT[:, kc, h * 512:(h + 1) * 512], start=(kc == 0), stop=(kc == 15)),
                            reads=[b_ring[sg_]] + b_hT[h * 4:(h + 1) * 4], writes=[b_psG[gi]])
                    for kc in range(16):
                        P.emit("pe", lambda e, kc=kc, f=f, h=h, gi=gi, Wu=Wu: e.matmul(
                            out=psU[gi][:, :], lhsT=Wu[:, kc, f * 128:(f + 1) * 128],
                            rhs=hT[:, kc, h * 512:(h + 1) * 512], start=(kc == 0), stop=(kc == 15)),
                            reads=[b_ring[su_]] + b_hT[h * 4:(h + 1) * 4], writes=[b_psU[gi]])
                    P.emit("act", lambda e, gi=gi: e.activation(out=sg[gi][:], in_=psG[gi][:], func=AF.Silu),
                           reads=[b_psG[gi]], writes=[b_sg[gi]])
                    P.emit("dve", lambda e, gi=gi, f=f, h=h: e.tensor_tensor(
                        out=he[:, f, h * 512:(h + 1) * 512], in0=psU[gi][:], in1=sg[gi][:], op=ALU.mult),
                        reads=[b_psU[gi], b_sg[gi]], writes=[b_he])
            for s in range(NS):
                for c in range(4):
                    yi = ycount % 2
                    ycount += 1
                    for f in range(4):
                        P.emit("pe", lambda e, f=f, s=s, c=c, yi=yi, Wd=Wd: e.matmul(
                            out=psY[yi][:, :], lhsT=he[:, f, s * 128:(s + 1) * 128],
                            rhs=Wd[:, f, c * 512:(c + 1) * 512], start=(f == 0), stop=(f == 3)),
                            reads=[b_he, b_ring[sd_]], writes=[b_psY[yi]])
                    P.emit("dve", lambda e, s=s, c=c, yi=yi, ex=ex: e.scalar_tensor_tensor(
                        out=yacc[:, s, c * 512:(c + 1) * 512], in0=psY[yi][:], scalar=gates[:, s, ex:ex + 1],
                        in1=yacc[:, s, c * 512:(c + 1) * 512], op0=ALU.mult, op1=ALU.add),
                        reads=[b_psY[yi], b_gates[s], b_yacc[s]], writes=[b_yacc[s]])
        for s in range(NS):
            r0 = t * TT + s * 128
            if g_final is not None:
                rmsnorm_rows(P, yacc[:, s, :], yacc[:, s, :], gfin[:], b_yacc[s], b_yacc[s], b_gf,
                             (hn[:], ssq[:, s:s + 1], rstd[:, s:s + 1], b_hn, b_ssq[s], b_rstd[s]), "f")
            P.out_ops.append(P.dma("sp", xout[r0:r0 + 128, :], yacc[:, s, :], k_x[s],
                                   reads=[b_yacc[s]], writes=[P.dram_buf(xout, r0)]))
    st.close()


TS = 2048
NT = 16
GELU_K = 1.5957691216057308


class Pool_:
    def __init__(self, P, tag):
        self.P, self.nc, self.tag = P, P.nc, tag
        self.st = ExitStack()

    def sb(self, shape, dt, name):
        t = self.st.enter_context(self.nc.sbuf_tensor(f"{name}_{self.tag}", list(shape), dt))
        return t, Buf(f"{name}_{self.tag}")

    def ps(self, shape, dt, name):
        t = self.st.enter_context(self.nc.psum_tensor(f"{name}_{self.tag}", list(shape), dt))
        return t, Buf(f"{name}_{self.tag}")

    def close(self):
        self.st.close()


def make_ident(P, ident, b_id):
    P.emit("pool", lambda e: e.memset(ident[:], 1.0), writes=[b_id])
    P.emit("pool", lambda e: e.affine_select(out=ident[:], in_=ident[:], pattern=[[-1, 128]],
                                              compare_op=ALU.is_equal, fill=0.0, base=0,
                                              channel_multiplier=1), reads=[b_id], writes=[b_id])


def build_hT(P, tag, xin, base, hT, b_hT, gbc, b_g, ident, b_id):
    A = Pool_(P, f"h{tag}")
    xt = [A.sb([128, D], F32, f"xt{i}") for i in range(2)]
    hn, b_hn = A.sb([128, D], BF16, "hn")
    ssq, b_ssq = A.sb([128, 2], F32, "ssq")
    rstd, b_rstd = A.sb([128, 2], F32, "rstd")
    psT = [A.ps([128, 8, 128], BF16, f"psT{i}") for i in range(2)]
    kx = [P.key(f"hx_{i}") for i in range(2)]
    for tt in range(NT):
        i = tt % 2
        x_t, b_x = xt[i]
        r0 = base + tt * 128
        P.dma("sp", x_t[:], xin[r0:r0 + 128, :], kx[i], reads=[P.dram_buf(xin, r0)], writes=[b_x])
        rmsnorm_rows(P, x_t[:], hn[:], gbc[:], b_x, b_hn, b_g,
                     (hn[:], ssq[:, 0:1], rstd[:, 0:1], b_hn, b_ssq, b_rstd), "h")
        for half in range(2):
            pt, b_pt = psT[half]
            for j in range(8):
                kc = half * 8 + j
                P.emit("pe", lambda e, pt=pt, j=j, kc=kc: e.transpose(
                    out=pt[:, j, :], in_=hn[:, kc * 128:(kc + 1) * 128], identity=ident[:]),
                    reads=[b_hn, b_id], writes=[b_pt])
            if half == 0:
                P.emit("act", lambda e, pt=pt, tt=tt: e.copy(out=hT[:, 0:8, tt * 128:(tt + 1) * 128], in_=pt[:, :, :]),
                       reads=[b_pt], writes=[b_hT])
            else:
                P.emit("dve", lambda e, pt=pt, tt=tt: e.tensor_copy(out=hT[:, 8:16, tt * 128:(tt + 1) * 128], in_=pt[:, :, :]),
                       reads=[b_pt], writes=[b_hT])
    A.close()


def load_cols(P, dst, src1d, n, key, b):
    P.dma("sp", dst, src1d.rearrange("(n f) -> f n", f=128), key, writes=[b], allow_slow_non_contiguous=True)


def conv4(P, src, b_src, cw, cb, out, b_out, b_c):
    P.emit("dve", lambda e: e.tensor_scalar(out=out, in0=src[:, 3:3 + TS], scalar1=cw[:, 3:4], scalar2=cb,
                                            op0=ALU.mult, op1=ALU.add), reads=[b_src] + list(b_c), writes=[b_out])
    for j in (2, 1, 0):
        P.emit("dve", lambda e, j=j: e.scalar_tensor_tensor(out=out, in0=src[:, j:j + TS], scalar=cw[:, j:j + 1],
                                                          in1=out, op0=ALU.mult, op1=ALU.add),
               reads=[b_src, b_out] + list(b_c), writes=[b_out])


def proj_fm(P, hT, b_hT, wblk, b_w, pss, evac):
    for ch in range(4):
        ps, b_ps = pss[ch % len(pss)]
        for kc in range(16):
            P.emit("pe", lambda e, kc=kc, ch=ch, ps=ps: e.matmul(
                out=ps[:, :], lhsT=wblk[:, kc, :], rhs=hT[:, kc, ch * 512:(ch + 1) * 512],
                start=(kc == 0), stop=(kc == 15)), reads=[b_hT, b_w], writes=[b_ps])
        evac(ch, ps, b_ps)


def outproj_phase(P, tag, xin, xout, base, yscr, Wlist, combine):
    A = Pool_(P, f"o{tag}")
    nW = len(Wlist)
    Wsb = [A.sb([128, 16, D], BF16, f"W{i}") for i in range(nW)]
    kW = P.key(f"oW", group=True)
    b_wc = [[Buf(f"W{tag}_{w}_{c}") for c in range(4)] for w in range(nW)]
    for w, ((w_t, b_w), wd) in enumerate(zip(Wsb, Wlist)):
        for c in range(4):
            P.dma("pool", w_t[:, :, c * 512:(c + 1) * 512],
                  wd[:, c * 512:(c + 1) * 512].rearrange("(kc p) n -> p kc n", p=128), kW, writes=[b_wc[w][c]])
    ys = [A.sb([128, 16, 128], BF16, f"ys{i}") for i in range(2)]
    xt = [A.sb([128, D], F32, f"xo{i}") for i in range(2)]
    tmp = [A.sb([128, 512], F32, f"tmp{i}") for i in range(2)]
    pss = [[A.ps([128, 512], F32, f"po{w}_{i}") for i in range(2)] for w in range(nW)]
    ky = [P.key(f"oy_{i}") for i in range(2)]
    kx = [P.key(f"ox_{i}") for i in range(2)]
    cnt = 0
    for tt in range(NT):
        i = tt % 2
        y_t, b_y = ys[i]
        x_t, b_x = xt[i]
        r0 = base + tt * 128
        P.dma("sp", y_t[:], yscr.rearrange("(kc p) t -> p kc t", p=128)[:, :, tt * 128:(tt + 1) * 128], ky[i],
              reads=[P.dram_buf(yscr, 0)], writes=[b_y])
        P.dma("sp", x_t[:], xin[r0:r0 + 128, :], kx[i], reads=[P.dram_buf(xin, r0)], writes=[b_x])
        for c in range(4):
            cur = []
            for w in range(nW):
                ps, b_ps = pss[w][cnt % 2]
                w_t, b_w = Wsb[w]
                for kc in range(16):
                    P.emit("pe", lambda e, kc=kc, c=c, ps=ps, w_t=w_t, y_t=y_t: e.matmul(
                        out=ps[:, :], lhsT=y_t[:, kc, :], rhs=w_t[:, kc, c * 512:(c + 1) * 512],
                        start=(kc == 0), stop=(kc == 15)), reads=[b_y, b_wc[w][c]], writes=[b_ps])
                cur.append((ps, b_ps))
            combine(P, cur, x_t, b_x, c, tmp[cnt % 2])
            cnt += 1
        P.out_ops.append(P.dma("sp", xout[r0:r0 + 128, :], x_t[:], kx[i], reads=[b_x],
                               writes=[P.dram_buf(xout, r0)]))
    A.close()


LN16 = 2.772588722239781
STOP = [0]


def mixer_a_stage(P, xin, xout, NSEQ, W, yscr_all, GELU_NATIVE=False):
    nc = P.nc
    u = P.uid()
    w_in = W["w_in"]
    def seq_body(sq):
        tag = f"a{u}s{sq}"
        base = sq * TS
        yscr = yscr_all[sq]
        O = Pool_(P, f"O{tag}")
        hT, b_hT = O.sb([128, 16, TS], BF16, "hT")
        ident, b_id = O.sb([128, 128], BF16, "ident")
        Gp = Pool_(P, f"G{tag}")
        gbc, b_g = Gp.sb([128, D], F32, "gbc")
        kc_ = P.key(f"c", group=True)
        kcp = P.key(f"cp", group=True)
        P.dma("sp", gbc[:], W["norm"].partition_broadcast(128), P.key(f"g"), writes=[b_g])
        make_ident(P, ident, b_id)
        build_hT(P, tag, xin, base, hT, b_hT, gbc, b_g, ident, b_id)
        barrier(P)
        Gp.close()
        if STOP[0] == 1:
            O.close()
            return
        wq = [O.sb([128, 16, 128], BF16, f"wq{i}") for i in range(3)]
        kwq = [P.key(f"wq_{i}") for i in range(3)]
        wqc = {"n": 0}

        def load_wblk(c0, width=128):
            i = wqc["n"] % 3
            wqc["n"] += 1
            t, b = wq[i]
            P.dma("pool", t[:, :, 0:width], w_in[:, c0:c0 + width].rearrange("(kc p) n -> p kc n", p=128),
                  kwq[i], writes=[b])
            return t, b

        def lru_phase():
            L = Pool_(P, f"L{tag}")
            sets = []
            for i_ in range(2):
                sets.append(L.sb([128, TS + 4], F32, f"xpad{i_}") + L.sb([128, TS], F32, f"xc{i_}") + L.sb([128, TS], BF16, f"xcb{i_}")
                            + L.sb([128, TS], F32, f"t1_{i_}") + L.sb([128, TS], F32, f"t2_{i_}"))
            t3, b_t3 = L.sb([128, TS], F32, "t3")
            hh, b_hh = L.sb([128, TS], F32, "hh")
            gz, b_gz = L.sb([128, TS], F32, "gz")
            yb, b_yb = L.sb([128, TS], BF16, "yb")
            cw, b_cw = L.sb([128, 8, 4], F32, "cw")
            cb, b_cb = L.sb([128, 8], F32, "cb")
            ba, b_ba = L.sb([128, 8], F32, "ba")
            bx, b_bx = L.sb([128, 8], F32, "bx")
            lam, b_lam = L.sb([128, 8], F32, "lam")
            cneg, b_cneg = L.sb([128, 8], F32, "cneg")
            cneg2, b_cneg2 = L.sb([128, 8], F32, "cneg2")
            waT, b_wa = L.sb([128, 8, 128], BF16, "waT")
            wxT, b_wx = L.sb([128, 8, 128], BF16, "wxT")
            pss = [L.ps([128, 512], F32, f"pl{i}") for i in range(4)]
            kyb = P.key(f"yb")
            b_cwj = [Buf(f"cwj{j}") for j in range(4)]
            for j in range(4):
                P.dma("sp", cw[:, :, j], W["lru_conv_w"][j, :].rearrange("(n f) -> f n", f=128), kc_,
                      writes=[b_cwj[j]], allow_slow_non_contiguous=True)
            load_cols(P, cb[:], W["lru_conv_b"], 8, kc_, b_cb)
            load_cols(P, ba[:], W["lru_b_a"], 8, kc_, b_ba)
            load_cols(P, bx[:], W["lru_b_x"], 8, kc_, b_bx)
            load_cols(P, lam[:], W["lru_lam"], 8, kc_, b_lam)
            P.dma("pool", waT[:], W["lru_w_a"].rearrange("n i j -> i n j"), kcp, writes=[b_wa])
            P.dma("pool", wxT[:], W["lru_w_x"].rearrange("n i j -> i n j"), kcp, writes=[b_wx])
            P.emit("act", lambda e: e.activation(out=cneg[:], in_=lam[:], func=AF.Exp, scale=-1.0), reads=[b_lam], writes=[b_cneg])
            P.emit("act", lambda e: e.activation(out=cneg[:], in_=cneg[:], func=AF.Ln, bias=1.0, scale=1.0), reads=[b_cneg], writes=[b_cneg])
            P.emit("dve", lambda e: e.tensor_scalar(out=cneg2[:], in0=cneg[:], scalar1=-16.0, scalar2=None, op0=ALU.mult), reads=[b_cneg], writes=[b_cneg2])
            P.emit("dve", lambda e: e.tensor_scalar(out=cneg[:], in0=cneg[:], scalar1=-8.0, scalar2=None, op0=ALU.mult), reads=[b_cneg, b_cneg2], writes=[b_cneg])
            for st_ in sets:
                P.emit("pool", lambda e, xp=st_[0]: e.memset(xp[:, 0:3], 0.0), writes=[st_[1]])

            def lru_front(n, xpad, b_xpad, xc, b_xc, xcb, b_xcb, t1, b_t1, t2, b_t2):
                wx_t, b_wxb = load_wblk(n * 128)

                def ev_x(ch, ps, b_ps):
                    P.emit("act", lambda e, ch=ch, ps=ps: e.copy(out=xpad[:, 3 + ch * 512:3 + (ch + 1) * 512], in_=ps[:, :]),
                           reads=[b_ps], writes=[b_xpad])
                proj_fm(P, hT, b_hT, wx_t, b_wxb, pss[0:2], ev_x)
                conv4(P, xpad, b_xpad, cw[:, n, :], cb[:, n:n + 1], xc[:], b_xc, b_cwj + [b_cb])
                P.emit("pool", lambda e: e.tensor_copy(out=xcb[:], in_=xc[:]), reads=[b_xc], writes=[b_xcb])
                for ch in range(4):
                    pr, b_pr = pss[ch % 2]
                    pi, b_pi = pss[2 + ch % 2]
                    P.emit("pe", lambda e, n=n, ch=ch, pr=pr: e.matmul(out=pr[:, :], lhsT=waT[:, n, :], rhs=xcb[:, ch * 512:(ch + 1) * 512],
                                                                        start=True, stop=True), reads=[b_wa, b_xcb], writes=[b_pr])
                    P.emit("pe", lambda e, n=n, ch=ch, pi=pi: e.matmul(out=pi[:, :], lhsT=wxT[:, n, :], rhs=xcb[:, ch * 512:(ch + 1) * 512],
                                                                        start=True, stop=True), reads=[b_wx, b_xcb], writes=[b_pi])
                    P.emit("act", lambda e, n=n, ch=ch, pr=pr: e.activation(out=t1[:, ch * 512:(ch + 1) * 512], in_=pr[:, :], func=AF.Sigmoid,
                                                                             bias=ba[:, n:n + 1], scale=1.0), reads=[b_pr, b_ba], writes=[b_t1])
                    P.emit("act", lambda e, n=n, ch=ch, pi=pi: e.activation(out=t2[:, ch * 512:(ch + 1) * 512], in_=pi[:, :], func=AF.Sigmoid,
                                                                             bias=bx[:, n:n + 1], scale=1.0), reads=[b_pi, b_bx], writes=[b_t2])

            def lru_back(n, xpad, b_xpad, xc, b_xc, xcb, b_xcb, t1, b_t1, t2, b_t2):
                wz_t, b_wzb = load_wblk(1024 + n * 128)
                P.emit("act", lambda e, n=n: e.activation(out=t3[:], in_=t1[:], func=AF.Exp, scale=cneg2[:, n:n + 1]), reads=[b_t1, b_cneg2], writes=[b_t3])
                P.emit("act", lambda e, n=n: e.activation(out=t1[:], in_=t1[:], func=AF.Exp, scale=cneg[:, n:n + 1]), reads=[b_t1, b_cneg, b_t3], writes=[b_t1])
                P.emit("act", lambda e: e.activation(out=t3[:], in_=t3[:], func=AF.Sqrt, bias=1.0, scale=-1.0), reads=[b_t3], writes=[b_t3])
                P.emit("dve", lambda e: e.tensor_tensor(out=t2[:], in0=t2[:], in1=xc[:], op=ALU.mult), reads=[b_t2, b_xc], writes=[b_t2])
                P.emit("dve", lambda e: e.tensor_tensor(out=t2[:], in0=t2[:], in1=t3[:], op=ALU.mult), reads=[b_t2, b_t3], writes=[b_t2])
                P.emit("dve", lambda e: e.tensor_tensor_scan(out=hh[:], data0=t1[:], data1=t2[:], initial=0.0, op0=ALU.mult, op1=ALU.add),
                       reads=[b_t1, b_t2], writes=[b_hh])

                def ev_z(ch, ps, b_ps):
                    sl = slice(ch * 512, (ch + 1) * 512)
                    if GELU_NATIVE:
                        P.emit("act", lambda e, ps=ps, sl=sl: e.activation(out=gz[:, sl], in_=ps[:, :], func=AF.Gelu_apprx_tanh),
                               reads=[b_ps], writes=[b_gz])
                    else:
                        P.emit("act", lambda e, ps=ps, sl=sl: e.activation(out=gz[:, sl], in_=ps[:, :], func=AF.Square), reads=[b_ps], writes=[b_gz])
                        P.emit("dve", lambda e, sl=sl: e.tensor_scalar(out=gz[:, sl], in0=gz[:, sl], scalar1=0.044715, scalar2=1.0, op0=ALU.mult, op1=ALU.add),
                               reads=[b_gz], writes=[b_gz])
                        P.emit("dve", lambda e, ps=ps, sl=sl: e.tensor_tensor(out=gz[:, sl], in0=ps[:, :], in1=gz[:, sl], op=ALU.mult),
                               reads=[b_gz, b_ps], writes=[b_gz])
                        P.emit("act", lambda e, sl=sl: e.activation(out=gz[:, sl], in_=gz[:, sl], func=AF.Sigmoid, scale=GELU_K), reads=[b_gz], writes=[b_gz])
                        P.emit("dve", lambda e, ps=ps, sl=sl: e.tensor_tensor(out=gz[:, sl], in0=ps[:, :], in1=gz[:, sl], op=ALU.mult),
                               reads=[b_gz, b_ps], writes=[b_gz])
                    P.emit("dve", lambda e, sl=sl: e.tensor_tensor(out=yb[:, sl], in0=hh[:, sl], in1=gz[:, sl], op=ALU.mult),
                           reads=[b_gz, b_hh], writes=[b_yb])
                proj_fm(P, hT, b_hT, wz_t, b_wzb, pss[2:4], ev_z)
                P.dma("sp", yscr[n * 128:(n + 1) * 128, :], yb[:], kyb, reads=[b_yb], writes=[P.dram_buf(yscr, 0)])
            lru_front(0, *sets[0])
            for n in range(8):
                if n + 1 < 8:
                    lru_front(n + 1, *sets[(n + 1) % 2])
                lru_back(n, *sets[n % 2])
            barrier(P)
            L.close()

        lru_phase()
        if STOP[0] == 2:
            O.close()
            return
        def mlstm_phase():
            M = Pool_(P, f"M{tag}")
            xpad, b_xpad = M.sb([128, TS + 4], F32, "xpad")
            xc, b_xc = M.sb([128, TS], F32, "xc")
            qT, b_qT = M.sb([128, 2, TS], BF16, "qT")
            kT, b_kT = M.sb([128, 2, TS], BF16, "kT")
            vext, b_v = M.sb([128, NT, 258], BF16, "vext")
            og, b_og = M.sb([128, NT, 256], BF16, "og")
            Fb, b_Fb = M.sb([128, TS], F32, "Fb")
            iT, b_iT = M.sb([4, TS], F32, "iT")
            fT, b_fT = M.sb([4, TS], F32, "fT")
            ones4, b_ones4 = M.sb([4, 1], F32, "ones4")
            biasT, b_bT = M.sb([128, NT, 4], F32, "biasT")
            wgi, b_wgi = M.sb([128, 16, 4], BF16, "wgi")
            wgf, b_wgf = M.sb([128, 16, 4], BF16, "wgf")
            ibias, b_ib = M.sb([4, 1], F32, "ibias")
            fbias, b_fb = M.sb([4, 1], F32, "fbias")
            selm, b_sel = M.sb([4, 4, 128], F32, "selm")
            id4, b_id4 = M.sb([4, 4], F32, "id4")
            tri, b_tri = M.sb([128, 128], F32, "tri")
            mcw, b_mcw = M.sb([128, 16, 4], F32, "mcw")
            mcb, b_mcb = M.sb([128, 16], F32, "mcb")
            mg, b_mg = M.sb([128, 1024], F32, "mg")
            dT = [M.sb([128, 512], F32, f"dT{i}") for i in range(2)]
            pT = [M.sb([128, 512], BF16, f"pT{i}") for i in range(2)]
            accS, _ = M.sb([128, 4, 258], F32, "accS")
            b_accS = [Buf(f"accS{q}") for q in range(4)]
            hm, b_hm = M.sb([128, 256], F32, "hm")
            ybh, b_ybh = M.sb([128, 256], BF16, "ybh")
            yTh, b_yTh = M.sb([128, 2, TS], BF16, "yTh")
            sml, b_sml = M.sb([128, 8], F32, "sml")
            epsc, b_epsc = M.sb([128, 1], F32, "epsc")
            P.emit("pool", lambda e: e.memset(epsc[:], EPS), writes=[b_epsc])
            wv = [M.sb([128, 16, 256], BF16, f"wv{i}") for i in range(2)]
            kwv = [P.key(f"wv_{i}") for i in range(2)]
            kyT = P.key(f"yT")
            kc2 = P.key(f"c2", group=True)
            kcp2 = P.key(f"cp2", group=True)
            psS = [M.ps([128, 512], F32, f"pS{i}") for i in range(2)]
            psA = [M.ps([128, 512], F32, f"pA{i}") for i in range(4)]
            psP = [M.ps([128, 512], F32, f"pP{i}") for i in range(2)]
            psB, b_psB = psP[0]
            psTt = psP[1][0][:, :].bitcast(BF16).rearrange("p (a b) -> p a b", a=8)
            b_psTt = psP[1][1]

            b_mcwj = [Buf(f"mcwj{j}") for j in range(4)]
            for j in range(4):
                P.dma("sp", mcw[:, :, j], W["m_conv_w"][j, :].rearrange("(n f) -> f n", f=128), kc2,
                      writes=[b_mcwj[j]], allow_slow_non_contiguous=True)
            load_cols(P, mcb[:], W["m_conv_b"], 16, kc2, b_mcb)
            P.dma("sp", mg[:], W["m_head_g"].partition_broadcast(128), kc2, writes=[b_mg])
            P.dma("sp", ibias[:], W["m_i_bias"].rearrange("o h -> h o"), kc2, writes=[b_ib], allow_slow_non_contiguous=True)
            P.dma("sp", fbias[:], W["m_f_bias"].rearrange("o h -> h o"), kc2, writes=[b_fb], allow_slow_non_contiguous=True)
            P.dma("pool", wgi[:], w_in[:, 6144:6148].rearrange("(kc p) n -> p kc n", p=128), kcp2, writes=[b_wgi])
            P.dma("pool", wgf[:], w_in[:, 6148:6152].rearrange("(kc p) n -> p kc n", p=128), kcp2, writes=[b_wgf])
            P.emit("pool", lambda e: e.memset(xpad[:, 0:3], 0.0), writes=[b_xpad])
            P.emit("pool", lambda e: e.memset(ones4[:], 1.0), writes=[b_ones4])
            P.emit("pool", lambda e: e.memset(vext[:, :, 256:258], 1.0), writes=[b_v])
            P.emit("pool", lambda e: e.memset(id4[:], 1.0), writes=[b_id4])
            P.emit("pool", lambda e: e.affine_select(out=id4[:], in_=id4[:], pattern=[[-1, 4]], compare_op=ALU.is_equal, fill=0.0,
                                                      base=0, channel_multiplier=1), reads=[b_id4], writes=[b_id4])
            P.emit("pool", lambda e: e.memset(selm[:], -1.0), writes=[b_sel])
            P.emit("pool", lambda e: e.affine_select(out=selm[:], in_=selm[:], pattern=[[-1, 4], [0, 128]], compare_op=ALU.is_equal, fill=0.0,
                                                      base=0, channel_multiplier=1), reads=[b_sel], writes=[b_sel])
            P.emit("pool", lambda e: e.memset(tri[:], 1.0), writes=[b_tri])
            P.emit("pool", lambda e: e.affine_select(out=tri[:], in_=tri[:], pattern=[[1, 128]], compare_op=ALU.is_ge, fill=0.0,
                                                      base=0, channel_multiplier=-1), reads=[b_tri], writes=[b_tri])
            for ch in range(4):
                sl = slice(ch * 512, (ch + 1) * 512)
                pi_, b_pi = psP[0]
                pf_, b_pf = psP[1]
                for kc in range(16):
                    P.emit("pe", lambda e, kc=kc, sl=sl, pi_=pi_: e.matmul(out=pi_[0:4, :], lhsT=wgi[:, kc, :], rhs=hT[:, kc, sl],
                                                                            start=(kc == 0), stop=(kc == 15)), reads=[b_hT, b_wgi], writes=[b_pi])
                for kc in range(16):
                    P.emit("pe", lambda e, kc=kc, sl=sl, pf_=pf_: e.matmul(out=pf_[0:4, :], lhsT=wgf[:, kc, :], rhs=hT[:, kc, sl],
                                                                            start=(kc == 0), stop=(kc == 15)), reads=[b_hT, b_wgf], writes=[b_pf])
                P.emit("act", lambda e, sl=sl, pi_=pi_: e.activation(out=iT[:, sl], in_=pi_[0:4, :], func=AF.Identity, bias=ibias[:, 0:1], scale=1.0),
                       reads=[b_pi, b_ib], writes=[b_iT])
                P.emit("act", lambda e, sl=sl, pf_=pf_: e.activation(out=fT[:, sl], in_=pf_[0:4, :], func=AF.Identity, bias=fbias[:, 0:1], scale=1.0),
                       reads=[b_pf, b_fb], writes=[b_fT])
            P.emit("act", lambda e: e.activation(out=fT[:], in_=fT[:], func=AF.Exp, scale=-1.0), reads=[b_fT], writes=[b_fT])
            P.emit("act", lambda e: e.activation(out=fT[:], in_=fT[:], func=AF.Ln, bias=1.0, scale=1.0), reads=[b_fT], writes=[b_fT])
            P.emit("dve", lambda e: e.tensor_tensor_scan(out=fT[:], data0=ones4[:, 0:1].to_broadcast([4, TS]), data1=fT[:], initial=0.0, op0=ALU.mult, op1=ALU.add),
                   reads=[b_fT, b_ones4], writes=[b_fT])
            P.emit("dve", lambda e: e.tensor_tensor(out=iT[:], in0=iT[:], in1=fT[:], op=ALU.add), reads=[b_iT, b_fT], writes=[b_iT])
            for jt in range(NT):
                P.emit("pe", lambda e, jt=jt: e.matmul(out=psB[:, jt * 4:(jt + 1) * 4], lhsT=iT[:, jt * 128:(jt + 1) * 128], rhs=id4[:],
                                                        start=True, stop=True), reads=[b_iT, b_id4], writes=[b_psB])
            P.emit("dve", lambda e: e.tensor_scalar(out=biasT[:].rearrange("p a b -> p (a b)"), in0=psB[:, 0:64], scalar1=-LN16, scalar2=None, op0=ALU.add),
                   reads=[b_psB], writes=[b_bT])
            wvc = {"n": 0}

            def load_wv(c0):
                i = wvc["n"] % 2
                wvc["n"] += 1
                t, b = wv[i]
                P.dma("pool", t[:], w_in[:, c0:c0 + 256].rearrange("(kc p) n -> p kc n", p=128), kwv[i], writes=[b])
                return t, b

            blk = 0
            for hd in range(4):
                for (dstT, b_dst, c0, t0) in ((qT, b_qT, 2048, 0), (kT, b_kT, 3072, 8)):
                    for c in range(2):
                        wt, b_wt = load_wblk(c0 + hd * 256 + c * 128)

                        def ev_q(ch, ps, b_ps):
                            P.emit("act", lambda e, ch=ch, ps=ps: e.copy(out=xpad[:, 3 + ch * 512:3 + (ch + 1) * 512], in_=ps[:, :]),
                                   reads=[b_ps], writes=[b_xpad])
                        proj_fm(P, hT, b_hT, wt, b_wt, psP, ev_q)
                        tcol = t0 + hd * 2 + c
                        conv4(P, xpad, b_xpad, mcw[:, tcol, :], mcb[:, tcol:tcol + 1], xc[:], b_xc, b_mcwj + [b_mcb])
                        P.emit("act", lambda e, dstT=dstT, c=c: e.activation(out=dstT[:, c, :], in_=xc[:], func=AF.Silu),
                               reads=[b_xc], writes=[b_dst])
                wv_t, b_wvt = load_wv(4096 + hd * 256)
                wo_t, b_wot = load_wv(5120 + hd * 256)
                for tt in range(NT):
                    for (w_t, b_w, isv) in ((wv_t, b_wvt, True), (wo_t, b_wot, False)):
                        ps, b_ps = psP[blk % 2]
                        blk += 1
                        for kc in range(16):
                            P.emit("pe", lambda e, kc=kc, tt=tt, ps=ps, w_t=w_t: e.matmul(
                                out=ps[:, 0:256], lhsT=hT[:, kc, tt * 128:(tt + 1) * 128], rhs=w_t[:, kc, :],
                                start=(kc == 0), stop=(kc == 15)), reads=[b_hT, b_w], writes=[b_ps])
                        if isv:
                            P.emit("dve", lambda e, tt=tt, ps=ps: e.tensor_copy(out=vext[:, tt, 0:256], in_=ps[:, 0:256]),
                                   reads=[b_ps], writes=[b_v])
                        else:
                            P.emit("act", lambda e, tt=tt, ps=ps: e.activation(out=og[:, tt, :], in_=ps[:, 0:256], func=AF.Sigmoid),
                                   reads=[b_ps], writes=[b_og])
                for ch in range(4):
                    ps, b_ps = psP[blk % 2]
                    blk += 1
                    P.emit("pe", lambda e, ch=ch, ps=ps, hd=hd: e.matmul(out=ps[:, :], lhsT=selm[:, hd, :], rhs=fT[:, ch * 512:(ch + 1) * 512],
                                                                          start=True, stop=True), reads=[b_sel, b_fT], writes=[b_ps])
                    P.emit("act", lambda e, ch=ch, ps=ps: e.copy(out=Fb[:, ch * 512:(ch + 1) * 512], in_=ps[:, :]), reads=[b_ps], writes=[b_Fb])
                def epilogue(tt, acc, b_acc, tsl):
                    dd, rec, ssq, rstd = sml[:, 0:1], sml[:, 1:2], sml[:, 2:3], sml[:, 3:4]
                    P.emit("dve", lambda e, acc=acc, dd=dd: e.tensor_scalar(out=dd, in0=acc[:, 256:257], scalar1=-1.0, scalar2=None, op0=ALU.mult),
                           reads=[b_acc], writes=[b_sml])
                    P.emit("dve", lambda e, acc=acc, dd=dd: e.tensor_tensor(out=dd, in0=dd, in1=acc[:, 256:257], op=ALU.max),
                           reads=[b_acc, b_sml], writes=[b_sml])
                    P.emit("dve", lambda e, dd=dd: e.tensor_scalar(out=dd, in0=dd, scalar1=1.0, scalar2=None, op0=ALU.max),
                           reads=[b_sml], writes=[b_sml])
                    P.emit("dve", lambda e, dd=dd, rec=rec: e.reciprocal(out=rec, in_=dd), reads=[b_sml], writes=[b_sml])
                    P.emit("dve", lambda e, acc=acc, rec=rec, tt=tt: e.scalar_tensor_tensor(out=hm[:], in0=acc[:, 0:256], scalar=rec, in1=og[:, tt, :],
                                                                                             op0=ALU.mult, op1=ALU.mult),
                           reads=[b_acc, b_sml, b_og], writes=[b_hm])
                    P.emit("dve", lambda e, ssq=ssq: e.scalar_tensor_tensor(out=ybh[:], in0=hm[:], scalar=1.0, in1=hm[:], op0=ALU.mult, op1=ALU.mult, accum_out=ssq),
                           reads=[b_hm], writes=[b_ybh, b_sml])
                    P.emit("act", lambda e, ssq=ssq, rstd=rstd: e.activation(out=rstd, in_=ssq, func=AF.Ln, bias=epsc[:, 0:1], scale=1.0 / 256), reads=[b_sml, b_epsc], writes=[b_sml])
                    P.emit("act", lambda e, rstd=rstd: e.activation(out=rstd, in_=rstd, func=AF.Exp, scale=-0.5), reads=[b_sml], writes=[b_sml])
                    P.emit("dve", lambda e, rstd=rstd, hd=hd: e.scalar_tensor_tensor(out=ybh[:], in0=hm[:], scalar=rstd, in1=mg[:, hd * 256:(hd + 1) * 256],
                                                                                      op0=ALU.mult, op1=ALU.mult),
                           reads=[b_hm, b_sml, b_mg], writes=[b_ybh])
                    for c in range(2):
                        P.emit("pe", lambda e, c=c: e.transpose(out=psTt[:, c, :], in_=ybh[:, c * 128:(c + 1) * 128], identity=ident[:]),
                               reads=[b_ybh, b_id], writes=[b_psTt])
                    P.emit("act", lambda e, tsl=tsl: e.copy(out=yTh[:, :, tsl], in_=psTt[:, 0:2, :]), reads=[b_psTt], writes=[b_yTh])
                cntS = 0
                pend = []
                for qg in range(4):
                    for jt in range(4 * qg + 4):
                        c0 = max(0, jt - 4 * qg)
                        ncol = (4 - c0) * 128
                        t0 = (4 * qg + c0) * 128
                        tsl = slice(t0, t0 + ncol)
                        jsl = slice(jt * 128, (jt + 1) * 128)
                        i2 = cntS % 2
                        cntS += 1
                        pS, b_pS = psS[i2]
                        d_t, b_d = dT[i2]
                        p_t, b_p = pT[i2]
                        for c in range(2):
                            P.emit("pe", lambda e, c=c, pS=pS, jsl=jsl, tsl=tsl, ncol=ncol: e.matmul(out=pS[:, 0:ncol], lhsT=kT[:, c, jsl], rhs=qT[:, c, tsl],
                                                                                                     start=(c == 0), stop=(c == 1)),
                                   reads=[b_kT, b_qT], writes=[b_pS])
                        P.emit("act", lambda e, d_t=d_t, tsl=tsl, jt=jt, hd=hd, ncol=ncol: e.activation(out=d_t[:, 0:ncol], in_=Fb[:, tsl], func=AF.Exp,
                                                                                                        bias=biasT[:, jt, hd:hd + 1], scale=1.0),
                               reads=[b_Fb, b_bT], writes=[b_d])
                        if jt >= 4 * qg:
                            P.emit("pool", lambda e, d_t=d_t: e.tensor_tensor(out=d_t[:, 0:128], in0=d_t[:, 0:128], in1=tri[:], op=ALU.mult),
                                   reads=[b_d, b_tri], writes=[b_d])
                        P.emit("dve", lambda e, d_t=d_t, p_t=p_t, pS=pS, ncol=ncol: e.tensor_tensor(out=p_t[:, 0:ncol], in0=pS[:, 0:ncol], in1=d_t[:, 0:ncol], op=ALU.mult),
                               reads=[b_pS, b_d], writes=[b_p])
                        for tq in range(c0, 4):
                            acc, b_acc = psA[tq]
                            tt = 4 * qg + tq
                            P.emit("pe", lambda e, p_t=p_t, jt=jt, acc=acc, tt=tt, tq=tq, c0=c0: e.matmul(
                                out=acc[:, 0:258], lhsT=p_t[:, (tq - c0) * 128:(tq - c0 + 1) * 128], rhs=vext[:, jt, :],
                                start=(jt == 0), stop=(jt == tt)), reads=[b_p, b_v], writes=[b_acc])
                        if pend:
                            epilogue(*pend.pop(0))
                    for tq in range(4):
                        acc, b_acc = psA[tq]
                        P.emit("act", lambda e, acc=acc, tq=tq: e.copy(out=accS[:, tq, :], in_=acc[:, 0:258]), reads=[b_acc], writes=[b_accS[tq]])
                    for tq in range(4):
                        tt = 4 * qg + tq
                        pend.append((tt, accS[:, tq, :], b_accS[tq], slice(tt * 128, (tt + 1) * 128)))
                    if qg == 3:
                        while pend:
                            epilogue(*pend.pop(0))
                for c in range(2):
                    r0 = 1024 + hd * 256 + c * 128
                    P.dma("sp", yscr[r0:r0 + 128, :], yTh[:, c, :], kyT, reads=[b_yTh], writes=[P.dram_buf(yscr, 0)])
            barrier(P)
            M.close()

        if STOP[0] != 3:
            mlstm_phase()
        O.close()
        if STOP[0] == 4:
            return
        def comb(P, cur, x_t, b_x, c, tmp):
            ps, b_ps = cur[0]
            P.emit("dve", lambda e, ps=ps, c=c, x_t=x_t: e.tensor_tensor(out=x_t[:, c * 512:(c + 1) * 512], in0=ps[:, :],
                                                                          in1=x_t[:, c * 512:(c + 1) * 512], op=ALU.add),
                   reads=[b_ps, b_x], writes=[b_x])
        outproj_phase(P, tag, xin, xout, base, yscr, [W["w_out"]], comb)
        barrier(P)

    for sq in range(NSEQ):
        seq_body(sq)


PI = 3.141592653589793
MAGIC = 12582912.0


def s5_stage(P, xin, xout, NSEQ, W, yscr_all, tabscr=None):
    nc = P.nc
    u = P.uid()
    w_in = W["w_in"]

    def seq_body(sq):
        tag = f"s{u}q{sq}"
        base = sq * TS
        yscr = yscr_all[sq]
        O = Pool_(P, f"O{tag}")
        hT, b_hT = O.sb([128, 16, TS], BF16, "hT")
        ident, b_id = O.sb([128, 128], BF16, "ident")
        Gp = Pool_(P, f"G{tag}")
        gbc, b_g = Gp.sb([128, D], F32, "gbc")
        P.dma("sp", gbc[:], W["norm"].partition_broadcast(128), P.key(f"g"), writes=[b_g])
        make_ident(P, ident, b_id)
        build_hT(P, tag, xin, base, hT, b_hT, gbc, b_g, ident, b_id)
        barrier(P)
        Gp.close()

        def main_phase():
            M = Pool_(P, f"M{tag}")
            wq = [M.sb([128, 16, 128], BF16, f"wq{i}") for i in range(3)]
            kwq = [P.key(f"wq_{i}") for i in range(3)]
            rhoB, b_rhoB = M.sb([128, 64], F32, "rhoB")
            thB, b_thB = M.sb([128, 64], F32, "thB")
            bbr, b_bbr = M.sb([128, 64, 16], F32, "bbr")
            bbi, b_bbi = M.sb([128, 64, 16], F32, "bbi")
            dsk, b_dsk = M.sb([128, 16], F32, "dsk")
            iota, b_iota = M.sb([128, TS], F32, "iota")
            kc_ = P.key(f"c", group=True)
            psA = [M.ps([128, 512], F32, f"pa{i}") for i in range(4)]
            psY = [M.ps([128, 512], F32, f"py{i}") for i in range(4)]
            psTb = psA[3][0][:, :].bitcast(BF16).rearrange("p (a b) -> p a b", a=8)
            b_psTb = psA[3][1]

            def setup():
                S = Pool_(P, f"S{tag}")
                are, b_are = S.sb([128, 64], F32, "are")
                aim, b_aim = S.sb([128, 64], F32, "aim")
                ls, b_ls = S.sb([128, 1], F32, "ls")
                rho, b_rho = S.sb([128, 64], F32, "rho")
                th, b_th = S.sb([128, 64], F32, "th")
                tmp, b_tmp = S.sb([128, 64], F32, "tmp")
                sn, b_sn = S.sb([128, 64], F32, "sn")
                cs, b_cs = S.sb([128, 64], F32, "cs")
                den, b_den = S.sb([128, 64], F32, "den")
                fr, b_fr = S.sb([128, 64], F32, "fr")
                fi, b_fi = S.sb([128, 64], F32, "fi")
                frB, b_frB = S.sb([128, 64], F32, "frB")
                fiB, b_fiB = S.sb([128, 64], F32, "fiB")
                E0, b_E0 = S.sb([128, 64], F32, "E0")
                E1, b_E1 = S.sb([128, 64], F32, "E1")
                bre, b_bre = S.sb([128, 64, 16], F32, "bre")
                bim, b_bim = S.sb([128, 64, 16], F32, "bim")
                t16, b_t16 = S.sb([128, 64, 16], F32, "t16")
                P.dma("sp", are[:], W["a_re"], kc_, writes=[b_are])
                P.dma("sp", aim[:], W["a_im"], kc_, writes=[b_aim])
                P.dma("sp", ls[:], W["log_step"].rearrange("(g o) -> g o", o=1), kc_, writes=[b_ls])
                b_bre2, b_bim2 = Buf("bre2"), Buf("bim2")
                for two in range(2):
                    P.dma("sp", bre[two * 64:(two + 1) * 64, :, :], W["b_re"].rearrange("(gp two) p c -> two p gp c", two=2)[two], kc_,
                          writes=[(b_bre, b_bre2)[two]])
                    P.dma("sp", bim[two * 64:(two + 1) * 64, :, :], W["b_im"].rearrange("(gp two) p c -> two p gp c", two=2)[two], kc_,
                          writes=[(b_bim, b_bim2)[two]])
                load_cols(P, dsk[:], W["d"], 16, kc_, b_dsk)
                P.emit("pool", lambda e: e.iota(out=iota[:], pattern=[[1, TS]], base=0, channel_multiplier=0,
                                                allow_small_or_imprecise_dtypes=True), writes=[b_iota])
                for (E, b_E, bs) in ((E0, b_E0, 0), (E1, b_E1, -1)):
                    P.emit("pool", lambda e, E=E: e.memset(E[:], 1.0), writes=[b_E])
                    P.emit("pool", lambda e, E=E, bs=bs: e.affine_select(out=E[:], in_=E[:], pattern=[[-2, 64]], compare_op=ALU.is_equal,
                                                                          fill=0.0, base=bs, channel_multiplier=1), reads=[b_E], writes=[b_E])
                dv = lambda fn, r, w: P.emit("dve", fn, reads=r, writes=w)
                ac = lambda fn, r, w: P.emit("act", fn, reads=r, writes=w)
                ac(lambda e: e.activation(out=ls[:], in_=ls[:], func=AF.Exp), [b_ls], [b_ls])
                dv(lambda e: e.tensor_scalar(out=rho[:], in0=are[:], scalar1=ls[:, 0:1], scalar2=None, op0=ALU.mult), [b_are, b_ls], [b_rho])
                ac(lambda e: e.activation(out=rho[:], in_=rho[:], func=AF.Exp), [b_rho], [b_rho])
                dv(lambda e: e.tensor_scalar(out=th[:], in0=aim[:], scalar1=ls[:, 0:1], scalar2=None, op0=ALU.mult), [b_aim, b_ls], [b_th])
                dv(lambda e: e.tensor_scalar(out=tmp[:], in0=th[:], scalar1=1.0 / (2 * PI), scalar2=MAGIC, op0=ALU.mult, op1=ALU.add), [b_th], [b_tmp])
                dv(lambda e: e.tensor_scalar(out=tmp[:], in0=tmp[:], scalar1=-MAGIC, scalar2=-2 * PI, op0=ALU.add, op1=ALU.mult), [b_tmp], [b_tmp])
                dv(lambda e: e.tensor_tensor(out=tmp[:], in0=tmp[:], in1=th[:], op=ALU.add), [b_tmp, b_th], [b_tmp])
                dv(lambda e: e.tensor_scalar(out=tmp[:], in0=tmp[:], scalar1=-PI, scalar2=PI, op0=ALU.max, op1=ALU.min), [b_tmp], [b_tmp])
                ac(lambda e: e.activation(out=sn[:], in_=tmp[:], func=AF.Sin), [b_tmp], [b_sn])
                ac(lambda e: e.activation(out=cs[:], in_=tmp[:], func=AF.Sin, scale=0.5), [b_tmp], [b_cs])
                dv(lambda e: e.tensor_tensor(out=cs[:], in0=cs[:], in1=cs[:], op=ALU.mult), [b_cs], [b_cs])
                dv(lambda e: e.tensor_scalar(out=cs[:], in0=cs[:], scalar1=-2.0, scalar2=1.0, op0=ALU.mult, op1=ALU.add), [b_cs], [b_cs])
                dv(lambda e: e.tensor_tensor(out=cs[:], in0=cs[:], in1=rho[:], op=ALU.mult), [b_cs, b_rho], [b_cs])
                dv(lambda e: e.tensor_scalar(out=cs[:], in0=cs[:], scalar1=-1.0, scalar2=None, op0=ALU.add), [b_cs], [b_cs])
                dv(lambda e: e.tensor_tensor(out=sn[:], in0=sn[:], in1=rho[:], op=ALU.mult), [b_sn, b_rho], [b_sn])
                dv(lambda e: e.tensor_tensor(out=den[:], in0=are[:], in1=are[:], op=ALU.mult), [b_are], [b_den])
                dv(lambda e: e.tensor_tensor(out=tmp[:], in0=aim[:], in1=aim[:], op=ALU.mult), [b_aim, b_cs], [b_tmp])
                dv(lambda e: e.tensor_tensor(out=den[:], in0=den[:], in1=tmp[:], op=ALU.add), [b_den, b_tmp], [b_den])
                dv(lambda e: e.reciprocal(out=den[:], in_=den[:]), [b_den], [b_den])
                dv(lambda e: e.tensor_tensor(out=fr[:], in0=cs[:], in1=are[:], op=ALU.mult), [b_cs, b_are], [b_fr])
                dv(lambda e: e.tensor_tensor(out=tmp[:], in0=sn[:], in1=aim[:], op=ALU.mult), [b_sn, b_aim, b_den], [b_tmp])
                dv(lambda e: e.tensor_tensor(out=fr[:], in0=fr[:], in1=tmp[:], op=ALU.add), [b_fr, b_tmp], [b_fr])
                dv(lambda e: e.tensor_tensor(out=fr[:], in0=fr[:], in1=den[:], op=ALU.mult), [b_fr, b_den], [b_fr])
                dv(lambda e: e.tensor_tensor(out=fi[:], in0=sn[:], in1=are[:], op=ALU.mult), [b_sn, b_are], [b_fi])
                dv(lambda e: e.tensor_tensor(out=tmp[:], in0=cs[:], in1=aim[:], op=ALU.mult), [b_cs, b_aim, b_fr], [b_tmp])
                dv(lambda e: e.tensor_tensor(out=fi[:], in0=fi[:], in1=tmp[:], op=ALU.subtract), [b_fi, b_tmp], [b_fi])
                dv(lambda e: e.tensor_tensor(out=fi[:], in0=fi[:], in1=den[:], op=ALU.mult), [b_fi, b_den], [b_fi])
                mev, b_mev = S.sb([128, 2], F32, "mev")
                l2, b_l2 = S.sb([128, 128], F32, "l2")
                dv(lambda e: e.reduce_sum(out=mev[:, 0:1], in_=E0[:], axis=AX.X), [b_E0], [b_mev])
                dv(lambda e: e.reduce_sum(out=mev[:, 1:2], in_=E1[:], axis=AX.X), [b_E1, b_mev], [b_mev])
                dv(lambda e: e.tensor_tensor(out=E0[:], in0=E0[:], in1=E1[:], op=ALU.add), [b_E0, b_E1, b_mev], [b_E0])
                for qi, (src, b_src, dst, b_dst) in enumerate(((rho, b_rho, rhoB, b_rhoB), (th, b_th, thB, b_thB),
                                                               (fr, b_fr, frB, b_frB), (fi, b_fi, fiB, b_fiB))):
                    ps, b_ps = psA[qi % 3]
                    for two in range(2):
                        dv(lambda e, src=src, two=two: e.tensor_scalar(out=l2[:, two * 64:(two + 1) * 64], in0=src[:], scalar1=mev[:, two:two + 1],
                                                                       scalar2=None, op0=ALU.mult), [b_src, b_mev, b_l2], [b_l2])
                    P.emit("pe", lambda e, ps=ps: e.matmul(out=ps[:, 0:64], lhsT=l2[:], rhs=E0[:], start=True, stop=True),
                           reads=[b_l2, b_E0], writes=[b_ps])
                    P.emit("act", lambda e, dst=dst, ps=ps: e.copy(out=dst[:], in_=ps[:, 0:64]), reads=[b_ps], writes=[b_dst])
                frb = frB[:].unsqueeze(2).to_broadcast([128, 64, 16])
                fib = fiB[:].unsqueeze(2).to_broadcast([128, 64, 16])
                dv(lambda e: e.tensor_tensor(out=bbr[:], in0=bre[:], in1=frb, op=ALU.mult), [b_bre, b_bre2, b_frB], [b_bbr])
                dv(lambda e: e.tensor_tensor(out=t16[:], in0=bim[:], in1=fib, op=ALU.mult), [b_bim, b_bim2, b_fiB], [b_t16])
                dv(lambda e: e.tensor_tensor(out=bbr[:], in0=bbr[:], in1=t16[:], op=ALU.subtract), [b_bbr, b_t16], [b_bbr])
                dv(lambda e: e.tensor_tensor(out=bbi[:], in0=bim[:], in1=frb, op=ALU.mult), [b_bim, b_bim2, b_frB], [b_bbi])
                dv(lambda e: e.tensor_tensor(out=t16[:], in0=bre[:], in1=fib, op=ALU.mult), [b_bre, b_bre2, b_fiB, b_bbr], [b_t16])
                dv(lambda e: e.tensor_tensor(out=bbi[:], in0=bbi[:], in1=t16[:], op=ALU.add), [b_bbi, b_t16], [b_bbi])
                barrier(P)
                S.close()

            if STOP[0] != 11:
                setup()
            if STOP[0] in (11, 12):
                barrier(P)
                M.close()
                return
            thq, b_thq = M.sb([128, 64], F32, "thq")
            P.emit("dve", lambda e: e.tensor_scalar(out=thq[:], in0=thB[:], scalar1=1.0 / (2 * PI), scalar2=None, op0=ALU.mult), reads=[b_thB], writes=[b_thq])

            Bf = [[M.sb([128, 4, 128], BF16, f"Bf{k}_{i}") for i in range(2)] for k in range(2)]
            BT = [[M.sb([128, 4, 128], BF16, f"BT{k}_{i}") for i in range(2)] for k in range(2)]
            CT = [[M.sb([128, 4, 128], BF16, f"CT{k}_{i}") for i in range(2)] for k in range(2)]
            Dg = [M.sb([128, 128], BF16, f"Dg{k}") for k in range(2)]
            Cn = [M.sb([128, 64], F32, f"Cn{i}") for i in range(2)]
            Cnb = [M.sb([128, 128], BF16, f"Cnb{i}") for i in range(2)]
            kcn = [P.key(f"cn_{i}") for i in range(2)]
            ubs = [M.sb([128, TS], BF16, f"ub{k}") for k in range(2)]
            nSs = [M.sb([128, TS], F32, f"nS{i}") for i in range(2)]
            nCs = [M.sb([128, TS], F32, f"nC{i}") for i in range(2)]
            wr, b_wr = M.sb([128, TS], F32, "wr")
            wi, b_wi = M.sb([128, TS], F32, "wi")
            xrs = [M.sb([128, TS], BF16, f"xr{i}") for i in range(2)]
            xis = [M.sb([128, TS], BF16, f"xi{i}") for i in range(2)]
            tas = [M.sb([128, 1024], F32, f"ta{i}") for i in range(2)]
            tbs = [M.sb([128, 1024], F32, f"tb{i}") for i in range(2)]
            b_wrs = [Buf(f"wr{c}") for c in range(4)]
            b_wis = [Buf(f"wi{c}") for c in range(4)]
            b_xrs = [[Buf(f"xr{i}_{h}") for h in range(2)] for i in range(2)]
            b_xis = [[Buf(f"xi{i}_{h}") for h in range(2)] for i in range(2)]
            yb, b_yb = M.sb([128, TS], BF16, "yb")
            kyb = P.key(f"yb")
            ktab = [[P.key(f"tab_{i}_{j}") for j in range(2)] for i in range(2)]
            wqc = {"n": 0}
            for k in range(2):
                for bf_, _b in Bf[k] + CT[k]:
                    P.emit("pool", lambda e, bf_=bf_: e.memset(bf_[:], 0.0), writes=[_b])

            def tables(gp):
                sl_ = gp % 2
                g1 = slice(gp, gp + 1)
                S_, b_S = nSs[sl_]
                C_, b_C = nCs[sl_]
                if tabscr is not None and sq > 0:
                    P.dma("sp", S_[:], tabscr[gp, 0], ktab[sl_][0], reads=[P.dram_buf(tabscr, gp * 2)], writes=[b_S])
                    P.dma("sp", C_[:], tabscr[gp, 1], ktab[sl_][1], reads=[P.dram_buf(tabscr, gp * 2 + 1)], writes=[b_C])
                    return
                P.emit("dve", lambda e, g1=g1, C_=C_: e.tensor_scalar(out=C_[:], in0=iota[:], scalar1=thq[:, g1], scalar2=MAGIC, op0=ALU.mult, op1=ALU.add),
                       reads=[b_iota, b_thq], writes=[b_C])
                P.emit("dve", lambda e, C_=C_: e.tensor_scalar(out=C_[:], in0=C_[:], scalar1=-MAGIC, scalar2=-2 * PI, op0=ALU.add, op1=ALU.mult),
                       reads=[b_C], writes=[b_C])
                P.emit("dve", lambda e, g1=g1, C_=C_: e.scalar_tensor_tensor(out=C_[:], in0=iota[:], scalar=thB[:, g1], in1=C_[:], op0=ALU.mult, op1=ALU.add),
                       reads=[b_iota, b_thB, b_C], writes=[b_C])
                P.emit("dve", lambda e, C_=C_, S_=S_: e.tensor_scalar(out=S_[:], in0=C_[:], scalar1=-PI, scalar2=PI, op0=ALU.max, op1=ALU.min),
                       reads=[b_C], writes=[b_S])
                P.emit("act", lambda e, C_=C_, S_=S_: e.activation(out=C_[:], in_=S_[:], func=AF.Sin, scale=0.5), reads=[b_S, b_C], writes=[b_C])
                P.emit("act", lambda e, C_=C_: e.activation(out=C_[:], in_=C_[:], func=AF.Square), reads=[b_C], writes=[b_C])
                P.emit("act", lambda e, S_=S_: e.activation(out=S_[:], in_=S_[:], func=AF.Sin), reads=[b_S, b_C], writes=[b_S])
                P.emit("act", lambda e, C_=C_: e.activation(out=C_[:], in_=C_[:], func=AF.Identity, bias=1.0, scale=-2.0),
                       reads=[b_C], writes=[b_C])
                if tabscr is not None and NSEQ > 1:
                    P.dma("sp", tabscr[gp, 0], S_[:], ktab[sl_][0], reads=[b_S], writes=[P.dram_buf(tabscr, gp * 2)])
                    P.dma("sp", tabscr[gp, 1], C_[:], ktab[sl_][1], reads=[b_C], writes=[P.dram_buf(tabscr, gp * 2 + 1)])

            def rotate(gp, r):
                sl_ = gp % 2
                k = (gp // 4) % 2
                g1 = slice(gp, gp + 1)
                nS, b_nS = nSs[sl_]
                nC, b_nC = nCs[sl_]
                xr, b_xr = xrs[sl_]
                xi, b_xi = xis[sl_]
                ub, b_ub = ubs[k]
                rho_bc = rhoB[:, g1].to_broadcast([128, TS])
                dv = lambda fn, r_, w_: P.emit("dve", fn, reads=r_, writes=w_)
                for pair in range(2):
                    cs = (2 * pair, 2 * pair + 1)
                    sls = [slice(ch * 512, (ch + 1) * 512) for ch in cs]
                    pRs = [psA[0], psA[2]]
                    pIs = [psA[1], psA[3]]
                    for j, ch in enumerate(cs):
                        pR, b_pR = pRs[j]
                        pI, b_pI = pIs[j]
                        sl = sls[j]
                        P.emit("pe", lambda e, r=r, sl=sl, pR=pR, k=k, ub=ub: e.matmul(out=pR[:, :], lhsT=BT[k][0][0][:, r, :], rhs=ub[:, sl], start=True, stop=True),
                               reads=[BT[k][0][1], b_ub], writes=[b_pR])
                        P.emit("pe", lambda e, r=r, sl=sl, pI=pI, k=k, ub=ub: e.matmul(out=pI[:, :], lhsT=BT[k][1][0][:, r, :], rhs=ub[:, sl], start=True, stop=True),
                               reads=[BT[k][1][1], b_ub], writes=[b_pI])
                    tq = [(tas[j][0][:, 0:512], tas[j][1], tbs[j][0][:, 0:512], tbs[j][1]) for j in range(2)]
                    for j in range(2):
                        dv(lambda e, sl=sls[j], pR=pRs[j][0]: e.tensor_tensor(out=wr[:, sl], in0=pR[:, :], in1=nC[:, sl], op=ALU.mult), [pRs[j][1], b_nC], [b_wrs[cs[j]]])
                    for j in range(2):
                        dv(lambda e, sl=sls[j], pI=pIs[j][0], ta=tq[j][0]: e.tensor_tensor(out=ta, in0=pI[:, :], in1=nS[:, sl], op=ALU.mult), [pIs[j][1], b_nS], [tq[j][1]])
                    for j in range(2):
                        dv(lambda e, sl=sls[j], ta=tq[j][0]: e.tensor_tensor(out=wr[:, sl], in0=wr[:, sl], in1=ta, op=ALU.add), [b_wrs[cs[j]], tq[j][1]], [b_wrs[cs[j]]])
                    for j in range(2):
                        dv(lambda e, sl=sls[j], pI=pIs[j][0]: e.tensor_tensor(out=wi[:, sl], in0=pI[:, :], in1=nC[:, sl], op=ALU.mult), [pIs[j][1], b_nC], [b_wis[cs[j]]])
                    for j in range(2):
                        dv(lambda e, sl=sls[j], pR=pRs[j][0], tb=tq[j][2]: e.tensor_tensor(out=tb, in0=pR[:, :], in1=nS[:, sl], op=ALU.mult), [pRs[j][1], b_nS], [tq[j][3]])
                    for j in range(2):
                        dv(lambda e, sl=sls[j], tb=tq[j][2]: e.tensor_tensor(out=wi[:, sl], in0=wi[:, sl], in1=tb, op=ALU.subtract), [b_wis[cs[j]], tq[j][3]], [b_wis[cs[j]]])
                dv(lambda e: e.tensor_tensor_scan(out=wr[:], data0=rho_bc, data1=wr[:], initial=0.0, op0=ALU.mult, op1=ALU.add), [b_rhoB] + b_wrs, b_wrs)
                dv(lambda e: e.tensor_tensor_scan(out=wi[:], data0=rho_bc, data1=wi[:], initial=0.0, op0=ALU.mult, op1=ALU.add), [b_rhoB] + b_wis, b_wis)
                hs = [slice(0, 1024), slice(1024, 2048)]
                TA = [(tas[j][0], tas[j][1]) for j in range(2)]
                TB = [(tbs[j][0], tbs[j][1]) for j in range(2)]
                bw = [b_wrs[0:2], b_wrs[2:4]]
                bi = [b_wis[0:2], b_wis[2:4]]
                for j in range(2):
                    dv(lambda e, h=hs[j], ta=TA[j][0]: e.tensor_tensor(out=ta[:], in0=wr[:, h], in1=nC[:, h], op=ALU.mult), bw[j] + [b_nC], [TA[j][1]])
                for j in range(2):
                    dv(lambda e, h=hs[j], tb=TB[j][0]: e.tensor_tensor(out=tb[:], in0=wi[:, h], in1=nS[:, h], op=ALU.mult), bi[j] + [b_nS], [TB[j][1]])
                for j in range(2):
                    dv(lambda e, h=hs[j], ta=TA[j][0], tb=TB[j][0]: e.tensor_tensor(out=xr[:, h], in0=ta[:], in1=tb[:], op=ALU.subtract), [TA[j][1], TB[j][1]], [b_xrs[sl_][j]])
                for j in range(2):
                    dv(lambda e, h=hs[j], ta=TA[j][0]: e.tensor_tensor(out=ta[:], in0=wr[:, h], in1=nS[:, h], op=ALU.mult), bw[j] + [b_nS], [TA[j][1]])
                for j in range(2):
                    dv(lambda e, h=hs[j], tb=TB[j][0]: e.tensor_tensor(out=tb[:], in0=wi[:, h], in1=nC[:, h], op=ALU.mult), bi[j] + [b_nC], [TB[j][1]])
                for j in range(2):
                    dv(lambda e, h=hs[j], ta=TA[j][0], tb=TB[j][0]: e.tensor_tensor(out=xi[:, h], in0=ta[:], in1=tb[:], op=ALU.add), [TA[j][1], TB[j][1]], [b_xis[sl_][j]])
                for ch in range(4):
                    sl = slice(ch * 512, (ch + 1) * 512)
                    pY, b_pY = psY[ch]
                    if r == 0:
                        P.emit("pe", lambda e, sl=sl, pY=pY, k=k, ub=ub: e.matmul(out=pY[:, :], lhsT=Dg[k][0][:], rhs=ub[:, sl], start=True, stop=False),
                               reads=[Dg[k][1], b_ub], writes=[b_pY])
                    P.emit("pe", lambda e, r=r, sl=sl, pY=pY, k=k, xr=xr: e.matmul(out=pY[:, :], lhsT=CT[k][0][0][:, r, :], rhs=xr[:, sl], start=False, stop=False),
                           reads=[CT[k][0][1], b_xrs[sl_][ch // 2]], writes=[b_pY])
                    P.emit("pe", lambda e, r=r, sl=sl, pY=pY, k=k, xi=xi: e.matmul(out=pY[:, :], lhsT=CT[k][1][0][:, r, :], rhs=xi[:, sl], start=False, stop=(r == 3)),
                           reads=[CT[k][1][1], b_xis[sl_][ch // 2]], writes=[b_pY])

            def prep(ct):
                k = ct % 2
                i3 = wqc["n"] % 3
                wqc["n"] += 1
                wt, b_wt = wq[i3]
                ub, b_ub = ubs[k]
                P.dma("pool", wt[:], w_in[:, ct * 128:(ct + 1) * 128].rearrange("(kc p) n -> p kc n", p=128), kwq[i3], writes=[b_wt])

                def ev_u(ch, ps, b_ps):
                    sl = slice(ch * 512, (ch + 1) * 512)
                    P.emit("act", lambda e, ps=ps, sl=sl, ub=ub: e.copy(out=ub[:, sl], in_=ps[:, :]), reads=[b_ps], writes=[b_ub])
                proj_fm(P, hT, b_hT, wt, b_wt, psA[0:2], ev_u)
                dg_, b_dg = Dg[k]
                P.emit("dve", lambda e, dg_=dg_, ct=ct: e.tensor_scalar(out=dg_[:], in0=ident[:], scalar1=dsk[:, ct:ct + 1], scalar2=None, op0=ALU.mult),
                       reads=[b_id, b_dsk], writes=[b_dg])
                for ri in range(2):
                    src = (bbr, bbi)[ri]
                    b_src = (b_bbr, b_bbi)[ri]
                    bf_, b_bf = Bf[k][ri]
                    for r in range(4):
                        gp = 4 * ct + r
                        for two in range(2):
                            col0 = (2 * r + two) * 16
                            P.emit("pool", lambda e, bf_=bf_, r=r, two=two, col0=col0, gp=gp, src=src: e.tensor_copy(
                                out=bf_[two * 64:(two + 1) * 64, r, col0:col0 + 16], in_=src[two * 64:(two + 1) * 64, gp, :]),
                                reads=[b_src, b_bf], writes=[b_bf])
                    for r in range(4):
                        P.emit("pe", lambda e, bf_=bf_, r=r: e.transpose(out=psTb[:, r, :], in_=bf_[:, r, :], identity=ident[:]),
                               reads=[b_bf, b_id], writes=[b_psTb])
                    bt_, b_bt = BT[k][ri]
                    P.emit("act", lambda e, bt_=bt_: e.copy(out=bt_[:], in_=psTb[:, 0:4, :]), reads=[b_psTb], writes=[b_bt])
                    cn_, b_cn = Cn[ri]
                    cnb_, b_cnb = Cnb[ri]
                    csrc = W["c_re"] if ri == 0 else W["c_im"]
                    P.dma("sp", cn_[:], csrc[ct * 8:(ct + 1) * 8].rearrange("g c p -> (g c) p"), kcn[ri], writes=[b_cn])
                    for two in range(2):
                        P.emit("act", lambda e, cn_=cn_, cnb_=cnb_, ri=ri, two=two: e.activation(out=cnb_[:, two * 64:(two + 1) * 64], in_=cn_[:], func=AF.Identity,
                                                                                                 scale=(1.0 if ri == 0 else -1.0)),
                               reads=[b_cn, b_cnb], writes=[b_cnb])
                    P.emit("pe", lambda e, cnb_=cnb_, ri=ri: e.transpose(out=psTb[:, 4 + ri, :], in_=cnb_[:, :], identity=ident[:]),
                           reads=[b_cnb, b_id], writes=[b_psTb])
                    ct_, b_ctt = CT[k][ri]
                    for r in range(4):
                        for two in range(2):
                            col0 = (2 * r + two) * 16
                            P.emit("act", lambda e, ct_=ct_, r=r, two=two, col0=col0, ri=ri: e.copy(
                                out=ct_[two * 64:(two + 1) * 64, r, col0:col0 + 16], in_=psTb[two * 64:(two + 1) * 64, 4 + ri, col0:col0 + 16]),
                                reads=[b_psTb, b_ctt], writes=[b_ctt])

            def gelu_out(ct):
                for ch in range(4):
                    sl = slice(ch * 512, (ch + 1) * 512)
                    pY, b_pY = psY[ch]
                    tb, b_tb = tbs[ch % 2][0][:, 0:512], tbs[ch % 2][1]
                    P.emit("act", lambda e, pY=pY, tb=tb: e.activation(out=tb, in_=pY[:, :], func=AF.Square), reads=[b_pY], writes=[b_tb])
                    P.emit("dve", lambda e, tb=tb: e.tensor_scalar(out=tb, in0=tb, scalar1=0.044715, scalar2=1.0, op0=ALU.mult, op1=ALU.add), reads=[b_tb], writes=[b_tb])
                    P.emit("dve", lambda e, tb=tb, pY=pY: e.tensor_tensor(out=tb, in0=pY[:, :], in1=tb, op=ALU.mult), reads=[b_tb, b_pY], writes=[b_tb])
                    P.emit("act", lambda e, tb=tb: e.activation(out=tb, in_=tb, func=AF.Sigmoid, scale=GELU_K), reads=[b_tb], writes=[b_tb])
                    P.emit("dve", lambda e, sl=sl, tb=tb, pY=pY: e.tensor_tensor(out=yb[:, sl], in0=pY[:, :], in1=tb, op=ALU.mult), reads=[b_tb, b_pY], writes=[b_yb])
                P.dma("sp", yscr[ct * 128:(ct + 1) * 128, :], yb[:], kyb, reads=[b_yb], writes=[P.dram_buf(yscr, 0)])

            prep(0)
            tables(0)
            for ct in range(16):
                for r in range(4):
                    gp = 4 * ct + r
                    if r == 1 and ct + 1 < 16:
                        prep(ct + 1)
                    if gp + 1 < 64:
                        tables(gp + 1)
                    rotate(gp, r)
                gelu_out(ct)
            barrier(P)
            M.close()

        main_phase()
        O.close()
        if STOP[0] >= 11:
            return

        def comb(P, cur, x_t, b_x, c, tmp):
            (pv, b_pv), (pg, b_pg) = cur
            tm, b_tm = tmp
            csl = slice(c * 512, (c + 1) * 512)
            P.emit("act", lambda e, pg=pg, tm=tm: e.activation(out=tm[:], in_=pg[:, :], func=AF.Sigmoid), reads=[b_pg], writes=[b_tm])
            P.emit("dve", lambda e, pv=pv, tm=tm: e.tensor_tensor(out=tm[:], in0=pv[:, :], in1=tm[:], op=ALU.mult), reads=[b_pv, b_tm], writes=[b_tm])
            P.emit("dve", lambda e, tm=tm, x_t=x_t, csl=csl: e.tensor_tensor(out=x_t[:, csl], in0=x_t[:, csl], in1=tm[:], op=ALU.add),
                   reads=[b_tm, b_x], writes=[b_x])
        outproj_phase(P, tag, xin, xout, base, yscr, [W["w_glu_v"], W["w_glu_g"]], comb)
        barrier(P)

    for sq in range(NSEQ):
        seq_body(sq)


NSEQ_CORE = 2
T_CORE = NSEQ_CORE * TS
N_CORES = 8

_SPECS = [
    ("x", [T_CORE, D]), ("norm_mix", [2, D]), ("norm_ffn", [2, D]), ("norm_final", [1, D]),
    ("ab_w_in", [D, 6152]), ("lru_conv_w", [4, 1024]), ("lru_conv_b", [1024]), ("lru_w_a", [8, 128, 128]),
    ("lru_b_a", [1024]), ("lru_w_x", [8, 128, 128]), ("lru_b_x", [1024]), ("lru_lam", [1024]),
    ("m_conv_w", [4, 2048]), ("m_conv_b", [2048]), ("m_i_bias", [1, 4]), ("m_f_bias", [1, 4]), ("m_head_g", [1, 1024]),
    ("ab_w_out", [D, D]), ("s5_w_in", [D, D]), ("s5_a_re", [128, 64]), ("s5_a_im", [128, 64]), ("s5_log_step", [128]),
    ("s5_b_re", [128, 64, 16]), ("s5_b_im", [128, 64, 16]), ("s5_c_re", [128, 16, 64]), ("s5_c_im", [128, 16, 64]),
    ("s5_d", [2048]), ("s5_w_glu_v", [D, D]), ("s5_w_glu_g", [D, D]),
    ("moe_w_coarse", [2, D, 4]), ("moe_b_coarse", [2, 4]), ("moe_w_fine", [2, D, 16]), ("moe_b_fine", [2, 16]),
    ("moe_w_gate", [2, NEXP, D, FF]), ("moe_w_up", [2, NEXP, D, FF]), ("moe_w_down", [2, NEXP, FF, D]),
]


def build_program():
    nc = bass.Bass("TRN2", target_bir_lowering=False)
    A = {n: nc.dram_tensor(n, list(shp), F32, kind="ExternalInput").ap() for n, shp in _SPECS}
    out = nc.dram_tensor("out", [T_CORE, D], F32, kind="ExternalOutput").ap()
    xs = [nc.dram_tensor(f"xs{i}", [T_CORE, D], F32, kind="Internal").ap() for i in range(3)]
    yscr = [nc.dram_tensor(f"yscr{i}", [D, TS], BF16, kind="Internal").ap() for i in range(NSEQ_CORE)]
    tabscr = nc.dram_tensor("s5tab", [64, 2, 128, TS], F32, kind="Internal").ap()
    P = Prog(nc)
    WA = {"norm": A["norm_mix"][0:1, :], "w_in": A["ab_w_in"], "lru_conv_w": A["lru_conv_w"], "lru_conv_b": A["lru_conv_b"],
          "lru_w_a": A["lru_w_a"], "lru_b_a": A["lru_b_a"], "lru_w_x": A["lru_w_x"], "lru_b_x": A["lru_b_x"],
          "lru_lam": A["lru_lam"], "m_conv_w": A["m_conv_w"], "m_conv_b": A["m_conv_b"], "m_i_bias": A["m_i_bias"],
          "m_f_bias": A["m_f_bias"], "m_head_g": A["m_head_g"], "w_out": A["ab_w_out"]}
    WS = {"norm": A["norm_mix"][1:2, :], "w_in": A["s5_w_in"], "a_re": A["s5_a_re"], "a_im": A["s5_a_im"],
          "log_step": A["s5_log_step"], "b_re": A["s5_b_re"], "b_im": A["s5_b_im"], "c_re": A["s5_c_re"],
          "c_im": A["s5_c_im"], "d": A["s5_d"], "w_glu_v": A["s5_w_glu_v"], "w_glu_g": A["s5_w_glu_g"]}

    def moe(layer, xi, xo, gfin):
        moe_stage(P, xi, xo, T_CORE, A["norm_ffn"][layer:layer + 1, :], A["moe_w_coarse"][layer],
                  A["moe_b_coarse"][layer:layer + 1, :], A["moe_w_fine"][layer], A["moe_b_fine"][layer:layer + 1, :],
                  A["moe_w_gate"][layer], A["moe_w_up"][layer], A["moe_w_down"][layer], g_final=gfin)
        barrier(P)

    mixer_a_stage(P, A["x"], xs[0], NSEQ_CORE, WA, yscr)
    moe(0, xs[0], xs[1], None)
    s5_stage(P, xs[1], xs[2], NSEQ_CORE, WS, yscr, tabscr)
    moe(1, xs[2], out, A["norm_final"])
    P.finish()
    P.build()
    P.close()
    return nc


def kernel(**inputs):
    f = lambda a: np.ascontiguousarray(np.asarray(a, dtype=np.float32))
    x = f(inputs["x"])
    B = x.shape[0]
    shared = {}
    for n, shp in _SPECS:
        if n == "x":
            continue
        shared[n] = f(inputs[n]).reshape(shp)
    nc = build_program()
    in_maps = []
    for c in range(N_CORES):
        m = dict(shared)
        m["x"] = x[c * NSEQ_CORE:(c + 1) * NSEQ_CORE].reshape(T_CORE, D)
        in_maps.append(m)
    res = run_bass_kernel_spmd(nc, in_maps, core_ids=list(range(N_CORES)))
    outs = [np.asarray(r["out"], dtype=np.float32).reshape(NSEQ_CORE, TS, D) for r in res.results]
    return np.concatenate(outs, axis=0)
```
